# Optimizing a Trainium2 kernel written in Bass

```python
import math
import jax
import jax.numpy as jnp
from jax import lax
import numpy as np

D_MODEL = 1024
BATCH = 4
SEQ = 8192
DEPTH = 4

GRID_W = 64
CTX_LEN = 256
HEAD_DIM = 64
N_SLOTS = D_MODEL // HEAD_DIM
CONV_CH = D_MODEL // 2
CONV_W = 3
NA_HEADS = N_SLOTS // 2
NA_ROWS = 8
NA_COLS = 16
SWA_HEADS = N_SLOTS // 2
SWA_KV_HEADS = max(1, SWA_HEADS // 4)
SWA_WINDOW = 128
BLOCK = 128
DIFF_HEADS = N_SLOTS // 4
D_FF = 256 * math.ceil(8 * D_MODEL / 3 / 256)
N_EXPERTS = 8
TOP_K = 2
D_FF_EXPERT = D_FF // 2
ROPE_THETA = 10000.0
EPS = 1e-6
NEG = -1e30
N_EVEN = (DEPTH + 1) // 2
N_ODD = DEPTH // 2
NA_W = NA_HEADS * HEAD_DIM
EV_Q = 3 * CONV_CH + NA_W
EV_KV = 2 * NA_W
EV_OUT = CONV_CH + NA_W
C_Q = SWA_HEADS * HEAD_DIM
C_KV = SWA_KV_HEADS * HEAD_DIM
D_QK = DIFF_HEADS * 2 * HEAD_DIM
D_V = DIFF_HEADS * 2 * HEAD_DIM
OD_Q = C_Q + D_QK
OD_KV = 2 * C_KV + D_QK + D_V
OD_OUT = C_Q + D_V

kernel_name = 'hybrid_dit_conv_na_swa_diff_moe'


def rms_norm(x, g):
    xf = x.astype(jnp.float32)
    y = xf * lax.rsqrt(jnp.mean(xf * xf, -1, keepdims=True) + EPS)
    return (y * g.astype(jnp.float32)).astype(x.dtype)


def split_heads(t, h):
    b, n, _ = t.shape
    return t.reshape(b, n, h, -1).transpose(0, 2, 1, 3)


def merge_heads(t):
    b, h, n, d = t.shape
    return t.transpose(0, 2, 1, 3).reshape(b, n, h * d)


def rope_tables(n):
    t = jnp.arange(n)
    pos = jnp.stack([t // GRID_W, t % GRID_W], -1).astype(jnp.float32)
    nq = HEAD_DIM // 4
    inv = ROPE_THETA ** (-jnp.arange(nq, dtype=jnp.float32) / nq)
    ang = pos[:, :, None] * inv
    return jnp.cos(ang), jnp.sin(ang)


def apply_rope(x, cos, sin):
    shp = x.shape
    xr = x.reshape(shp[:-1] + (2, 2, HEAD_DIM // 4))
    a, b = xr[..., 0, :], xr[..., 1, :]
    cos = cos.astype(x.dtype)
    sin = sin.astype(x.dtype)
    return jnp.stack([a * cos - b * sin, b * cos + a * sin], -2).reshape(shp)


def joint_softmax(parts, extra=None):
    m = parts[0].max(-1, keepdims=True)
    for p in parts[1:]:
        m = jnp.maximum(m, p.max(-1, keepdims=True))
    if extra is not None:
        m = jnp.maximum(m, extra)
    es = [jnp.exp(p - m) for p in parts]
    den = es[0].sum(-1, keepdims=True)
    for e in es[1:]:
        den = den + e.sum(-1, keepdims=True)
    if extra is not None:
        den = den + jnp.exp(extra - m)
    return [e / den for e in es]


def dense_attention(q, k, v, sink=None):
    b, hq, lq, d = q.shape
    hkv = k.shape[1]
    g = hq // hkv
    qg = q.reshape(b, hkv, g, lq, d)
    s = jnp.einsum('bkgqd,bkld->bkgql', qg, k, preferred_element_type=jnp.float32) * d ** -0.5
    extra = None if sink is None else sink.astype(jnp.float32).reshape(1, hkv, g, 1, 1)
    (p,) = joint_softmax([s], extra)
    o = jnp.einsum('bkgql,bkld->bkgqd', p.astype(v.dtype), v)
    return o.reshape(b, hq, lq, d)


def dwconv3(u, w):
    return lax.conv_general_dilated(u, w[:, None, :].astype(u.dtype), window_strides=(1,),
                                    padding=[(1, 1)], dimension_numbers=('NWC', 'WIO', 'NWC'),
                                    feature_group_count=u.shape[-1])


def neighbourhood_attention(q, k, v, k_ctx, v_ctx, rpb):
    b, h, n, d = q.shape
    rows = n // GRID_W
    kh = min(NA_ROWS, rows)
    r = jnp.arange(rows)
    col = jnp.arange(GRID_W)
    r0 = jnp.clip(r - kh // 2, 0, rows - kh)
    row_idx = r0[:, None] + jnp.arange(kh)[None, :]
    c0 = jnp.clip(col - NA_COLS // 2, 0, GRID_W - NA_COLS)
    qg = q.reshape(b, h, rows, GRID_W, d)
    kg = k.reshape(b, h, rows, GRID_W, d)[:, :, row_idx]
    vg = v.reshape(b, h, rows, GRID_W, d)[:, :, row_idx]
    dr = row_idx - r[:, None] + NA_ROWS - 1
    dc = jnp.clip(col[None, :] - col[:, None] + NA_COLS - 1, 0, 2 * NA_COLS - 2)
    col_ok = (col[None, :] >= c0[:, None]) & (col[None, :] < c0[:, None] + NA_COLS)
    bias = rpb.astype(jnp.float32)[:, dr[:, None, :, None], dc[None, :, None, :]]
    bias = jnp.where(col_ok[None, None, :, None, :], bias, NEG)
    scale = d ** -0.5
    s_loc = jnp.einsum('bhrqd,bhrjkd->bhrqjk', qg, kg, preferred_element_type=jnp.float32) * scale + bias
    s_ctx = jnp.einsum('bhrqd,bhld->bhrql', qg, k_ctx, preferred_element_type=jnp.float32) * scale
    p_loc, p_ctx = joint_softmax([s_loc.reshape(b, h, rows, GRID_W, kh * GRID_W), s_ctx])
    p_loc = p_loc.reshape(b, h, rows, GRID_W, kh, GRID_W).astype(v.dtype)
    o = (jnp.einsum('bhrqjk,bhrjkd->bhrqd', p_loc, vg)
         + jnp.einsum('bhrql,bhld->bhrqd', p_ctx.astype(v.dtype), v_ctx))
    return o.reshape(b, h, n, d)


def window_gqa(q, k, v, k_ctx, v_ctx, sink):
    b, hq, n, d = q.shape
    hkv = k.shape[1]
    g = hq // hkv
    nb = n // BLOCK
    qb = q.reshape(b, hkv, g, nb, BLOCK, d)
    pad = ((0, 0), (0, 0), (BLOCK, BLOCK), (0, 0))
    band = (jnp.arange(nb) * BLOCK)[:, None] + jnp.arange(3 * BLOCK)[None, :]
    kb = jnp.pad(k, pad)[:, :, band]
    vb = jnp.pad(v, pad)[:, :, band]
    qpos = (jnp.arange(nb) * BLOCK)[:, None, None] + jnp.arange(BLOCK)[None, :, None]
    kpos = band[:, None, :] - BLOCK
    ok = (jnp.abs(kpos - qpos) <= SWA_WINDOW) & (kpos >= 0) & (kpos < n)
    scale = d ** -0.5
    s_loc = jnp.where(ok, jnp.einsum('bkgnqd,bknmd->bkgnqm', qb, kb, preferred_element_type=jnp.float32) * scale, NEG)
    s_ctx = jnp.einsum('bkgnqd,bkld->bkgnql', qb, k_ctx, preferred_element_type=jnp.float32) * scale
    extra = sink.astype(jnp.float32).reshape(1, hkv, g, 1, 1, 1)
    p_loc, p_ctx = joint_softmax([s_loc, s_ctx], extra)
    o = (jnp.einsum('bkgnqm,bknmd->bkgnqd', p_loc.astype(v.dtype), vb)
         + jnp.einsum('bkgnql,bkld->bkgnqd', p_ctx.astype(v.dtype), v_ctx))
    return o.reshape(b, hq, n, d)


def diff_attend(q, kvs, lam):
    scale = q.shape[-1] ** -0.5
    ss = [jnp.einsum('bhiqd,bhikd->bhiqk', q, k, preferred_element_type=jnp.float32) * scale for k, _ in kvs]
    ps = joint_softmax(ss)
    out = None
    for p, (_, v) in zip(ps, kvs):
        a = (p[:, :, 0] - lam * p[:, :, 1]).astype(v.dtype)
        o = jnp.einsum('bhqk,bhkd->bhqd', a, v)
        out = o if out is None else out + o
    return out


def diff_attention_latent(q, k, v, k_ctx, v_ctx, lam):
    b, h, _, n, d = q.shape
    nb = n // BLOCK
    qb = jnp.moveaxis(q.reshape(b, h, 2, nb, BLOCK, d), 3, 0)
    o = lax.map(lambda qi: diff_attend(qi, [(k, v), (k_ctx, v_ctx)], lam), qb)
    return jnp.moveaxis(o, 0, 2).reshape(b, h, n, v.shape[-1])


def swiglu(x, w1, w3, w2):
    return (jax.nn.silu(x @ w1) * (x @ w3)) @ w2


def moe_swiglu(x, router, w1, w3, w2):
    shp = x.shape
    xt = x.reshape(-1, shp[-1])
    logits = (xt @ router).astype(jnp.float32)
    top_v, top_i = lax.top_k(logits, TOP_K)
    top_w = jax.nn.softmax(top_v, axis=-1)
    gates = jnp.sum(jax.nn.one_hot(top_i, N_EXPERTS, dtype=jnp.float32) * top_w[..., None], axis=1)
    gates = gates.astype(x.dtype)
    out = gates[:, 0:1] * swiglu(xt, w1[0], w3[0], w2[0])
    for e in range(1, N_EXPERTS):
        out = out + gates[:, e:e + 1] * swiglu(xt, w1[e], w3[e], w2[e])
    return out.reshape(shp)


def even_mixer(h_lat, h_ctx, w_in, conv_w, q_g, k_g, rpb, w_out, need_ctx):
    def query_side(p):
        gb, gc, u, q = jnp.split(p, [CONV_CH, 2 * CONV_CH, 3 * CONV_CH], -1)
        y_conv = gb * dwconv3(gc * u, conv_w)
        return y_conv, rms_norm(split_heads(q, NA_HEADS), q_g)

    def kv_side(p):
        k, v = jnp.split(p, 2, -1)
        return rms_norm(split_heads(k, NA_HEADS), k_g), split_heads(v, NA_HEADS)

    p_lat = h_lat @ w_in
    yc_lat, q_lat = query_side(p_lat[..., :EV_Q])
    k_lat, v_lat = kv_side(p_lat[..., EV_Q:])
    p_ctx = h_ctx @ (w_in if need_ctx else w_in[:, EV_Q:])
    k_ctx, v_ctx = kv_side(p_ctx[..., -EV_KV:])
    y_na = merge_heads(neighbourhood_attention(q_lat, k_lat, v_lat, k_ctx, v_ctx, rpb))
    y_lat = jnp.concatenate([yc_lat, y_na], -1) @ w_out
    y_ctx = None
    if need_ctx:
        yc_ctx, q_ctx = query_side(p_ctx[..., :EV_Q])
        y_na_ctx = merge_heads(dense_attention(q_ctx, k_ctx, v_ctx))
        y_ctx = jnp.concatenate([yc_ctx, y_na_ctx], -1) @ w_out
    return y_lat, y_ctx


def odd_mixer(h_lat, h_ctx, w_in, cq_g, ck_g, sink, dq_g, dk_g, lam_q1, lam_k1, lam_q2, lam_k2,
              subln_g, w_out, lam_init, cos, sin, need_ctx):
    def diff_split(t):
        b, n, _ = t.shape
        return t.reshape(b, n, DIFF_HEADS, 2, HEAD_DIM).transpose(0, 2, 3, 1, 4)

    def query_side(p, rope):
        cq, dq = jnp.split(p, [C_Q], -1)
        cq = rms_norm(split_heads(cq, SWA_HEADS), cq_g)
        dq = rms_norm(diff_split(dq), dq_g)
        if rope:
            cq, dq = apply_rope(cq, cos, sin), apply_rope(dq, cos, sin)
        return cq, dq

    def kv_side(p, rope):
        ck, cv, dk, dv = jnp.split(p, [C_KV, 2 * C_KV, 2 * C_KV + D_QK], -1)
        ck = rms_norm(split_heads(ck, SWA_KV_HEADS), ck_g)
        dk = rms_norm(diff_split(dk), dk_g)
        if rope:
            ck, dk = apply_rope(ck, cos, sin), apply_rope(dk, cos, sin)
        return ck, split_heads(cv, SWA_KV_HEADS), dk, split_heads(dv, DIFF_HEADS)

    f32 = jnp.float32
    lam = (jnp.exp(jnp.sum(lam_q1.astype(f32) * lam_k1.astype(f32)))
           - jnp.exp(jnp.sum(lam_q2.astype(f32) * lam_k2.astype(f32))) + lam_init)

    def merge(yc, yd):
        yd = rms_norm(yd, subln_g) * (1.0 - lam_init)
        return jnp.concatenate([merge_heads(yc), merge_heads(yd)], -1) @ w_out

    p_lat = h_lat @ w_in
    cq_l, dq_l = query_side(p_lat[..., :OD_Q], True)
    ck_l, cv_l, dk_l, dv_l = kv_side(p_lat[..., OD_Q:], True)
    p_ctx = h_ctx @ (w_in if need_ctx else w_in[:, OD_Q:])
    ck_c, cv_c, dk_c, dv_c = kv_side(p_ctx[..., -OD_KV:], False)
    y_lat = merge(window_gqa(cq_l, ck_l, cv_l, ck_c, cv_c, sink),
                  diff_attention_latent(dq_l, dk_l, dv_l, dk_c, dv_c, lam))
    y_ctx = None
    if need_ctx:
        cq_c, dq_c = query_side(p_ctx[..., :OD_Q], False)
        y_ctx = merge(dense_attention(cq_c, ck_c, cv_c, sink), diff_attend(dq_c, [(dk_c, dv_c)], lam))
    return y_lat, y_ctx


def setup_inputs(seed: int = 0) -> dict:
    key = jax.random.key(seed)
    ks = iter(jax.random.split(key, 40))
    D = D_MODEL

    def nrm(shape, scale=1.0):
        return jax.random.normal(next(ks), shape, jnp.float32) * scale

    def gain(shape):
        return 1.0 + nrm(shape, 0.05)

    return {
        'x': nrm((BATCH, SEQ, D)),
        'c': nrm((BATCH, D)),
        'ctx': nrm((BATCH, CTX_LEN, D)),
        'c_ctx': nrm((D,)),
        'ada_w': nrm((DEPTH, D, 6 * D), 0.5 * D ** -0.5),
        'ada_b': nrm((DEPTH, 6 * D), 0.02),
        'norm1_g': gain((DEPTH, D)),
        'norm2_g': gain((DEPTH, D)),
        'ev_w_in': nrm((N_EVEN, D, EV_Q + EV_KV), D ** -0.5),
        'ev_conv_w': nrm((N_EVEN, CONV_W, CONV_CH), CONV_W ** -0.5),
        'ev_q_g': gain((N_EVEN, HEAD_DIM)),
        'ev_k_g': gain((N_EVEN, HEAD_DIM)),
        'ev_rpb': nrm((N_EVEN, NA_HEADS, 2 * NA_ROWS - 1, 2 * NA_COLS - 1), 0.1),
        'ev_w_out': nrm((N_EVEN, EV_OUT, D), EV_OUT ** -0.5),
        'ffn_w1': nrm((N_EVEN, D, D_FF), D ** -0.5),
        'ffn_w3': nrm((N_EVEN, D, D_FF), D ** -0.5),
        'ffn_w2': nrm((N_EVEN, D_FF, D), D_FF ** -0.5),
        'od_w_in': nrm((N_ODD, D, OD_Q + OD_KV), D ** -0.5),
        'od_cq_g': gain((N_ODD, HEAD_DIM)),
        'od_ck_g': gain((N_ODD, HEAD_DIM)),
        'od_sink': nrm((N_ODD, SWA_HEADS), 0.5),
        'od_dq_g': gain((N_ODD, HEAD_DIM)),
        'od_dk_g': gain((N_ODD, HEAD_DIM)),
        'od_lam_q1': nrm((N_ODD, HEAD_DIM), 0.1),
        'od_lam_k1': nrm((N_ODD, HEAD_DIM), 0.1),
        'od_lam_q2': nrm((N_ODD, HEAD_DIM), 0.1),
        'od_lam_k2': nrm((N_ODD, HEAD_DIM), 0.1),
        'od_subln_g': gain((N_ODD, 2 * HEAD_DIM)),
        'od_w_out': nrm((N_ODD, OD_OUT, D), OD_OUT ** -0.5),
        'moe_router': nrm((N_ODD, D, N_EXPERTS), D ** -0.5),
        'moe_w1': nrm((N_ODD, N_EXPERTS, D, D_FF_EXPERT), D ** -0.5),
        'moe_w3': nrm((N_ODD, N_EXPERTS, D, D_FF_EXPERT), D ** -0.5),
        'moe_w2': nrm((N_ODD, N_EXPERTS, D_FF_EXPERT, D), D_FF_EXPERT ** -0.5),
    }


def reference(x, c, ctx, c_ctx, ada_w, ada_b, norm1_g, norm2_g, ev_w_in, ev_conv_w, ev_q_g, ev_k_g,
              ev_rpb, ev_w_out, ffn_w1, ffn_w3, ffn_w2, od_w_in, od_cq_g, od_ck_g, od_sink, od_dq_g,
              od_dk_g, od_lam_q1, od_lam_k1, od_lam_q2, od_lam_k2, od_subln_g, od_w_out, moe_router,
              moe_w1, moe_w3, moe_w2):
    n = x.shape[1]
    cos, sin = rope_tables(n)
    h, hc = x, ctx
    s_lat = jax.nn.silu(c)
    s_ctx = jax.nn.silu(c_ctx)
    for l in range(DEPTH):
        last = l == DEPTH - 1
        i = l // 2
        mod = (s_lat @ ada_w[l] + ada_b[l])[:, None, :]
        sh1, sc1, g1, sh2, sc2, g2 = jnp.split(mod, 6, -1)
        n_mod = 2 if last else 6
        modc = jnp.split(s_ctx @ ada_w[l][:, :n_mod * D_MODEL] + ada_b[l][:n_mod * D_MODEL], n_mod)
        a_lat = rms_norm(h, norm1_g[l]) * (1 + sc1) + sh1
        a_ctx = rms_norm(hc, norm1_g[l]) * (1 + modc[1]) + modc[0]
        if l % 2 == 0:
            y, yc = even_mixer(a_lat, a_ctx, ev_w_in[i], ev_conv_w[i], ev_q_g[i], ev_k_g[i], ev_rpb[i],
                               ev_w_out[i], not last)
            ffn = lambda t: swiglu(t, ffn_w1[i], ffn_w3[i], ffn_w2[i])
        else:
            lam_init = 0.8 - 0.6 * math.exp(-0.3 * l)
            y, yc = odd_mixer(a_lat, a_ctx, od_w_in[i], od_cq_g[i], od_ck_g[i], od_sink[i], od_dq_g[i],
                              od_dk_g[i], od_lam_q1[i], od_lam_k1[i], od_lam_q2[i], od_lam_k2[i],
                              od_subln_g[i], od_w_out[i], lam_init, cos, sin, not last)
            ffn = lambda t: moe_swiglu(t, moe_router[i], moe_w1[i], moe_w3[i], moe_w2[i])
        h = h + g1 * y
        h = h + g2 * ffn(rms_norm(h, norm2_g[l]) * (1 + sc2) + sh2)
        if not last:
            hc = hc + modc[2] * yc
            hc = hc + modc[5] * ffn(rms_norm(hc, norm2_g[l]) * (1 + modc[4]) + modc[3])
    return h
```

```python
import math
import os
from contextlib import ExitStack

import numpy as np
import concourse.bass as bass
import concourse.mybir as mybir
from concourse.bass_utils import run_bass_kernel_spmd

F32 = mybir.dt.float32
BF16 = mybir.dt.bfloat16
AF = mybir.ActivationFunctionType
ALU = mybir.AluOpType
AX = mybir.AxisListType

D = 1024
NLAT = 8192
NCTX = 256
T = NLAT + NCTX
NTILE = T // 128
NT = NLAT // 128
NST = NLAT // 512
DEPTH = 4
EPS = 1e-6
DFF = 2816
DFE = 1408
NEXP = 8
SAME_ENGINE_SYNC = True
NCORES = 4
PAIRS = [[0, 1], [2, 3], [4, 5], [6, 7]]


class Buf:
    __slots__ = ("name", "w", "r", "sem", "cnt")

    def __init__(self, name):
        self.name = name
        self.w = {}
        self.r = {}
        self.sem = None
        self.cnt = 0


class Eng:
    def __init__(self, name, handle, sem):
        self.name = name
        self.h = handle
        self.sem = sem
        self.cnt = 0
        self.known = {}


def _merge(d, s):
    for k, v in s.items():
        if d.get(k, 0) < v:
            d[k] = v


class Sched:
    def __init__(self, nc, es):
        self.nc = nc
        self.free_sems = []
        self.all_sems = []
        names = [("pe", nc.tensor), ("act", nc.scalar), ("dve", nc.vector), ("pool", nc.gpsimd), ("sp", nc.sync)]
        self.eng = {}
        for n, h in names:
            s = es.enter_context(nc.semaphore("e_" + n))
            self.eng[n] = Eng(n, h, s)
        for i in range(80):
            s = es.enter_context(nc.semaphore(f"d{i}"))
            self.free_sems.append([s, 0])
        self.semcnt = {}
        self.phase_bufs = []

    def buf(self, name):
        b = Buf(name)
        self.phase_bufs.append(b)
        return b

    def bufs(self, name, n):
        return [self.buf(f"{name}{i}") for i in range(n)]

    def _deps(self, e, reads, writes, partial):
        need = {}
        for b in reads:
            _merge(need, b.w)
        for b in writes:
            _merge(need, b.r)
            if not partial:
                _merge(need, b.w)
        for sem, val in need.items():
            if sem is e.sem and (e.name in ("pe", "sp") or not SAME_ENGINE_SYNC):
                continue
            if e.known.get(sem, 0) >= val:
                continue
            e.h.wait_ge(sem, val)
            e.known[sem] = val

    def _post(self, tok, reads, writes, partial):
        sem, val = tok
        for b in reads:
            if b.r.get(sem, 0) < val:
                b.r[sem] = val
        for b in writes:
            if partial:
                if b.w.get(sem, 0) < val:
                    b.w[sem] = val
            else:
                b.w = {sem: val}
                b.r = {}

    def op(self, en, fn, reads=(), writes=(), signal=True, partial=False):
        e = self.eng[en]
        self._deps(e, reads, writes, partial)
        inst = fn(e.h)
        if signal:
            e.cnt += 1
            inst.then_inc(e.sem, 1)
            tok = (e.sem, e.cnt)
        else:
            tok = (e.sem, e.cnt + 1)
        self._post(tok, reads, writes, partial)
        return inst

    def dma(self, en, out, in_, sembuf, reads=(), writes=(), partial=False, **kw):
        e = self.eng[en]
        self._deps(e, reads, writes, partial)
        if sembuf.sem is None:
            s = self.free_sems.pop()
            sembuf.sem, sembuf.cnt = s[0], s[1]
        inst = e.h.dma_start(out=out, in_=in_, **kw)
        sembuf.cnt += 16
        inst.then_inc(sembuf.sem, 16)
        tok = (sembuf.sem, sembuf.cnt)
        self.semcnt[id(sembuf.sem)] = tok
        self._post(tok, reads, writes, partial)
        return inst

    def collective(self, ins, outs, sembuf, reads=(), writes=()):
        e = self.eng["pool"]
        self._deps(e, reads, writes, False)
        if sembuf.sem is None:
            s = self.free_sems.pop()
            sembuf.sem, sembuf.cnt = s[0], s[1]
        inst = e.h.collective_compute("AllGather", ALU.bypass, replica_groups=PAIRS, ins=ins, outs=outs)
        sembuf.cnt += 1
        inst.then_inc(sembuf.sem, 1)
        tok = (sembuf.sem, sembuf.cnt)
        self.semcnt[id(sembuf.sem)] = tok
        self._post(tok, reads, writes, False)

    def barrier(self, release=True):
        toks = {}
        for e in self.eng.values():
            if e.cnt > 0:
                toks[e.sem] = e.cnt
        for sem, val in self.semcnt.values():
            toks[sem] = val
        for e in self.eng.values():
            for sem, val in toks.items():
                if sem is e.sem and e.name in ("pe", "sp"):
                    continue
                if e.known.get(sem, 0) >= val:
                    continue
                e.h.wait_ge(sem, val)
                e.known[sem] = val
        if release:
            for b in self.phase_bufs:
                if b.sem is not None:
                    self.free_sems.append([b.sem, b.cnt])
                    b.sem = None
                b.w = {}
                b.r = {}
            self.phase_bufs = []


def _na_tables():
    classes = []
    def tile_chunks(i):
        if i == 0:
            return 0, 6
        if i == NT - 1:
            return NT - 2, 6
        return i, 5
    order = [("I", 5), (0, 6), (1, 5), (NT - 2, 5), (NT - 1, 6)]
    dr = np.zeros((27, 128, 128), np.int64)
    dc = np.zeros((27, 128, 128), np.int64)
    mk = np.zeros((27, 128, 128), np.float32)
    kk = np.arange(128)[:, None]
    qq = np.arange(128)[None, :]
    t = 0
    for cls, n in order:
        i = 5 if cls == "I" else cls
        e0, n2 = tile_chunks(i)
        assert n2 == n
        g = i
        if cls == "I":
            g = 10
            e0 = None
        for ci in range(n):
            if cls == "I":
                m = g - 2 + ci
            else:
                m = e0 + ci - 2
            r = 2 * g + qq // 64
            qc = qq % 64
            kr = 2 * m + kk // 64
            kc = kk % 64
            r0 = np.clip(r - 4, 0, 120)
            c0 = np.clip(qc - 8, 0, 48)
            ok = (kr >= r0) & (kr <= r0 + 7) & (kc >= c0) & (kc < c0 + 16) & (m >= 0) & (m <= NT - 1)
            ok = np.broadcast_to(ok, (128, 128))
            dri = np.clip(kr - r + 7, 0, 14)
            dci = np.clip(kc - qc + 15, 0, 30)
            dr[t] = np.where(ok, dri, 0)
            dc[t] = np.where(ok, dci, 0)
            mk[t] = ok.astype(np.float32)
            t += 1
    assert t == 27
    return dr, dc, mk


def _rope_tables():
    t = np.arange(NLAT)
    pos = np.stack([t // 64, t % 64], -1).astype(np.float32)
    nq = 16
    inv = (np.float32(10000.0) ** (-np.arange(nq, dtype=np.float32) / nq)).astype(np.float32)
    ang = pos[:, :, None] * inv
    cos = np.cos(ang).astype(np.float32)
    sin = np.sin(ang).astype(np.float32)
    C = np.zeros((128, NLAT), np.float32)
    S = np.zeros((128, NLAT), np.float32)
    for hh in range(2):
        for ax in range(2):
            for hf in range(2):
                p0 = hh * 64 + ax * 32 + hf * 16
                C[p0:p0 + 16] = cos[:, ax, :].T
                S[p0:p0 + 16] = sin[:, ax, :].T
    return C, S


def _rot_matrix():
    R = np.zeros((128, 128), np.float32)
    for hh in range(2):
        for ax in range(2):
            for j in range(16):
                a = hh * 64 + ax * 32 + j
                b = a + 16
                R[a, b] = -1.0
                R[b, a] = 1.0
    return np.ascontiguousarray(R.T)


def _consts():
    c = {}
    c["ident"] = np.eye(128, dtype=np.float32)
    bo = np.zeros((128, 128), np.float32)
    bo[:64, :64] = 1.0
    bo[64:, 64:] = 1.0
    c["blockones"] = bo
    c["rotT"] = _rot_matrix()
    C, S = _rope_tables()
    c["ropeC"] = C
    c["ropeS"] = S
    kk = np.arange(128)[:, None]
    qq = np.arange(128)[None, :]
    prev = (kk >= qq).astype(np.float32)
    nxt = (kk <= qq).astype(np.float32)
    sm = np.stack([prev, nxt], 1)
    c["swamask"] = np.ascontiguousarray(sm)
    _, _, mk = _na_tables()
    c["namask"] = np.ascontiguousarray(mk.transpose(1, 0, 2))
    return c


class Builder:
    def __init__(self, nlayers=DEPTH, debug=False):
        self.nlayers = nlayers
        self.l0 = int(os.environ.get("KL0", "0"))
        self.debug = debug
        self.nc = bass.Bass("TRN2", target_bir_lowering=False)
        self.inputs = {}

    def din(self, name, shape, dt=F32):
        t = self.nc.dram_tensor(name, list(shape), dt, kind="ExternalInput")
        self.inputs[name] = t
        return t.ap()

    def dscr(self, name, shape, dt):
        return self.nc.dram_tensor(name, list(shape), dt).ap()

    def sb(self, es, name, shape, dt):
        self.uid = getattr(self, "uid", 0) + 1
        return es.enter_context(self.nc.sbuf_tensor(f"{name}_u{self.uid}", list(shape), dt))

    def ps(self, es, name, shape, dt=F32):
        self.uid = getattr(self, "uid", 0) + 1
        return es.enter_context(self.nc.psum_tensor(f"{name}_u{self.uid}", list(shape), dt))

    def build(self):
        nc = self.nc
        NL = self.nlayers
        bld = self
        SHAPES = {
            "xin": [T, D], "cc": [128, 8, 2], "ident": [128, 128], "blockones": [128, 128], "rotT": [128, 128],
            "ropeC": [128, NLAT], "ropeS": [128, NLAT], "swamask": [128, 2, 128], "namask": [128, 27, 128],
        }
        LSHAPES = {
            "ada_w": [D, 6 * D], "ada_b": [6 * D], "norm1_g": [D], "norm2_g": [D],
            "ev_w_in": [D, 3072], "ev_convw": [128, 4, 3], "ev_qg": [128, 1], "ev_kg": [128, 1], "ev_bias": [128, 27, 8, 128],
            "ev_w_out": [D, D], "ffn_w1": [D, DFF], "ffn_w3": [D, DFF], "ffn_w2": [DFF, D],
            "od_w_in": [D, 2304], "od_g": [128, 4], "od_sink": [8], "od_lam": [4, 64], "od_subln": [128], "od_w_out": [D, D],
            "moe_router": [D, NEXP], "moe_routerT": [NEXP, D], "moe_w1": [NEXP, D, DFE], "moe_w3": [NEXP, D, DFE], "moe_w2": [NEXP, DFE, D],
        }

        class LayerView:
            def __init__(self, name):
                self.name = name
                self.cache = {}

            def __getitem__(self, idx):
                if isinstance(idx, tuple):
                    l, rest = idx[0], idx[1:]
                else:
                    l, rest = idx, None
                if l not in self.cache:
                    self.cache[l] = bld.din(f"{self.name}_{l}", LSHAPES[self.name])
                ap = self.cache[l]
                return ap if rest is None else ap[rest]

        class LazyInputs:
            def __init__(self):
                self.cache = {}

            def __getitem__(self, name):
                if name not in self.cache:
                    if name in SHAPES:
                        self.cache[name] = bld.din(name, SHAPES[name])
                    else:
                        self.cache[name] = LayerView(name)
                return self.cache[name]

        I = LazyInputs()
        self.I = I
        self.out_rows = T if self.debug else NLAT
        out = nc.dram_tensor("out", [self.out_rows, D], F32, kind="ExternalOutput").ap()
        self.out = out
        S = {}
        S["hX"] = self.dscr("hX", [T, D], F32)
        S["hY"] = self.dscr("hY", [T, D], F32)
        S["accA"] = self.dscr("accA", [T, D], F32)
        S["accB"] = self.dscr("accB", [T, D], F32)
        S["modv"] = self.dscr("modv", [DEPTH, 2, 6 * D], F32)
        S["a2T"] = self.dscr("a2T", [8, 128, T], BF16)
        S["gates"] = self.dscr("gates", [T, NEXP], F32)
        S["e_gbT"] = self.dscr("e_gbT", [4, 128, T], BF16)
        S["e_qT"] = self.dscr("e_qT", [4, 128, T], BF16)
        S["e_kctx"] = self.dscr("e_kctx", [4, 128, NCTX], BF16)
        S["e_zctx"] = self.dscr("e_zctx", [4, 128, NCTX + 2], BF16)
        S["e_vctx"] = self.dscr("e_vctx", [128, 2 * 520], BF16)
        S["e_kext"] = self.dscr("e_kext", [4, 128, (NT + 4) * 128], BF16)
        S["e_vext"] = self.dscr("e_vext", [128, (NT + 4) * 520], BF16)
        S["e_zext"] = self.dscr("e_zext", [4, 128, NLAT + 2], BF16)
        S["o_qT"] = self.dscr("o_qT", [8, 128, T], BF16)
        S["o_blobK"] = self.dscr("o_blobK", [5 * 128, NLAT], BF16)
        S["o_kctx"] = self.dscr("o_kctx", [5, 128, NCTX], BF16)
        S["o_blobV"] = self.dscr("o_blobV", [5 * 128, NT * 130], BF16)
        S["o_vctx"] = self.dscr("o_vctx", [5, 128, 2 * 130], BF16)
        self.S = S

        with ExitStack() as es:
            self.sch = Sched(nc, es)
            sch = self.sch
            self.DB = {k: Buf(k) for k in list(S.keys()) + ["xin", "out", "dbg_h"]}
            self.gates_sb = self.sb(es, "gates_sb", [128, NTILE, NEXP], F32)
            self.gates_b = Buf("gates_sb")
            self.phase_mod()
            stop = os.environ.get("KSTOP", "")
            h_cur, h_cur_name = I["xin"], "xin"
            names = ["hX", "hY"]
            ni = 0
            for l in range(self.l0, NL):
                last = l == DEPTH - 1
                if stop == "mod":
                    break
                if l % 2 == 0:
                    self.phase_p1_even(l, h_cur, h_cur_name)
                    if stop == "p1":
                        break
                    h_mid, h_mid_name = S[names[ni]], names[ni]
                    ni ^= 1
                    self.phase_p2_even(l, h_cur, h_cur_name, h_mid, h_mid_name, last)
                else:
                    self.phase_p1_odd(l, h_cur, h_cur_name)
                    if stop == "p1":
                        break
                    h_mid, h_mid_name = S[names[ni]], names[ni]
                    ni ^= 1
                    self.phase_p2_odd(l, h_cur, h_cur_name, h_mid, h_mid_name, last)
                if stop == "p2":
                    cpb = sch.buf("cp_dbg")
                    sch.dma("pool", out[0:self.out_rows, :], h_mid[0:self.out_rows, :], cpb, reads=[self.DB[h_mid_name]], writes=[self.DB["out"]])
                    break
                final = (l == NL - 1)
                if final:
                    h_nxt, h_nxt_name = out, "out"
                else:
                    h_nxt, h_nxt_name = S[names[ni]], names[ni]
                    ni ^= 1
                self.phase_p3a(l, h_mid, h_mid_name, last)
                if stop == "p3a":
                    cpb = sch.buf("cp_dbg")
                    sch.dma("pool", out[0:self.out_rows, :], h_mid[0:self.out_rows, :], cpb, reads=[self.DB[h_mid_name]], writes=[self.DB["out"]])
                    break
                self.phase_p3b(l, h_mid, h_mid_name, h_nxt, h_nxt_name, last, final)
                h_cur, h_cur_name = h_nxt, h_nxt_name
            sch.barrier(release=False)
        return nc

    def load_consts(self, es, names):
        sch = self.sch
        res = {}
        for nm, shape, want_bf in names:
            t = self.sb(es, "c_" + nm, shape, F32)
            b = sch.buf("c_" + nm)
            sch.dma("sp", t[:], self.I[nm] if not isinstance(nm, tuple) else None, b, writes=[b])
            if want_bf:
                tb = self.sb(es, "cb_" + nm, shape, BF16)
                bb = sch.buf("cb_" + nm)
                sch.op("dve", lambda e, tb=tb, t=t: e.tensor_copy(out=tb[:], in_=t[:]), reads=[b], writes=[bb])
                res[nm] = (tb, bb)
            else:
                res[nm] = (t, b)
        return res

    def load_weight_bf16(self, es, name, src_ap, kchunks, ncols, stage, stage_bufs, cast_eng="pool", col_chunk=None):
        sch = self.sch
        wt = self.sb(es, name, [128, kchunks, ncols], BF16)
        wb = sch.buf(name)
        src = src_ap.rearrange("(k p) n -> p k n", p=128)
        cc = col_chunk or stage[0].shape[-1]
        i = 0
        for k in range(kchunks):
            for c0 in range(0, ncols, cc):
                c1 = min(ncols, c0 + cc)
                s = i % len(stage_bufs)
                i += 1
                st, stb = stage[s], stage_bufs[s]
                sch.dma("sp", st[:, 0:c1 - c0], src[:, k, c0:c1], stb, writes=[stb])
                eng = cast_eng if not isinstance(cast_eng, (list, tuple)) else cast_eng[i % len(cast_eng)]
                if eng == "act":
                    sch.op("act", lambda e, k=k, c0=c0, c1=c1, st=st: e.copy(out=wt[:, k, c0:c1], in_=st[:, 0:c1 - c0]),
                           reads=[stb], writes=[wb], partial=True)
                else:
                    sch.op(eng, lambda e, k=k, c0=c0, c1=c1, st=st: e.tensor_copy(out=wt[:, k, c0:c1], in_=st[:, 0:c1 - c0]),
                           reads=[stb], writes=[wb], partial=True)
        return wt, wb

    def make_stage(self, es, name, n, cols):
        tiles = [self.sb(es, f"{name}{i}", [128, cols], F32) for i in range(n)]
        return tiles, self.sch.bufs(name, n)

    def load_bcast(self, es, name, src_row_ap, n):
        t = self.sb(es, name, [128, n], F32)
        b = self.sch.buf(name)
        self.sch.dma("sp", t[:], src_row_ap.partition_broadcast(128), b, writes=[b])
        return t, b

    def make_eps(self, es, name):
        self.eps_t = self.sb(es, name, [128, 1], F32)
        self.eps_b = self.sch.buf(name)
        self.sch.op("pool", lambda e: e.memset(self.eps_t[:], EPS), writes=[self.eps_b])

    def rsqrt(self, out_ap, outb, in_ap, inb, scale):
        sch = self.sch
        eps_t, eps_b = self.eps_t, self.eps_b
        sch.op("act", lambda e: e.activation(out=out_ap, in_=in_ap, func=AF.Sqrt, bias=eps_t[:, 0:1], scale=scale),
               reads=[inb, eps_b], writes=[outb])
        sch.op("dve", lambda e: e.reciprocal(out=out_ap, in_=out_ap), reads=[outb], writes=[outb])

    def rms_mod_tile(self, htile, hb, ss, ssb, junk, junkb, rstd, rstdb, tmp, tmpb, Gb, SHb, out_ap, outb):
        sch = self.sch
        G, Gbuf = Gb
        SH, SHbuf = SHb
        sch.op("act", lambda e: e.activation(out=junk[:], in_=htile[:], func=AF.Square), reads=[hb], writes=[junkb])
        sch.op("dve", lambda e: e.tensor_reduce(out=ss[:, 0:1], in_=junk[:], axis=AX.X, op=ALU.add), reads=[junkb], writes=[ssb])
        self.rsqrt(rstd[:, 0:1], rstdb, ss[:, 0:1], ssb, 1.0 / D)
        sch.op("dve", lambda e: e.scalar_tensor_tensor(out=tmp[:], in0=htile[:], scalar=rstd[:, 0:1], in1=G[:],
                                                       op0=ALU.mult, op1=ALU.mult),
               reads=[hb, rstdb, Gbuf], writes=[tmpb])
        sch.op("dve", lambda e: e.tensor_tensor(out=out_ap, in0=tmp[:], in1=SH[:], op=ALU.add),
               reads=[tmpb, SHbuf], writes=[outb])

    def phase_mod(self):
        nc, sch, I, S = self.nc, self.sch, self.I, self.S
        with ExitStack() as es:
            cc = self.sb(es, "m_cc", [128, 8, 2], F32)
            ccb = sch.buf("m_cc")
            sg = self.sb(es, "m_sg", [128, 8, 2], F32)
            sgb = sch.buf("m_sg")
            sil = self.sb(es, "m_sil", [128, 8, 2], F32)
            silb = sch.buf("m_sil")
            sch.dma("sp", cc[:], I["cc"], ccb, writes=[ccb])
            sch.op("act", lambda e: e.activation(out=sg[:], in_=cc[:], func=AF.Sigmoid), reads=[ccb], writes=[sgb])
            sch.op("dve", lambda e: e.tensor_tensor(out=sil[:], in0=cc[:], in1=sg[:], op=ALU.mult),
                   reads=[ccb, sgb], writes=[silb])
            wst = [self.sb(es, f"m_w{i}", [128, 8, 512], F32) for i in range(2)]
            wstb = sch.bufs("m_w", 2)
            row = self.sb(es, "m_row", [2, 6 * D], F32)
            rowb = sch.buf("m_row")
            bias = self.sb(es, "m_bias", [2, 6 * D], F32)
            biasb = sch.buf("m_bias")
            ng = self.sb(es, "m_ng", [2, 2 * D], F32)
            ngb = sch.buf("m_ng")
            pm = [self.ps(es, f"m_ps{i}", [2, 512]) for i in range(2)]
            pmb = sch.bufs("m_ps", 2)
            it = 0
            for l in range(self.l0, self.nlayers):
                sch.dma("sp", bias[:], I["ada_b"][l].partition_broadcast(2), biasb, writes=[biasb])
                sch.dma("sp", ng[:, 0:D], I["norm1_g"][l].partition_broadcast(2), ngb, writes=[ngb])
                sch.dma("sp", ng[:, D:2 * D], I["norm2_g"][l].partition_broadcast(2), ngb, writes=[ngb], partial=True)
                wsrc = I["ada_w"][l].rearrange("(k p) n -> p k n", p=128)
                for c in range(12):
                    s = it % 2
                    it += 1
                    sch.dma("sp", wst[s][:], wsrc[:, :, c * 512:(c + 1) * 512], wstb[s], writes=[wstb[s]])
                    for k in range(8):
                        sch.op("pe", lambda e, s=s, k=k: e.matmul(pm[s][:], lhsT=sil[:, k, :], rhs=wst[s][:, k, :],
                                                                 start=(k == 0), stop=(k == 7)),
                               reads=[silb, wstb[s]], writes=[pmb[s]], signal=(k == 7))
                    sch.op("dve", lambda e, s=s, c=c: e.tensor_tensor(out=row[:, c * 512:(c + 1) * 512], in0=pm[s][:],
                                                                      in1=bias[:, c * 512:(c + 1) * 512], op=ALU.add),
                           reads=[pmb[s], biasb], writes=[rowb], partial=True)
                sch.op("dve", lambda e: e.scalar_tensor_tensor(out=row[:, D:2 * D], in0=row[:, D:2 * D], scalar=1.0,
                                                               in1=ng[:, 0:D], op0=ALU.add, op1=ALU.mult),
                       reads=[rowb, ngb], writes=[rowb])
                sch.op("dve", lambda e: e.scalar_tensor_tensor(out=row[:, 4 * D:5 * D], in0=row[:, 4 * D:5 * D], scalar=1.0,
                                                               in1=ng[:, D:2 * D], op0=ALU.add, op1=ALU.mult),
                       reads=[rowb, ngb], writes=[rowb])
                sch.dma("pool", S["modv"][l], row[:], rowb, reads=[rowb], writes=[self.DB["modv"]], partial=True)
            sch.barrier()

    def load_mod(self, es, l, idxs):
        res = {}
        for w in (0, 1):
            for ix in idxs:
                t = self.sb(es, f"mod{w}_{ix}", [128, D], F32)
                b = self.sch.buf(f"mod{w}_{ix}")
                self.sch.dma("sp", t[:], self.S["modv"][l, w, ix * D:(ix + 1) * D].partition_broadcast(128), b,
                             reads=[self.DB["modv"]], writes=[b])
                res[(w, ix)] = (t, b)
        return res

    def phase_p1(self, l, h_cur, h_name, even):
        nc, sch, I, S, DB = self.nc, self.sch, self.I, self.S, self.DB
        li = l // 2
        with ExitStack() as es:
            ncols = 3072 if even else 2304
            wsrc = (I["ev_w_in"] if even else I["od_w_in"])[li]
            stage, stageb = self.make_stage(es, "p1_stg", 2, 2048)
            W, Wb = self.load_weight_bf16(es, "p1_W", wsrc, 8, ncols, stage, stageb, cast_eng=["pool", "dve"], col_chunk=1536 if even else 1152)
            mod = self.load_mod(es, l, [0, 1])
            self.make_eps(es, "p1_eps")
            identf, identfb = self.sb(es, "p1_idf", [128, 128], F32), sch.buf("p1_idf")
            ident, identb = self.sb(es, "p1_id", [128, 128], BF16), sch.buf("p1_id")
            sch.dma("sp", identf[:], I["ident"], identfb, writes=[identfb])
            sch.op("dve", lambda e: e.tensor_copy(out=ident[:], in_=identf[:]), reads=[identfb], writes=[identb])
            bof, bofb = self.sb(es, "p1_bof", [128, 128], F32), sch.buf("p1_bof")
            bo, bob = self.sb(es, "p1_bo", [128, 128], BF16), sch.buf("p1_bo")
            sch.dma("sp", bof[:], I["blockones"], bofb, writes=[bofb])
            sch.op("dve", lambda e: e.tensor_copy(out=bo[:], in_=bof[:]), reads=[bofb], writes=[bob])
            if even:
                gq, gqb = self.sb(es, "p1_gq", [128, 1], F32), sch.buf("p1_gq")
                gk, gkb = self.sb(es, "p1_gk", [128, 1], F32), sch.buf("p1_gk")
                sch.dma("sp", gq[:], I["ev_qg"][li], gqb, writes=[gqb])
                sch.dma("sp", gk[:], I["ev_kg"][li], gkb, writes=[gkb])
                sch.op("dve", lambda e: e.tensor_scalar(out=gq[:], in0=gq[:], scalar1=0.125, scalar2=None, op0=ALU.mult),
                       reads=[gqb], writes=[gqb])
                nfm = 20
            else:
                g4, g4b = self.sb(es, "p1_g4", [128, 4], F32), sch.buf("p1_g4")
                sch.dma("sp", g4[:], I["od_g"][li], g4b, writes=[g4b])
                sch.op("dve", lambda e: e.tensor_scalar(out=g4[:, 0:1], in0=g4[:, 0:1], scalar1=0.125, scalar2=None, op0=ALU.mult),
                       reads=[g4b], writes=[g4b])
                sch.op("dve", lambda e: e.tensor_scalar(out=g4[:, 2:3], in0=g4[:, 2:3], scalar1=0.125, scalar2=None, op0=ALU.mult),
                       reads=[g4b], writes=[g4b])
                rotf, rotfb = self.sb(es, "p1_rotf", [128, 128], F32), sch.buf("p1_rotf")
                rot, rotb = self.sb(es, "p1_rot", [128, 128], BF16), sch.buf("p1_rot")
                sch.dma("sp", rotf[:], I["rotT"], rotfb, writes=[rotfb])
                sch.op("dve", lambda e: e.tensor_copy(out=rot[:], in_=rotf[:]), reads=[rotfb], writes=[rotb])
                ropeCt = [self.sb(es, f"p1_rc{i}", [128, 512], F32) for i in range(2)]
                ropeCtb = sch.bufs("p1_rc", 2)
                ropeSt = [self.sb(es, f"p1_rs{i}", [128, 512], F32) for i in range(2)]
                ropeStb = sch.bufs("p1_rs", 2)
            ht = [self.sb(es, f"p1_h{i}", [128, D], F32) for i in range(2)]
            htb = sch.bufs("p1_h", 2)
            junk, junkb = self.sb(es, "p1_junk", [128, D], F32), sch.buf("p1_junk")
            tmp, tmpb = self.sb(es, "p1_tmp", [128, D], F32), sch.buf("p1_tmp")
            ss, ssb = self.sb(es, "p1_ss", [128, 1], F32), sch.buf("p1_ss")
            rstd, rstdb = self.sb(es, "p1_rstd", [128, 1], F32), sch.buf("p1_rstd")
            abf = [self.sb(es, f"p1_a{i}", [128, D], BF16) for i in range(2)]
            abfb = sch.bufs("p1_a", 2)
            aT = [self.sb(es, f"p1_aT{i}", [128, 8, 512], BF16) for i in range(2)]
            aTb = sch.bufs("p1_aT", 2)
            pT = [self.ps(es, f"p1_pT{i}", [128, 8, 128], BF16) for i in range(2)]
            pTb = sch.bufs("p1_pT", 2)
            pf = [self.ps(es, f"p1_pf{i}", [128, 512]) for i in range(3)]
            pfb = sch.bufs("p1_pf", 3)
            pms = [self.ps(es, f"p1_pms{i}", [128, 512]) for i in range(2)]
            pmsb = sch.bufs("p1_pms", 2)
            pv = self.ps(es, "p1_pv", [128, 512])
            pvb = sch.buf("p1_pv")
            sq = [self.sb(es, f"p1_sq{i}", [128, 512], BF16) for i in range(2)]
            sqb = sch.bufs("p1_sq", 2)
            rs = [self.sb(es, f"p1_rs{i}", [128, 512], F32) for i in range(2)]
            rsb = sch.bufs("p1_rsb", 2)
            usb = [self.sb(es, f"p1_u{i}", [128, 512], F32) for i in range(2)]
            usbb = sch.bufs("p1_u", 2)
            if even:
                stg = [self.sb(es, f"p1_st{i}", [128, 16, 512], BF16) for i in range(2)]
                vst = [self.sb(es, f"p1_vst{i}", [128, 8, 65], BF16) for i in range(2)]
            else:
                stg = [self.sb(es, f"p1_st{i}", [128, 13, 512], BF16) for i in range(2)]
                vst = [self.sb(es, f"p1_vst{i}", [128, 650], BF16) for i in range(2)]
                xn = [self.sb(es, f"p1_xn{i}", [128, 512], F32) for i in range(2)]
                xnb = sch.bufs("p1_xn", 2)
                xnh = [self.sb(es, f"p1_xnh{i}", [128, 512], BF16) for i in range(2)]
                xnhb = sch.bufs("p1_xnh", 2)
                t1 = [self.sb(es, f"p1_t1{i}", [128, 512], F32) for i in range(2)]
                t1b = sch.bufs("p1_t1", 2)
            stgb = sch.bufs("p1_st", 2)
            vstb = sch.bufs("p1_vst", 2)
            for i in range(2):
                sch.op("pool", lambda e, i=i: e.memset(vst[i][:], 1.0), writes=[vstb[i]])
            if even:
                zt, ztb = self.sb(es, "p1_zero", [128, 1040], BF16), sch.buf("p1_zero")
                sch.op("pool", lambda e: e.memset(zt[:], 0.0), writes=[ztb])
                kxz = S["e_kext"].rearrange("c p t -> p c t")
                zxz = S["e_zext"].rearrange("c p t -> p c t")
                z4 = zt[:, 0:1024].rearrange("p (c t) -> p c t", c=4)
                sch.dma("pool", kxz[:, :, 0:256], z4, ztb, reads=[ztb], writes=[DB["e_kext"]], partial=True)
                sch.dma("pool", kxz[:, :, 256 + NLAT:512 + NLAT], z4, ztb, reads=[ztb], writes=[DB["e_kext"]], partial=True)
                sch.dma("pool", S["e_vext"][:, 0:1040], zt[:, 0:1040], ztb, reads=[ztb], writes=[DB["e_vext"]], partial=True)
                sch.dma("pool", S["e_vext"][:, (NT + 2) * 520:(NT + 4) * 520], zt[:, 0:1040], ztb, reads=[ztb], writes=[DB["e_vext"]], partial=True)
                sch.dma("pool", zxz[:, :, 0:1], zt[:, 0:4].rearrange("p (c o) -> p c o", o=1), ztb, reads=[ztb], writes=[DB["e_zext"]], partial=True, allow_slow_non_contiguous=True)
                sch.dma("pool", zxz[:, :, NLAT + 1:NLAT + 2], zt[:, 4:8].rearrange("p (c o) -> p c o", o=1), ztb, reads=[ztb], writes=[DB["e_zext"]], partial=True, allow_slow_non_contiguous=True)
                zc = S["e_zctx"].rearrange("c p t -> p c t")
                sch.dma("pool", zc[:, :, 0:1], zt[:, 0:4].rearrange("p (c o) -> p c o", o=1), ztb, reads=[ztb], writes=[DB["e_zctx"]], partial=True, allow_slow_non_contiguous=True)
                sch.dma("pool", zc[:, :, NCTX + 1:NCTX + 2], zt[:, 4:8].rearrange("p (c o) -> p c o", o=1), ztb, reads=[ztb], writes=[DB["e_zctx"]], partial=True, allow_slow_non_contiguous=True)

            cnt = {"ms": 0, "pf": 0, "u": 0, "xn": 0}

            def norm_chunk(psrc, psrcb, nt, gvec, gvecb, dst, dstb, rope_tok0=None):
                k = cnt["ms"] % 2
                cnt["ms"] += 1
                sch.op("act", lambda e: e.activation(out=sq[k][:, 0:nt], in_=psrc[:, 0:nt], func=AF.Square),
                       reads=[psrcb], writes=[sqb[k]])
                sch.op("pe", lambda e: e.matmul(pms[k][:, 0:nt], lhsT=bo[:], rhs=sq[k][:, 0:nt], start=True, stop=True),
                       reads=[bob, sqb[k]], writes=[pmsb[k]])
                self.rsqrt(rs[k][:, 0:nt], rsb[k], pms[k][:, 0:nt], pmsb[k], 1.0 / 64)
                if rope_tok0 is None:
                    sch.op("dve", lambda e: e.scalar_tensor_tensor(out=dst, in0=psrc[:, 0:nt], scalar=gvec, in1=rs[k][:, 0:nt],
                                                                   op0=ALU.mult, op1=ALU.mult),
                           reads=[psrcb, gvecb, rsb[k]], writes=[dstb], partial=True)
                else:
                    x = cnt["xn"] % 2
                    cnt["xn"] += 1
                    ropeC, ropeCb, ropeS, ropeSb = cnt["rope"]
                    sch.op("dve", lambda e: e.scalar_tensor_tensor(out=xn[x][:, 0:nt], in0=psrc[:, 0:nt], scalar=gvec, in1=rs[k][:, 0:nt],
                                                                   op0=ALU.mult, op1=ALU.mult),
                           reads=[psrcb, gvecb, rsb[k]], writes=[xnb[x]])
                    sch.op("act", lambda e: e.copy(out=xnh[x][:, 0:nt], in_=xn[x][:, 0:nt]), reads=[xnb[x]], writes=[xnhb[x]])
                    k2 = cnt["ms"] % 2
                    cnt["ms"] += 1
                    sch.op("pe", lambda e: e.matmul(pms[k2][:, 0:nt], lhsT=rot[:], rhs=xnh[x][:, 0:nt], start=True, stop=True),
                           reads=[rotb, xnhb[x]], writes=[pmsb[k2]])
                    sch.op("dve", lambda e: e.tensor_tensor(out=t1[x][:, 0:nt], in0=xn[x][:, 0:nt], in1=ropeC[:, 0:nt], op=ALU.mult),
                           reads=[xnb[x], ropeCb], writes=[t1b[x]])
                    sch.op("dve", lambda e: e.tensor_tensor(out=xn[x][:, 0:nt], in0=pms[k2][:, 0:nt], in1=ropeS[:, 0:nt], op=ALU.mult),
                           reads=[pmsb[k2], ropeSb], writes=[xnb[x]])
                    sch.op("dve", lambda e: e.tensor_tensor(out=dst, in0=t1[x][:, 0:nt], in1=xn[x][:, 0:nt], op=ALU.add),
                           reads=[t1b[x], xnb[x]], writes=[dstb], partial=True)

            def proj_fm(j, a, ab, nt):
                k = cnt["pf"] % 3
                cnt["pf"] += 1
                for kc in range(8):
                    sch.op("pe", lambda e, kc=kc: e.matmul(pf[k][:, 0:nt], lhsT=W[:, kc, j * 128:(j + 1) * 128], rhs=a[:, kc, 0:nt],
                                                           start=(kc == 0), stop=(kc == 7)),
                           reads=[Wb, ab], writes=[pfb[k]], signal=(kc == 7))
                return pf[k], pfb[k]

            nst = NST + 1
            tile_i = 0
            for st in range(nst):
                isctx = st == NST
                nt = 256 if isctx else 512
                nsub = nt // 128
                tok0 = st * 512
                w = 1 if isctx else 0
                sa = st % 2
                a, ab = aT[sa], aTb[sa]
                for sub in range(nsub):
                    s2 = tile_i % 2
                    tile_i += 1
                    r0 = tok0 + sub * 128
                    sch.dma("sp", ht[s2][:], h_cur[r0:r0 + 128, :], htb[s2], reads=[DB[h_name]], writes=[htb[s2]])
                    self.rms_mod_tile(ht[s2], htb[s2], ss, ssb, junk, junkb, rstd, rstdb, tmp, tmpb,
                                      mod[(w, 1)], mod[(w, 0)], abf[s2][:], abfb[s2])
                    for kc in range(8):
                        sch.op("pe", lambda e, kc=kc: e.transpose(out=pT[s2][:, kc, :], in_=abf[s2][:, kc * 128:(kc + 1) * 128], identity=ident[:]),
                               reads=[abfb[s2], identb], writes=[pTb[s2]], signal=(kc == 7), partial=True)
                    sch.op("act", lambda e: e.copy(out=a[:, :, sub * 128:(sub + 1) * 128], in_=pT[s2][:]),
                           reads=[pTb[s2]], writes=[ab], partial=True)
                sg = st % 2
                sgt, sgb = stg[sg], stgb[sg]
                rope0 = None if (even or isctx) else tok0
                if rope0 is not None:
                    rr = st % 2
                    sch.dma("sp", ropeCt[rr][:], I["ropeC"][:, tok0:tok0 + 512], ropeCtb[rr], writes=[ropeCtb[rr]])
                    sch.dma("sp", ropeSt[rr][:], I["ropeS"][:, tok0:tok0 + 512], ropeStb[rr], writes=[ropeStb[rr]])
                    cnt["rope"] = (ropeCt[rr], ropeCtb[rr], ropeSt[rr], ropeStb[rr])
                if even:
                    for c in range(4):
                        pu, pub = proj_fm(8 + c, a, ab, nt)
                        ku = cnt["u"] % 2
                        cnt["u"] += 1
                        sch.op("act", lambda e: e.copy(out=usb[ku][:, 0:nt], in_=pu[:, 0:nt]), reads=[pub], writes=[usbb[ku]])
                        pg, pgb = proj_fm(4 + c, a, ab, nt)
                        sch.op("dve", lambda e: e.tensor_tensor(out=sgt[:, 4 + c, 0:nt], in0=pg[:, 0:nt], in1=usb[ku][:, 0:nt], op=ALU.mult),
                               reads=[pgb, usbb[ku]], writes=[sgb], partial=True)
                        pb, pbb = proj_fm(c, a, ab, nt)
                        sch.op("act", lambda e: e.copy(out=sgt[:, c, 0:nt], in_=pb[:, 0:nt]), reads=[pbb], writes=[sgb], partial=True)
                    for c in range(4):
                        pq, pqb = proj_fm(12 + c, a, ab, nt)
                        norm_chunk(pq, pqb, nt, gq[:, 0:1], gqb, sgt[:, 8 + c, 0:nt], sgb)
                        pk, pkb = proj_fm(16 + c, a, ab, nt)
                        norm_chunk(pk, pkb, nt, gk[:, 0:1], gkb, sgt[:, 12 + c, 0:nt], sgb)
                else:
                    for c in range(8):
                        pq, pqb = proj_fm(c, a, ab, nt)
                        gi = 0 if c < 4 else 2
                        norm_chunk(pq, pqb, nt, g4[:, gi:gi + 1], g4b, sgt[:, c, 0:nt], sgb, rope0)
                    pk, pkb = proj_fm(8, a, ab, nt)
                    norm_chunk(pk, pkb, nt, g4[:, 1:2], g4b, sgt[:, 8, 0:nt], sgb, rope0)
                    for c in range(4):
                        pk, pkb = proj_fm(10 + c, a, ab, nt)
                        norm_chunk(pk, pkb, nt, g4[:, 3:4], g4b, sgt[:, 9 + c, 0:nt], sgb, rope0)
                for sub in range(nsub):
                    tl = st * 4 + sub
                    sv = tl % 2
                    if even:
                        for kc in range(8):
                            sch.op("pe", lambda e, kc=kc: e.matmul(pv[:, :], lhsT=a[:, kc, sub * 128:(sub + 1) * 128], rhs=W[:, kc, 2560:3072],
                                                                   start=(kc == 0), stop=(kc == 7)),
                                   reads=[Wb, ab], writes=[pvb], signal=(kc == 7))
                        sch.op("act", lambda e: e.copy(out=vst[sv][:, :, 0:64], in_=pv[:, :].rearrange("p (h d) -> p h d", d=64)),
                               reads=[pvb], writes=[vstb[sv]])
                        src = vst[sv][:].rearrange("p h d -> p (h d)")
                        if isctx:
                            sch.dma("pool", S["e_vctx"][:, sub * 520:(sub + 1) * 520], src, vstb[sv], reads=[vstb[sv]], writes=[DB["e_vctx"]], partial=True)
                        else:
                            sch.dma("pool", S["e_vext"][:, (tl + 2) * 520:(tl + 3) * 520], src, vstb[sv], reads=[vstb[sv]], writes=[DB["e_vext"]], partial=True)
                    else:
                        for kc in range(8):
                            sch.op("pe", lambda e, kc=kc: e.matmul(pv[:, 0:128], lhsT=a[:, kc, sub * 128:(sub + 1) * 128], rhs=W[:, kc, 1152:1280],
                                                                   start=(kc == 0), stop=(kc == 7)),
                                   reads=[Wb, ab], writes=[pvb], signal=(kc == 7))
                        sch.op("act", lambda e: e.copy(out=vst[sv][:, 520:650].rearrange("p (h d) -> p h d", d=65)[:, :, 0:64],
                                                       in_=pv[:, 0:128].rearrange("p (h d) -> p h d", d=64)),
                               reads=[pvb], writes=[vstb[sv]])
                        for kc in range(8):
                            sch.op("pe", lambda e, kc=kc: e.matmul(pv[:, :], lhsT=a[:, kc, sub * 128:(sub + 1) * 128], rhs=W[:, kc, 1792:2304],
                                                                   start=(kc == 0), stop=(kc == 7)),
                                   reads=[Wb, ab], writes=[pvb], signal=(kc == 7))
                        sch.op("act", lambda e: e.copy(out=vst[sv][:, 0:520].rearrange("p (h d) -> p h d", d=130)[:, :, 0:128],
                                                       in_=pv[:, :].rearrange("p (h d) -> p h d", d=128)),
                               reads=[pvb], writes=[vstb[sv]], partial=True)
                        for sec in range(5):
                            src = vst[sv][:, sec * 130:(sec + 1) * 130]
                            if isctx:
                                sch.dma("pool", S["o_vctx"][sec, :, sub * 130:(sub + 1) * 130], src, vstb[sv], reads=[vstb[sv]], writes=[DB["o_vctx"]], partial=True)
                            else:
                                sch.dma("pool", S["o_blobV"][sec * 128:(sec + 1) * 128, tl * 130:(tl + 1) * 130], src, vstb[sv],
                                        reads=[vstb[sv]], writes=[DB["o_blobV"]], partial=True)
                if even:
                    sch.dma("pool", S["e_gbT"].rearrange("c p t -> p c t")[:, :, tok0:tok0 + nt], sgt[:, 0:4, 0:nt], sgb, reads=[sgb], writes=[DB["e_gbT"]], partial=True)
                    sch.dma("pool", S["e_qT"].rearrange("c p t -> p c t")[:, :, tok0:tok0 + nt], sgt[:, 8:12, 0:nt], sgb, reads=[sgb], writes=[DB["e_qT"]], partial=True)
                    if isctx:
                        sch.dma("pool", S["e_zctx"].rearrange("c p t -> p c t")[:, :, 1:NCTX + 1], sgt[:, 4:8, 0:nt], sgb, reads=[sgb], writes=[DB["e_zctx"]], partial=True)
                        sch.dma("pool", S["e_kctx"].rearrange("c p t -> p c t"), sgt[:, 12:16, 0:nt], sgb, reads=[sgb], writes=[DB["e_kctx"]], partial=True)
                    else:
                        sch.dma("pool", S["e_kext"].rearrange("c p t -> p c t")[:, :, 256 + tok0:256 + tok0 + nt], sgt[:, 12:16, 0:nt], sgb, reads=[sgb], writes=[DB["e_kext"]], partial=True)
                        sch.dma("pool", S["e_zext"].rearrange("c p t -> p c t")[:, :, 1 + tok0:1 + tok0 + nt], sgt[:, 4:8, 0:nt], sgb, reads=[sgb], writes=[DB["e_zext"]], partial=True)
                else:
                    sch.dma("pool", S["o_qT"].rearrange("c p t -> p c t")[:, :, tok0:tok0 + nt], sgt[:, 0:8, 0:nt], sgb, reads=[sgb], writes=[DB["o_qT"]], partial=True)
                    if isctx:
                        sch.dma("pool", S["o_kctx"].rearrange("c p t -> p c t"), sgt[:, 8:13, 0:nt], sgb, reads=[sgb], writes=[DB["o_kctx"]], partial=True)
                    else:
                        bk = S["o_blobK"].rearrange("(c p) t -> p c t", p=128)
                        sch.dma("pool", bk[:, :, tok0:tok0 + nt], sgt[:, 8:13, 0:nt], sgb, reads=[sgb], writes=[DB["o_blobK"]], partial=True)
            sch.barrier()

    def phase_p1_even(self, l, h_cur, h_name):
        self.phase_p1(l, h_cur, h_name, True)

    def phase_p1_odd(self, l, h_cur, h_name):
        self.phase_p1(l, h_cur, h_name, False)

    def phase_p2_even(self, l, h_cur, h_name, h_out, h_out_name, last):
        nc, sch, I, S, DB = self.nc, self.sch, self.I, self.S, self.DB
        li = l // 2
        with ExitStack() as es:
            stage, stageb = self.make_stage(es, "p2_stg", 2, 1024)
            Wo, Wob = self.load_weight_bf16(es, "p2_Wo", I["ev_w_out"][li], 8, D, stage, stageb, cast_eng=["pool", "dve"])
            mod = self.load_mod(es, l, [2])
            identf, identfb = self.sb(es, "p2_idf", [128, 128], F32), sch.buf("p2_idf")
            ident, identb = self.sb(es, "p2_id", [128, 128], BF16), sch.buf("p2_id")
            sch.dma("sp", identf[:], I["ident"], identfb, writes=[identfb])
            sch.op("dve", lambda e: e.tensor_copy(out=ident[:], in_=identf[:]), reads=[identfb], writes=[identb])
            cw, cwb = self.sb(es, "p2_cw", [128, 4, 3], F32), sch.buf("p2_cw")
            sch.dma("sp", cw[:], I["ev_convw"][li], cwb, writes=[cwb])
            EB, EBb = self.sb(es, "p2_EB", [128, 27, 8, 128], BF16), sch.buf("p2_EB")
            mk, mkb = self.sb(es, "p2_mk", [128, 27, 128], F32), sch.buf("p2_mk")
            sch.dma("sp", mk[:], I["namask"], mkb, writes=[mkb])
            ebt, ebtb = self.sb(es, "p2_ebt", [128, 1024], F32), sch.buf("p2_ebt")
            for t in range(27):
                s = t % 2
                sch.dma("sp", stage[s][:], I["ev_bias"][li, :, t].rearrange("p h q -> p (h q)"), stageb[s], writes=[stageb[s]])
                sch.op("act", lambda e, s=s: e.activation(out=ebt[:], in_=stage[s][:], func=AF.Exp), reads=[stageb[s]], writes=[ebtb])
                sch.op("dve", lambda e, t=t: e.tensor_tensor(out=EB[:, t, :, :], in0=ebt[:].rearrange("p (h q) -> p h q", h=8),
                                                             in1=mk[:, t:t + 1, :].to_broadcast([128, 8, 128]), op=ALU.mult),
                       reads=[ebtb, mkb], writes=[EBb], partial=True)
            kctx, kctxb = self.sb(es, "p2_kctx", [128, 4, NCTX], BF16), sch.buf("p2_kctx")
            vctx, vctxb = self.sb(es, "p2_vctx", [128, 2 * 520], BF16), sch.buf("p2_vctx")
            sch.dma("sp", kctx[:], S["e_kctx"].rearrange("c p t -> p c t"), kctxb, reads=[DB["e_kctx"]], writes=[kctxb])
            sch.dma("sp", vctx[:], S["e_vctx"], vctxb, reads=[DB["e_vctx"]], writes=[vctxb])
            qt = [self.sb(es, f"p2_q{i}", [128, 4, 128], BF16) for i in range(2)]
            qtb = sch.bufs("p2_q", 2)
            kw = [self.sb(es, f"p2_kw{i}", [128, 4, 768], BF16) for i in range(2)]
            kwb = sch.bufs("p2_kw", 2)
            vw = [self.sb(es, f"p2_vw{i}", [128, 6 * 520], BF16) for i in range(2)]
            vwb = sch.bufs("p2_vw", 2)
            zw = [self.sb(es, f"p2_zw{i}", [128, 4, 130], BF16) for i in range(2)]
            zwb = sch.bufs("p2_zw", 2)
            gbw = [self.sb(es, f"p2_gb{i}", [128, 4, 128], BF16) for i in range(2)]
            gbwb = sch.bufs("p2_gb", 2)
            ht = [self.sb(es, f"p2_h{i}", [128, D], F32) for i in range(2)]
            htb = sch.bufs("p2_h", 2)
            ho = [self.sb(es, f"p2_ho{i}", [128, D], F32) for i in range(2)]
            hob = sch.bufs("p2_ho", 2)
            cacc, caccb = self.sb(es, "p2_cacc", [128, 128], F32), sch.buf("p2_cacc")
            mixT = [self.sb(es, f"p2_mixT{i}", [128, 8, 128], BF16) for i in range(2)]
            mixTb = sch.bufs("p2_mixT", 2)
            E = [self.sb(es, f"p2_E{i}", [128, 8, 128], BF16) for i in range(2)]
            Eb = sch.bufs("p2_E", 2)
            Em = [self.sb(es, f"p2_Em{i}", [128, 6, 128], BF16) for i in range(2)]
            Emb = sch.bufs("p2_Em", 2)
            yna, ynab = self.sb(es, "p2_yna", [128, 8, 64], BF16), sch.buf("p2_yna")
            rden, rdenb = self.sb(es, "p2_rden", [128, 4, 1], F32), sch.buf("p2_rden")
            tmp, tmpb = self.sb(es, "p2_tmp", [128, D], F32), sch.buf("p2_tmp")
            pS = [self.ps(es, f"p2_pS{i}", [128, 8, 128]) for i in range(2)]
            pSb = sch.bufs("p2_pS", 2)
            po = [self.ps(es, f"p2_po{i}", [128, 4, 65]) for i in range(2)]
            pob = sch.bufs("p2_po", 2)
            pT = self.ps(es, "p2_pT", [128, 4, 128], BF16)
            pTb = sch.buf("p2_pT")
            py = pS[0][:].rearrange("p (a x) b -> p a (x b)", a=2)
            pyb = pSb[0]

            ntiles = NT if last else NT + 2
            hcnt = 0
            for i in range(ntiles):
                isctx = i >= NT
                s = i % 2
                tok0 = i * 128
                w = 1 if isctx else 0
                if isctx:
                    n = 0
                    tcls = None
                else:
                    if i == 0:
                        e0, n, tcls = 0, 6, 5
                    elif i == 1:
                        e0, n, tcls = 1, 5, 11
                    elif i == NT - 2:
                        e0, n, tcls = NT - 2, 5, 16
                    elif i == NT - 1:
                        e0, n, tcls = NT - 2, 6, 21
                    else:
                        e0, n, tcls = i, 5, 0
                sch.dma("sp", qt[s][:], S["e_qT"].rearrange("c p t -> p c t")[:, :, tok0:tok0 + 128], qtb[s], reads=[DB["e_qT"]], writes=[qtb[s]])
                sch.dma("sp", gbw[s][:], S["e_gbT"].rearrange("c p t -> p c t")[:, :, tok0:tok0 + 128], gbwb[s], reads=[DB["e_gbT"]], writes=[gbwb[s]])
                if isctx:
                    c0 = (i - NT) * 128
                    sch.dma("sp", zw[s][:], S["e_zctx"].rearrange("c p t -> p c t")[:, :, c0:c0 + 130], zwb[s], reads=[DB["e_zctx"]], writes=[zwb[s]])
                else:
                    sch.dma("sp", zw[s][:], S["e_zext"].rearrange("c p t -> p c t")[:, :, tok0:tok0 + 130], zwb[s], reads=[DB["e_zext"]], writes=[zwb[s]])
                    sch.dma("sp", kw[s][:, :, 0:n * 128], S["e_kext"].rearrange("c p t -> p c t")[:, :, e0 * 128:(e0 + n) * 128], kwb[s],
                            reads=[DB["e_kext"]], writes=[kwb[s]])
                    sch.dma("sp", vw[s][:, 0:n * 520], S["e_vext"][:, e0 * 520:(e0 + n) * 520], vwb[s], reads=[DB["e_vext"]], writes=[vwb[s]])
                sch.dma("sp", ht[s][:], h_cur[tok0:tok0 + 128, :], htb[s], reads=[DB[h_name]], writes=[htb[s]])
                mt, mtb = mixT[s], mixTb[s]
                for c in range(4):
                    sch.op("dve", lambda e, c=c: e.tensor_scalar(out=cacc[:], in0=zw[s][:, c, 1:129], scalar1=cw[:, c, 1:2], scalar2=None, op0=ALU.mult),
                           reads=[zwb[s], cwb], writes=[caccb])
                    sch.op("dve", lambda e, c=c: e.scalar_tensor_tensor(out=cacc[:], in0=zw[s][:, c, 0:128], scalar=cw[:, c, 0:1], in1=cacc[:],
                                                                        op0=ALU.mult, op1=ALU.add), reads=[zwb[s], cwb, caccb], writes=[caccb])
                    sch.op("dve", lambda e, c=c: e.scalar_tensor_tensor(out=cacc[:], in0=zw[s][:, c, 2:130], scalar=cw[:, c, 2:3], in1=cacc[:],
                                                                        op0=ALU.mult, op1=ALU.add), reads=[zwb[s], cwb, caccb], writes=[caccb])
                    sch.op("dve", lambda e, c=c: e.tensor_tensor(out=mt[:, c, :], in0=cacc[:], in1=gbw[s][:, c, :], op=ALU.mult),
                           reads=[caccb, gbwb[s]], writes=[mtb], partial=True)
                for h in range(8):
                    cr, hh = h // 2, h % 2
                    p0, p1 = hh * 64, hh * 64 + 64
                    hs = hcnt % 2
                    hcnt += 1
                    nchunks = n + 2
                    for ci in range(n):
                        sch.op("pe", lambda e, ci=ci: e.matmul(pS[hs][:, ci, :], lhsT=kw[s][p0:p1, cr, ci * 128:(ci + 1) * 128], rhs=qt[s][p0:p1, cr, :],
                                                               start=True, stop=True),
                               reads=[kwb[s], qtb[s]], writes=[pSb[hs]], signal=False, partial=True)
                    for j in range(2):
                        sch.op("pe", lambda e, j=j: e.matmul(pS[hs][:, n + j, :], lhsT=kctx[p0:p1, cr, j * 128:(j + 1) * 128], rhs=qt[s][p0:p1, cr, :],
                                                             start=True, stop=True),
                               reads=[kctxb, qtb[s]], writes=[pSb[hs]], signal=(j == 1), partial=True)
                    sch.op("act", lambda e: e.activation(out=E[hs][:, 0:nchunks, :], in_=pS[hs][:, 0:nchunks, :], func=AF.Exp),
                           reads=[pSb[hs]], writes=[Eb[hs]])
                    if n > 0:
                        sch.op("dve", lambda e: e.tensor_tensor(out=Em[hs][:, 0:n, :], in0=E[hs][:, 0:n, :], in1=EB[:, tcls:tcls + n, h, :], op=ALU.mult),
                               reads=[Eb[hs], EBb], writes=[Emb[hs]])
                    pg = po[h // 4]
                    pgb = pob[h // 4]
                    hq = h % 4
                    for ci in range(n):
                        sch.op("pe", lambda e, ci=ci: e.matmul(pg[:, hq, :], lhsT=Em[hs][:, ci, :], rhs=vw[s][:, ci * 520 + h * 65:ci * 520 + (h + 1) * 65],
                                                               start=(ci == 0), stop=False),
                               reads=[Emb[hs], vwb[s]], writes=[pgb], signal=False, partial=True)
                    for j in range(2):
                        sch.op("pe", lambda e, j=j: e.matmul(pg[:, hq, :], lhsT=E[hs][:, n + j, :], rhs=vctx[:, j * 520 + h * 65:j * 520 + (h + 1) * 65],
                                                             start=(n == 0 and j == 0), stop=(j == 1)),
                               reads=[Eb[hs], vctxb], writes=[pgb], signal=(j == 1), partial=True)
                    if hq == 3:
                        g4 = h // 4
                        sch.op("dve", lambda e: e.reciprocal(out=rden[:], in_=pg[:, :, 64:65]), reads=[pgb], writes=[rdenb])
                        sch.op("dve", lambda e: e.tensor_tensor(out=yna[:, g4 * 4:(g4 + 1) * 4, :], in0=pg[:, :, 0:64],
                                                                in1=rden[:].to_broadcast([128, 4, 64]), op=ALU.mult),
                               reads=[pgb, rdenb], writes=[ynab], partial=True)
                ynaf = yna[:].rearrange("p h d -> p (h d)")
                for c in range(4):
                    sch.op("pe", lambda e, c=c: e.transpose(out=pT[:, c, :], in_=ynaf[:, c * 128:(c + 1) * 128], identity=ident[:]),
                           reads=[ynab, identb], writes=[pTb], signal=(c == 3), partial=True)
                sch.op("act", lambda e: e.copy(out=mt[:, 4:8, :], in_=pT[:]), reads=[pTb], writes=[mtb], partial=True)
                for hf in range(2):
                    for kc in range(8):
                        sch.op("pe", lambda e, kc=kc, hf=hf: e.matmul(py[:, hf, :], lhsT=mt[:, kc, :], rhs=Wo[:, kc, hf * 512:(hf + 1) * 512],
                                                                      start=(kc == 0), stop=(kc == 7)),
                               reads=[mtb, Wob], writes=[pyb], signal=(kc == 7 and hf == 1), partial=True)
                g1, g1b = mod[(w, 2)]
                sch.op("dve", lambda e: e.tensor_tensor(out=tmp[:], in0=pS[0][:].rearrange("p a b -> p (a b)"), in1=g1[:], op=ALU.mult),
                       reads=[pyb, g1b], writes=[tmpb])
                sch.op("dve", lambda e: e.tensor_tensor(out=ho[s][:], in0=tmp[:], in1=ht[s][:], op=ALU.add),
                       reads=[tmpb, htb[s]], writes=[hob[s]])
                sch.dma("pool", h_out[tok0:tok0 + 128, :], ho[s][:], hob[s], reads=[hob[s]], writes=[DB[h_out_name]], partial=True)
            sch.barrier()

    def phase_p2_odd(self, l, h_cur, h_name, h_out, h_out_name, last):
        nc, sch, I, S, DB = self.nc, self.sch, self.I, self.S, self.DB
        li = l // 2
        lam_init = 0.8 - 0.6 * math.exp(-0.3 * l)
        with ExitStack() as es:
            stage, stageb = self.make_stage(es, "q2_stg", 2, 1024)
            Wo, Wob = self.load_weight_bf16(es, "q2_Wo", I["od_w_out"][li], 8, D, stage, stageb, cast_eng=["pool", "dve"])
            mod = self.load_mod(es, l, [2])
            self.make_eps(es, "q2_eps")
            identf, identfb = self.sb(es, "q2_idf", [128, 128], F32), sch.buf("q2_idf")
            ident, identb = self.sb(es, "q2_id", [128, 128], BF16), sch.buf("q2_id")
            sch.dma("sp", identf[:], I["ident"], identfb, writes=[identfb])
            sch.op("dve", lambda e: e.tensor_copy(out=ident[:], in_=identf[:]), reads=[identfb], writes=[identb])
            smf, smfb = self.sb(es, "q2_smf", [128, 2, 128], F32), sch.buf("q2_smf")
            sm, smb = self.sb(es, "q2_sm", [128, 2, 128], BF16), sch.buf("q2_sm")
            sch.dma("sp", smf[:], I["swamask"], smfb, writes=[smfb])
            sch.op("dve", lambda e: e.tensor_copy(out=sm[:], in_=smf[:]), reads=[smfb], writes=[smb])
            es8, es8b = self.sb(es, "q2_es8", [128, 8, 1], F32), sch.buf("q2_es8")
            sch.dma("sp", es8[:].rearrange("p h o -> p (h o)"), I["od_sink"][li].partition_broadcast(128), es8b, writes=[es8b])
            sch.op("act", lambda e: e.activation(out=es8[:], in_=es8[:], func=AF.Exp), reads=[es8b], writes=[es8b])
            lamt, lamtb = self.sb(es, "q2_lamt", [128, 2, 2, 64], F32), sch.buf("q2_lamt")
            sch.dma("sp", lamt[:].rearrange("p a b d -> p (a b d)"), I["od_lam"][li].rearrange("a d -> (a d)").partition_broadcast(128), lamtb, writes=[lamtb])
            lp, lpb = self.sb(es, "q2_lp", [128, 2, 64], F32), sch.buf("q2_lp")
            sch.op("dve", lambda e: e.tensor_tensor(out=lp[:], in0=lamt[:, :, 0, :], in1=lamt[:, :, 1, :], op=ALU.mult), reads=[lamtb], writes=[lpb])
            ls, lsb = self.sb(es, "q2_ls", [128, 2], F32), sch.buf("q2_ls")
            sch.op("dve", lambda e: e.tensor_reduce(out=ls[:], in_=lp[:], axis=AX.X, op=ALU.add), reads=[lpb], writes=[lsb])
            sch.op("act", lambda e: e.activation(out=ls[:], in_=ls[:], func=AF.Exp), reads=[lsb], writes=[lsb])
            nlam, nlamb = self.sb(es, "q2_nlam", [128, 1], F32), sch.buf("q2_nlam")
            sch.op("dve", lambda e: e.tensor_tensor(out=nlam[:], in0=ls[:, 1:2], in1=ls[:, 0:1], op=ALU.subtract), reads=[lsb], writes=[nlamb])
            sch.op("dve", lambda e: e.tensor_scalar(out=nlam[:], in0=nlam[:], scalar1=-lam_init, scalar2=None, op0=ALU.add), reads=[nlamb], writes=[nlamb])
            slg, slgb = self.sb(es, "q2_slg", [128, 128], F32), sch.buf("q2_slg")
            sch.dma("sp", slg[:], I["od_subln"][li].partition_broadcast(128), slgb, writes=[slgb])
            sch.op("dve", lambda e: e.tensor_scalar(out=slg[:], in0=slg[:], scalar1=1.0 - lam_init, scalar2=None, op0=ALU.mult), reads=[slgb], writes=[slgb])
            kctx, kctxb = self.sb(es, "q2_kctx", [128, 5, NCTX], BF16), sch.buf("q2_kctx")
            sch.dma("sp", kctx[:], S["o_kctx"].rearrange("c p t -> p c t"), kctxb, reads=[DB["o_kctx"]], writes=[kctxb])
            ckc = [self.sb(es, f"q2_ckc{g}", [128, NCTX], BF16) for g in range(2)]
            ckcb = sch.bufs("q2_ckc", 2)
            for g in range(2):
                for hh in range(2):
                    sch.dma("sp", ckc[g][hh * 64:(hh + 1) * 64, :], S["o_kctx"][0, g * 64:(g + 1) * 64, :], ckcb[g],
                            reads=[DB["o_kctx"]], writes=[ckcb[g]], partial=(hh == 1))
            vctx, vctxb = self.sb(es, "q2_vctx", [128, 5, 260], BF16), sch.buf("q2_vctx")
            sch.dma("sp", vctx[:], S["o_vctx"].rearrange("c p t -> p c t"), vctxb, reads=[DB["o_vctx"]], writes=[vctxb])
            qt = [self.sb(es, f"q2_q{i}", [128, 8, 512], BF16) for i in range(2)]
            qtb = sch.bufs("q2_q", 2)
            kd = [[self.sb(es, f"q2_kd{i}_{g}", [128, 384], BF16) for g in range(2)] for i in range(2)]
            kdb = [sch.bufs(f"q2_kd{i}_", 2) for i in range(2)]
            vw = [self.sb(es, f"q2_vw{i}", [128, 3, 130], BF16) for i in range(2)]
            vwb = sch.bufs("q2_vw", 2)
            KH = [self.sb(es, f"q2_KH{i}", [128, NLAT], BF16) for i in range(2)]
            KHb = sch.bufs("q2_KH", 2)
            VH = [self.sb(es, f"q2_VH{i}", [128, NT * 130], BF16) for i in range(2)]
            VHb = sch.bufs("q2_VH", 2)
            ht = [self.sb(es, f"q2_h{i}", [128, D], F32) for i in range(2)]
            htb = sch.bufs("q2_h", 2)
            ho = [self.sb(es, f"q2_ho{i}", [128, D], F32) for i in range(2)]
            hob = sch.bufs("q2_ho", 2)
            tmp, tmpb = self.sb(es, "q2_tmp", [128, D], F32), sch.buf("q2_tmp")
            mixT = [self.sb(es, f"q2_mixT{i}", [128, 8, 128], BF16) for i in range(4)]
            mixTb = sch.bufs("q2_mixT", 4)
            Esw = [self.sb(es, f"q2_Esw{i}", [128, 5, 128], BF16) for i in range(2)]
            Eswb = sch.bufs("q2_Esw", 2)
            Ed = [self.sb(es, f"q2_Ed{i}", [128, 512], BF16) for i in range(3)]
            Edb = sch.bufs("q2_Ed", 3)
            yc, ycb = self.sb(es, "q2_yc", [128, 8, 64], BF16), sch.buf("q2_yc")
            rden, rdenb = self.sb(es, "q2_rden", [128, 4, 1], F32), sch.buf("q2_rden")
            rd2, rd2b = self.sb(es, "q2_rd2", [128, 2, 1], F32), sch.buf("q2_rd2")
            yd0 = [self.sb(es, f"q2_yd0{i}", [128, 2, 128], F32) for i in range(2)]
            yd0b = sch.bufs("q2_yd0", 2)
            o1, o1b = self.sb(es, "q2_o1", [128, 2, 128], F32), sch.buf("q2_o1")
            ydf, ydfb = self.sb(es, "q2_ydf", [128, 2, 128], F32), sch.buf("q2_ydf")
            sqj, sqjb = self.sb(es, "q2_sqj", [128, 2, 128], F32), sch.buf("q2_sqj")
            ss2, ss2b = self.sb(es, "q2_ss2", [128, 2], F32), sch.buf("q2_ss2")
            rs2, rs2b = self.sb(es, "q2_rs2", [128, 2], F32), sch.buf("q2_rs2")
            ydn = [self.sb(es, f"q2_ydn{i}", [128, 4, 128], BF16) for i in range(4)]
            ydnb = sch.bufs("q2_ydn", 4)
            pSall = self.ps(es, "q2_pS", [128, 2, 512])
            pSb = sch.bufs("q2_pS", 2)
            po = [self.ps(es, f"q2_po{i}", [128, 4, 65]) for i in range(2)]
            pob = sch.bufs("q2_po", 2)
            pacc = [self.ps(es, f"q2_pacc{i}", [128, 2, 130]) for i in range(2)]
            paccb = sch.bufs("q2_pacc", 2)
            pT = self.ps(es, "q2_pT", [128, 4, 128], BF16)
            pTb = sch.buf("q2_pT")

            nst = NST if last else NST + 1
            hcnt = 0
            dcnt = 0
            khc = 0
            tcnt = 0
            for st in range(nst):
                isctx = st == NST
                nt = 256 if isctx else 512
                nsub = nt // 128
                tok0 = st * 512
                w = 1 if isctx else 0
                sa = st % 2
                sch.dma("sp", qt[sa][:, :, 0:nt], S["o_qT"].rearrange("c p t -> p c t")[:, :, tok0:tok0 + nt], qtb[sa], reads=[DB["o_qT"]], writes=[qtb[sa]])
                for sub in range(nsub):
                    i = st * 4 + sub
                    s = tcnt % 2
                    tcnt += 1
                    mt, mtb = mixT[sub], mixTb[sub]
                    if isctx:
                        slots = []
                        jl, jh = 0, 0
                    else:
                        lo_t, hi_t = max(i - 1, 0), min(i + 1, NT - 1)
                        slots = list(range(lo_t - (i - 1), hi_t - (i - 1) + 1))
                        jl, jh = slots[0], slots[-1] + 1
                        for g in range(2):
                            for hh in range(2):
                                sch.dma("sp", kd[s][g][hh * 64:(hh + 1) * 64, jl * 128:jh * 128], S["o_blobK"][g * 64:(g + 1) * 64, lo_t * 128:(hi_t + 1) * 128],
                                        kdb[s][g], reads=[DB["o_blobK"]], writes=[kdb[s][g]], partial=(hh == 1))
                        sch.dma("sp", vw[s][:, jl:jh, :], S["o_blobV"][512:640, lo_t * 130:(hi_t + 1) * 130].rearrange("p (j c) -> p j c", c=130),
                                vwb[s], reads=[DB["o_blobV"]], writes=[vwb[s]])
                    for h in range(8):
                        g = h // 4
                        cr, hh = h // 2, h % 2
                        p0, p1 = hh * 64, hh * 64 + 64
                        hs = hcnt % 2
                        hcnt += 1
                        qv = qt[sa][p0:p1, cr, sub * 128:(sub + 1) * 128]
                        for j in slots:
                            sch.op("pe", lambda e, j=j: e.matmul(pSall[:, 0, j * 128:(j + 1) * 128], lhsT=kd[s][g][p0:p1, j * 128:(j + 1) * 128], rhs=qv,
                                                                 start=True, stop=True),
                                   reads=[kdb[s][g], qtb[sa]], writes=[pSb[0]], signal=(j == slots[-1]), partial=(j != slots[0]))
                        for jj in range(2):
                            sch.op("pe", lambda e, jj=jj: e.matmul(pSall[:, 1, jj * 128:(jj + 1) * 128], lhsT=ckc[g][p0:p1, jj * 128:(jj + 1) * 128], rhs=qv,
                                                                   start=True, stop=True),
                                   reads=[ckcb[g], qtb[sa]], writes=[pSb[1]], signal=(jj == 1), partial=(jj == 1))
                        E, Eb_ = Esw[hs], Eswb[hs]
                        if slots:
                            sch.op("act", lambda e: e.activation(out=E[:, jl:jh, :], in_=pSall[:, 0, jl * 128:jh * 128].rearrange("p (j c) -> p j c", c=128), func=AF.Exp),
                                   reads=[pSb[0]], writes=[Eb_])
                        sch.op("act", lambda e: e.activation(out=E[:, 3:5, :], in_=pSall[:, 1, 0:256].rearrange("p (j c) -> p j c", c=128), func=AF.Exp),
                               reads=[pSb[1]], writes=[Eb_], partial=bool(slots))
                        if 0 in slots:
                            sch.op("dve", lambda e: e.tensor_tensor(out=E[:, 0, :], in0=E[:, 0, :], in1=sm[:, 0, :], op=ALU.mult), reads=[Eb_, smb], writes=[Eb_])
                        if 2 in slots:
                            sch.op("dve", lambda e: e.tensor_tensor(out=E[:, 2, :], in0=E[:, 2, :], in1=sm[:, 1, :], op=ALU.mult), reads=[Eb_, smb], writes=[Eb_])
                        pg, pgb = po[h // 4], pob[h // 4]
                        hq = h % 4
                        for j in slots:
                            sch.op("pe", lambda e, j=j: e.matmul(pg[:, hq, :], lhsT=E[:, j, :], rhs=vw[s][:, j, g * 65:(g + 1) * 65], start=(j == slots[0]), stop=False),
                                   reads=[Eb_, vwb[s]], writes=[pgb], signal=False, partial=(not (hq == 0 and j == slots[0])))
                        for jj in range(2):
                            sch.op("pe", lambda e, jj=jj: e.matmul(pg[:, hq, :], lhsT=E[:, 3 + jj, :], rhs=vctx[:, 4, jj * 130 + g * 65:jj * 130 + (g + 1) * 65],
                                                                   start=(not slots and jj == 0), stop=(jj == 1)),
                                   reads=[Eb_, vctxb], writes=[pgb], signal=(jj == 1), partial=(not (hq == 0 and jj == 0 and not slots)))
                        if hq == 3:
                            g4 = h // 4
                            sch.op("dve", lambda e: e.tensor_tensor(out=rden[:], in0=pg[:, :, 64:65], in1=es8[:, g4 * 4:(g4 + 1) * 4, :], op=ALU.add),
                                   reads=[pgb, es8b], writes=[rdenb])
                            sch.op("dve", lambda e: e.reciprocal(out=rden[:], in_=rden[:]), reads=[rdenb], writes=[rdenb])
                            sch.op("dve", lambda e: e.tensor_tensor(out=yc[:, g4 * 4:(g4 + 1) * 4, :], in0=pg[:, :, 0:64],
                                                                    in1=rden[:].to_broadcast([128, 4, 64]), op=ALU.mult),
                                   reads=[pgb, rdenb], writes=[ycb], partial=(g4 == 1))
                    ycf = yc[:].rearrange("p h d -> p (h d)")
                    for c in range(4):
                        sch.op("pe", lambda e, c=c: e.transpose(out=pT[:, c, :], in_=ycf[:, c * 128:(c + 1) * 128], identity=ident[:]),
                               reads=[ycb, identb], writes=[pTb], signal=(c == 3), partial=(c > 0))
                    sch.op("act", lambda e: e.copy(out=mt[:, 0:4, :], in_=pT[:]), reads=[pTb], writes=[mtb])
                npair = nsub // 2
                for h in range(4):
                    kb = khc % 2
                    khc += 1
                    if not isctx:
                        sch.dma("sp", KH[kb][:], S["o_blobK"][(1 + h) * 128:(2 + h) * 128, :], KHb[kb], reads=[DB["o_blobK"]], writes=[KHb[kb]])
                        sch.dma("sp", VH[kb][:], S["o_blobV"][h * 128:(h + 1) * 128, :], VHb[kb], reads=[DB["o_blobV"]], writes=[VHb[kb]])
                        chunks = [("l", kt) for kt in range(NT)] + [("c", 0), ("c", 1)]
                    else:
                        chunks = [("c", 0), ("c", 1)]
                    for m in range(2):
                        p0, p1 = m * 64, m * 64 + 64
                        for ci, (kind, kt) in enumerate(chunks):
                            k = dcnt % 2
                            k3 = dcnt % 3
                            dcnt += 1
                            if kind == "l":
                                lh, lhb = KH[kb][p0:p1, kt * 128:(kt + 1) * 128], KHb[kb]
                                rv, rvb = VH[kb][:, kt * 130:kt * 130 + 129], VHb[kb]
                            else:
                                lh, lhb = kctx[p0:p1, 1 + h, kt * 128:(kt + 1) * 128], kctxb
                                rv, rvb = vctx[:, h, kt * 130:kt * 130 + 129], vctxb
                            sch.op("pe", lambda e, lh=lh, k=k: e.matmul(pSall[:, k, 0:nt], lhsT=lh, rhs=qt[sa][p0:p1, 4 + h, 0:nt], start=True, stop=True),
                                   reads=[lhb, qtb[sa]], writes=[pSb[k]])
                            sch.op("act", lambda e, k=k, k3=k3: e.activation(out=Ed[k3][:, 0:nt], in_=pSall[:, k, 0:nt], func=AF.Exp),
                                   reads=[pSb[k]], writes=[Edb[k3]])
                            for sub in range(nsub):
                                sch.op("pe", lambda e, sub=sub, k3=k3, rv=rv: e.matmul(pacc[sub // 2][:, sub % 2, 0:129], lhsT=Ed[k3][:, sub * 128:(sub + 1) * 128], rhs=rv,
                                                                                      start=(ci == 0 and sub % 2 == 0), stop=(ci == len(chunks) - 1)),
                                       reads=[Edb[k3], rvb], writes=[paccb[sub // 2]], signal=(sub % 2 == 1 and ci == len(chunks) - 1),
                                       partial=(not (ci == 0 and sub % 2 == 0)))
                        for pp in range(npair):
                            sch.op("dve", lambda e, pp=pp: e.reciprocal(out=rd2[:], in_=pacc[pp][:, :, 128:129]), reads=[paccb[pp]], writes=[rd2b])
                            if m == 0:
                                sch.op("dve", lambda e, pp=pp: e.tensor_tensor(out=yd0[pp][:], in0=pacc[pp][:, :, 0:128], in1=rd2[:].to_broadcast([128, 2, 128]), op=ALU.mult),
                                       reads=[paccb[pp], rd2b], writes=[yd0b[pp]])
                            else:
                                sch.op("dve", lambda e, pp=pp: e.tensor_tensor(out=o1[:], in0=pacc[pp][:, :, 0:128], in1=rd2[:].to_broadcast([128, 2, 128]), op=ALU.mult),
                                       reads=[paccb[pp], rd2b], writes=[o1b])
                                sch.op("dve", lambda e, pp=pp: e.scalar_tensor_tensor(out=ydf[:], in0=o1[:], scalar=nlam[:, 0:1], in1=yd0[pp][:], op0=ALU.mult, op1=ALU.add),
                                       reads=[o1b, nlamb, yd0b[pp]], writes=[ydfb])
                                sch.op("act", lambda e: e.activation(out=sqj[:], in_=ydf[:], func=AF.Square), reads=[ydfb], writes=[sqjb])
                                sch.op("dve", lambda e: e.tensor_reduce(out=ss2[:], in_=sqj[:], axis=AX.X, op=ALU.add), reads=[sqjb], writes=[ss2b])
                                self.rsqrt(rs2[:], rs2b, ss2[:], ss2b, 1.0 / 128)
                                for j in range(2):
                                    sub = pp * 2 + j
                                    sch.op("dve", lambda e, j=j, sub=sub: e.scalar_tensor_tensor(out=ydn[sub][:, h, :], in0=ydf[:, j, :], scalar=rs2[:, j:j + 1], in1=slg[:],
                                                                                                op0=ALU.mult, op1=ALU.mult),
                                           reads=[ydfb, rs2b, slgb], writes=[ydnb[sub]], partial=(h > 0))
                py = pSall
                for sub in range(nsub):
                    r0 = tok0 + sub * 128
                    s = tcnt % 2
                    tcnt += 1
                    mt, mtb = mixT[sub], mixTb[sub]
                    for c in range(4):
                        sch.op("pe", lambda e, c=c, sub=sub: e.transpose(out=pT[:, c, :], in_=ydn[sub][:, c, :], identity=ident[:]),
                               reads=[ydnb[sub], identb], writes=[pTb], signal=(c == 3), partial=(c > 0))
                    sch.op("act", lambda e, mt=mt: e.copy(out=mt[:, 4:8, :], in_=pT[:]), reads=[pTb], writes=[mtb], partial=True)
                    sch.dma("sp", ht[s][:], h_cur[r0:r0 + 128, :], htb[s], reads=[DB[h_name]], writes=[htb[s]])
                    for hf in range(2):
                        for kc in range(8):
                            sch.op("pe", lambda e, kc=kc, hf=hf, mt=mt: e.matmul(py[:, hf, :], lhsT=mt[:, kc, :], rhs=Wo[:, kc, hf * 512:(hf + 1) * 512],
                                                                                 start=(kc == 0), stop=(kc == 7)),
                                   reads=[mtb, Wob], writes=[pSb[hf]], signal=(kc == 7))
                    g1, g1b = mod[(w, 2)]
                    sch.op("dve", lambda e: e.tensor_tensor(out=tmp[:], in0=py[:].rearrange("p a b -> p (a b)"), in1=g1[:], op=ALU.mult),
                           reads=[pSb[0], pSb[1], g1b], writes=[tmpb])
                    sch.op("dve", lambda e, s=s: e.tensor_tensor(out=ho[s][:], in0=tmp[:], in1=ht[s][:], op=ALU.add),
                           reads=[tmpb, htb[s]], writes=[hob[s]])
                    sch.dma("pool", h_out[r0:r0 + 128, :], ho[s][:], hob[s], reads=[hob[s]], writes=[DB[h_out_name]], partial=True)
            sch.barrier()

    def phase_p3a(self, l, h_mid, h_name, last):
        nc, sch, I, S, DB = self.nc, self.sch, self.I, self.S, self.DB
        li = l // 2
        moe = (l % 2 == 1)
        with ExitStack() as es:
            mod = self.load_mod(es, l, [3, 4])
            self.make_eps(es, "p3_eps")
            identf, identfb = self.sb(es, "p3_idf", [128, 128], F32), sch.buf("p3_idf")
            sch.dma("sp", identf[:], I["ident"], identfb, writes=[identfb])
            if moe:
                rt, rtb = self.sb(es, "p3_rt", [128, NEXP, D], F32), sch.buf("p3_rt")
                sch.dma("sp", rt[:].rearrange("p e d -> p (e d)"), I["moe_routerT"][li].rearrange("e d -> (e d)").partition_broadcast(128), rtb, writes=[rtb])
                prod, prodb = self.sb(es, "p3_prod", [128, NEXP, D], F32), sch.buf("p3_prod")
            ht = [self.sb(es, f"p3_h{i}", [128, D], F32) for i in range(2)]
            htb = sch.bufs("p3_h", 2)
            junk, junkb = self.sb(es, "p3_junk", [128, D], F32), sch.buf("p3_junk")
            tmp, tmpb = self.sb(es, "p3_tmp", [128, D], F32), sch.buf("p3_tmp")
            ss, ssb = self.sb(es, "p3_ss", [128, 1], F32), sch.buf("p3_ss")
            rstd, rstdb = self.sb(es, "p3_rstd", [128, 1], F32), sch.buf("p3_rstd")
            a2 = [self.sb(es, f"p3_a{i}", [128, D], F32) for i in range(2)]
            a2b = sch.bufs("p3_a", 2)
            aTh = [self.sb(es, f"p3_aTh{i}", [128, 8, 128], BF16) for i in range(2)]
            aThb = sch.bufs("p3_aTh", 2)
            pT = [self.ps(es, f"p3_pT{i}", [128, 8, 128]) for i in range(2)]
            pTb = sch.bufs("p3_pT", 2)
            if moe:
                lg, lgb = self.sb(es, "p3_lg", [128, NEXP], F32), sch.buf("p3_lg")
                l2, l2b = self.sb(es, "p3_l2", [128, NEXP], F32), sch.buf("p3_l2")
                eq1, eq1b = self.sb(es, "p3_eq1", [128, NEXP], F32), sch.buf("p3_eq1")
                eq2, eq2b = self.sb(es, "p3_eq2", [128, NEXP], F32), sch.buf("p3_eq2")
                m1, m1b = self.sb(es, "p3_m1", [128, 1], F32), sch.buf("p3_m1")
                m2, m2b = self.sb(es, "p3_m2", [128, 1], F32), sch.buf("p3_m2")
                w2, w2b = self.sb(es, "p3_w2", [128, 1], F32), sch.buf("p3_w2")
                w1, w1b = self.sb(es, "p3_w1", [128, 1], F32), sch.buf("p3_w1")
            ntiles = NT if last else NT + 2
            for i in range(ntiles):
                s = i % 2
                w = 1 if i >= NT else 0
                tok0 = i * 128
                sch.dma("sp", ht[s][:], h_mid[tok0:tok0 + 128, :], htb[s], reads=[DB[h_name]], writes=[htb[s]])
                self.rms_mod_tile(ht[s], htb[s], ss, ssb, junk, junkb, rstd, rstdb, tmp, tmpb,
                                  mod[(w, 4)], mod[(w, 3)], a2[s][:], a2b[s])
                for kc in range(8):
                    sch.op("pe", lambda e, kc=kc: e.transpose(out=pT[s][:, kc, :], in_=a2[s][:, kc * 128:(kc + 1) * 128], identity=identf[:]),
                           reads=[a2b[s], identfb], writes=[pTb[s]], signal=(kc == 7), partial=True)
                sch.op("act", lambda e: e.copy(out=aTh[s][:], in_=pT[s][:]), reads=[pTb[s]], writes=[aThb[s]])
                sch.dma("pool", S["a2T"].rearrange("c p t -> p c t")[:, :, tok0:tok0 + 128], aTh[s][:], aThb[s], reads=[aThb[s]], writes=[DB["a2T"]], partial=True)
                if moe:
                    sch.op("dve", lambda e: e.tensor_tensor(out=prod[:], in0=rt[:], in1=a2[s][:].rearrange("p (o d) -> p o d", o=1).to_broadcast([128, NEXP, D]), op=ALU.mult),
                           reads=[rtb, a2b[s]], writes=[prodb])
                    sch.op("dve", lambda e: e.tensor_reduce(out=lg[:], in_=prod[:], axis=AX.X, op=ALU.add), reads=[prodb], writes=[lgb])
                    sch.op("dve", lambda e: e.tensor_reduce(out=m1[:], in_=lg[:], axis=AX.X, op=ALU.max), reads=[lgb], writes=[m1b])
                    sch.op("dve", lambda e: e.tensor_scalar(out=eq1[:], in0=lg[:], scalar1=m1[:, 0:1], scalar2=None, op0=ALU.is_equal),
                           reads=[lgb, m1b], writes=[eq1b])
                    sch.op("dve", lambda e: e.scalar_tensor_tensor(out=l2[:], in0=eq1[:], scalar=-1e30, in1=lg[:], op0=ALU.mult, op1=ALU.add),
                           reads=[eq1b, lgb], writes=[l2b])
                    sch.op("dve", lambda e: e.tensor_reduce(out=m2[:], in_=l2[:], axis=AX.X, op=ALU.max), reads=[l2b], writes=[m2b])
                    sch.op("dve", lambda e: e.tensor_scalar(out=eq2[:], in0=l2[:], scalar1=m2[:, 0:1], scalar2=None, op0=ALU.is_equal),
                           reads=[l2b, m2b], writes=[eq2b])
                    sch.op("dve", lambda e: e.tensor_tensor(out=w2[:], in0=m2[:], in1=m1[:], op=ALU.subtract), reads=[m1b, m2b], writes=[w2b])
                    sch.op("act", lambda e: e.activation(out=w2[:], in_=w2[:], func=AF.Sigmoid), reads=[w2b], writes=[w2b])
                    sch.op("dve", lambda e: e.tensor_scalar(out=w1[:], in0=w2[:], scalar1=-1.0, scalar2=1.0, op0=ALU.mult, op1=ALU.add),
                           reads=[w2b], writes=[w1b])
                    sch.op("dve", lambda e: e.tensor_scalar(out=eq1[:], in0=eq1[:], scalar1=w1[:, 0:1], scalar2=None, op0=ALU.mult),
                           reads=[eq1b, w1b], writes=[eq1b])
                    sch.op("dve", lambda e, i=i: e.scalar_tensor_tensor(out=self.gates_sb[:, i, :], in0=eq2[:], scalar=w2[:, 0:1], in1=eq1[:], op0=ALU.mult, op1=ALU.add),
                           reads=[eq2b, w2b, eq1b], writes=[self.gates_b], partial=True)
            sch.barrier()

    def phase_p3b(self, l, h_mid, h_name, h_nxt, h_nxt_name, last, final):
        nc, sch, I, S, DB = self.nc, self.sch, self.I, self.S, self.DB
        li = l // 2
        moe = (l % 2 == 1)
        nex = NEXP if moe else 2
        with ExitStack() as es:
            mod = self.load_mod(es, l, [5])
            stage, stageb = self.make_stage(es, "p3b_stg", 3, 1408)
            W1 = self.sb(es, "p3b_W1", [128, 8, DFE], BF16)
            W3 = self.sb(es, "p3b_W3", [128, 8, DFE], BF16)
            W2 = self.sb(es, "p3b_W2", [128, 11, D], BF16)
            W1b, W3b, W2b = sch.buf("W1"), sch.buf("W3"), sch.buf("W2")
            aT = [self.sb(es, f"p3b_aT{i}", [128, 8, 512], BF16) for i in range(2)]
            aTb = sch.bufs("p3b_aT", 2)
            gT = [self.sb(es, f"p3b_gT{i}", [128, 11, 512], BF16) for i in range(2)]
            gTb = sch.bufs("p3b_gT", 2)
            sl = [self.sb(es, f"p3b_sl{i}", [128, 512], F32) for i in range(2)]
            slb = sch.bufs("p3b_sl", 2)
            acc = [self.sb(es, f"p3b_acc{i}", [128, D], F32) for i in range(3)]
            accb = sch.bufs("p3b_acc", 3)
            tmp = [self.sb(es, f"p3b_tmp{i}", [128, D], F32) for i in range(2)]
            tmpb = sch.bufs("p3b_tmp", 2)
            ones, onesb = self.sb(es, "p3b_ones", [128, 1], F32), sch.buf("p3b_ones")
            sch.op("pool", lambda e: e.memset(ones[:], 1.0), writes=[onesb])
            p1 = [self.ps(es, f"p3b_p1{i}", [128, 512]) for i in range(2)]
            p1b = sch.bufs("p3b_p1", 2)
            p3 = [self.ps(es, f"p3b_p3{i}", [128, 512]) for i in range(2)]
            p3b_ = sch.bufs("p3b_p3", 2)
            py = [self.ps(es, f"p3b_py{i}", [128, 2, 512]) for i in range(2)]
            pyb = sch.bufs("p3b_py", 2)
            nst = NST if last else NST + 1
            stc = 0
            jc = 0
            tc_ = 0
            chain = [(h_mid, h_name)]
            ab = [(S["accA"], "accA"), (S["accB"], "accB")]
            for e_ in range(nex - 1):
                chain.append(ab[e_ % 2])
            chain.append((h_nxt, h_nxt_name))
            for ex in range(nex):
                src, srcn = chain[ex]
                dst, dstn = chain[ex + 1]
                if moe:
                    w1s = I["moe_w1"][li, ex].rearrange("(k p) n -> p k n", p=128)
                    w3s = I["moe_w3"][li, ex].rearrange("(k p) n -> p k n", p=128)
                    w2s = I["moe_w2"][li, ex].rearrange("(k p) n -> p k n", p=128)
                else:
                    w1s = I["ffn_w1"][li].rearrange("(k p) n -> p k n", p=128)[:, :, ex * DFE:(ex + 1) * DFE]
                    w3s = I["ffn_w3"][li].rearrange("(k p) n -> p k n", p=128)[:, :, ex * DFE:(ex + 1) * DFE]
                    w2s = I["ffn_w2"][li][ex * DFE:(ex + 1) * DFE, :].rearrange("(k p) n -> p k n", p=128)
                for (wt, wb, ws, nk, ncol) in ((W1, W1b, w1s, 8, DFE), (W3, W3b, w3s, 8, DFE), (W2, W2b, w2s, 11, D)):
                    for k in range(nk):
                        sx = stc % 3
                        stc += 1
                        sch.dma("sp", stage[sx][:, 0:ncol], ws[:, k, :], stageb[sx], writes=[stageb[sx]])
                        if stc % 2 == 0:
                            sch.op("pool", lambda e, wt=wt, k=k, sx=sx, ncol=ncol: e.tensor_copy(out=wt[:, k, :], in_=stage[sx][:, 0:ncol]),
                                   reads=[stageb[sx]], writes=[wb], partial=(k > 0))
                        else:
                            sch.op("dve", lambda e, wt=wt, k=k, sx=sx, ncol=ncol: e.tensor_copy(out=wt[:, k, :], in_=stage[sx][:, 0:ncol]),
                                   reads=[stageb[sx]], writes=[wb], partial=(k > 0))
                for st in range(nst):
                    isctx = st == NST
                    nt = 256 if isctx else 512
                    nsub = nt // 128
                    tok0 = st * 512
                    w = 1 if isctx else 0
                    sa = st % 2
                    sch.dma("sp", aT[sa][:, :, 0:nt], S["a2T"].rearrange("c p t -> p c t")[:, :, tok0:tok0 + nt], aTb[sa], reads=[DB["a2T"]], writes=[aTb[sa]])
                    g, gb_ = gT[sa], gTb[sa]
                    for j in range(11):
                        k = jc % 2
                        jc += 1
                        for kc in range(8):
                            sch.op("pe", lambda e, kc=kc: e.matmul(p1[k][:, 0:nt], lhsT=W1[:, kc, j * 128:(j + 1) * 128], rhs=aT[sa][:, kc, 0:nt],
                                                                   start=(kc == 0), stop=(kc == 7)),
                                   reads=[W1b, aTb[sa]], writes=[p1b[k]], signal=(kc == 7))
                        for kc in range(8):
                            sch.op("pe", lambda e, kc=kc: e.matmul(p3[k][:, 0:nt], lhsT=W3[:, kc, j * 128:(j + 1) * 128], rhs=aT[sa][:, kc, 0:nt],
                                                                   start=(kc == 0), stop=(kc == 7)),
                                   reads=[W3b, aTb[sa]], writes=[p3b_[k]], signal=(kc == 7))
                        sch.op("act", lambda e: e.activation(out=sl[k][:, 0:nt], in_=p1[k][:, 0:nt], func=AF.Silu), reads=[p1b[k]], writes=[slb[k]])
                        sch.op("dve", lambda e: e.tensor_tensor(out=g[:, j, 0:nt], in0=sl[k][:, 0:nt], in1=p3[k][:, 0:nt], op=ALU.mult),
                               reads=[slb[k], p3b_[k]], writes=[gb_], partial=True)
                    for sub in range(nsub):
                        t = tc_ % 2
                        t3 = tc_ % 3
                        tc_ += 1
                        r0 = tok0 + sub * 128
                        sch.dma("sp", acc[t3][:], src[r0:r0 + 128, :], accb[t3], reads=[DB[srcn]], writes=[accb[t3]])
                        for hf in range(2):
                            for j in range(11):
                                sch.op("pe", lambda e, j=j, hf=hf: e.matmul(py[t][:, hf, :], lhsT=g[:, j, sub * 128:(sub + 1) * 128], rhs=W2[:, j, hf * 512:(hf + 1) * 512],
                                                                           start=(j == 0), stop=(j == 10)),
                                       reads=[gb_, W2b], writes=[pyb[t]], signal=(j == 10 and hf == 1), partial=True)
                        g2, g2b = mod[(w, 5)]
                        if moe:
                            gsc, gscb = self.gates_sb[:, st * 4 + sub, ex:ex + 1], self.gates_b
                        else:
                            gsc, gscb = ones[:, 0:1], onesb
                        sch.op("dve", lambda e, gsc=gsc: e.scalar_tensor_tensor(out=tmp[t][:], in0=py[t][:].rearrange("p a b -> p (a b)"), scalar=gsc, in1=g2[:],
                                                                                op0=ALU.mult, op1=ALU.mult),
                               reads=[pyb[t], gscb, g2b], writes=[tmpb[t]])
                        sch.op("pool", lambda e: e.tensor_tensor(out=acc[t3][:], in0=tmp[t][:], in1=acc[t3][:], op=ALU.add),
                               reads=[tmpb[t], accb[t3]], writes=[accb[t3]])
                        if dstn == "out" and r0 >= self.out_rows:
                            continue
                        sch.dma("pool", dst[r0:r0 + 128, :], acc[t3][:], accb[t3], reads=[accb[t3]], writes=[DB[dstn]], partial=True)
                        if self.debug and ex == nex - 1 and not final:
                            pass
            sch.barrier()


def _prep_inputs(inp):
    f = lambda a: np.ascontiguousarray(np.asarray(a, dtype=np.float32))
    x, c, ctx, c_ctx = f(inp["x"]), f(inp["c"]), f(inp["ctx"]), f(inp["c_ctx"])
    shared = {}
    for k in ["ada_w", "ada_b", "norm1_g", "norm2_g", "ev_w_in", "ev_w_out", "ffn_w1", "ffn_w3", "ffn_w2",
              "od_w_in", "od_sink", "od_w_out", "moe_router", "moe_w1", "moe_w3", "moe_w2"]:
        shared[k] = f(inp[k])
    cw = f(inp["ev_conv_w"])
    shared["ev_convw"] = np.ascontiguousarray(cw.reshape(2, 3, 4, 128).transpose(0, 3, 2, 1))
    shared["ev_qg"] = np.ascontiguousarray(np.tile(f(inp["ev_q_g"]), (1, 2))[:, :, None])
    shared["ev_kg"] = np.ascontiguousarray(np.tile(f(inp["ev_k_g"]), (1, 2))[:, :, None])
    shared["od_g"] = np.ascontiguousarray(np.stack([np.tile(f(inp[k]), (1, 2)) for k in ("od_cq_g", "od_ck_g", "od_dq_g", "od_dk_g")], -1))
    shared["od_lam"] = np.ascontiguousarray(np.stack([f(inp[k]) for k in ("od_lam_q1", "od_lam_k1", "od_lam_q2", "od_lam_k2")], 1))
    shared["od_subln"] = f(inp["od_subln_g"])
    shared["moe_routerT"] = np.ascontiguousarray(f(inp["moe_router"]).transpose(0, 2, 1))
    rpb = f(inp["ev_rpb"])
    cst = _consts()
    dr, dc, _ = _na_tables()
    bias = rpb[:, :, dr, dc]
    cst["ev_bias"] = np.ascontiguousarray(bias.transpose(0, 3, 2, 1, 4))

    def split(d):
        o = {}
        for k, v in d.items():
            if k in ("ident", "blockones", "rotT", "ropeC", "ropeS", "swamask", "namask"):
                o[k] = v
            else:
                for l in range(v.shape[0]):
                    o[f"{k}_{l}"] = np.ascontiguousarray(v[l])
        return o
    shared = split(shared)
    shared.update(split(cst))
    in_maps = []
    for core in range(NCORES):
        b = core % 4
        m = dict(shared)
        m["xin"] = np.ascontiguousarray(np.concatenate([x[b], ctx[b]], 0))
        cc = np.stack([c[b].reshape(8, 128).T, c_ctx.reshape(8, 128).T], -1)
        m["cc"] = np.ascontiguousarray(cc)
        in_maps.append(m)
    return in_maps


def _run(inp, nlayers=DEPTH, debug=False):
    bld = Builder(nlayers=nlayers, debug=debug)
    nc = bld.build()
    in_maps = _prep_inputs(inp)
    names = set(bld.inputs.keys())
    in_maps = [{k: v for k, v in m.items() if k in names} for m in in_maps]
    res = run_bass_kernel_spmd(nc, in_maps, core_ids=list(range(NCORES)))
    return res.results


def kernel(**inputs):
    results = _run(inputs, DEPTH, False)
    B = 4
    out = np.empty((B, NLAT, D), np.float32)
    for b in range(B):
        out[b] = results[b]["out"]
    return out
```

```python
import math
import os
from contextlib import ExitStack

import numpy as np
import concourse.bass as bass
import concourse.mybir as mybir
from concourse.bass_utils import run_bass_kernel_spmd

F32 = mybir.dt.float32
BF16 = mybir.dt.bfloat16
AF = mybir.ActivationFunctionType
ALU = mybir.AluOpType
AX = mybir.AxisListType

D = 1024
NLAT = 8192
NCTX = 256
T = NLAT + NCTX
NTILE = T // 128
NT = NLAT // 128
NST = NLAT // 512
DEPTH = 4
EPS = 1e-6
DFF = 2816
DFE = 1408
NEXP = 8
SAME_ENGINE_SYNC = True
NCORES = 4
PAIRS = [[0, 1], [2, 3], [4, 5], [6, 7]]


class Buf:
    __slots__ = ("name", "w", "r", "sem", "cnt")

    def __init__(self, name):
        self.name = name
        self.w = {}
        self.r = {}
        self.sem = None
        self.cnt = 0


class Eng:
    def __init__(self, name, handle, sem):
        self.name = name
        self.h = handle
        self.sem = sem
        self.cnt = 0
        self.known = {}


def _merge(d, s):
    for k, v in s.items():
        if d.get(k, 0) < v:
            d[k] = v


class Sched:
    def __init__(self, nc, es):
        self.nc = nc
        self.free_sems = []
        self.all_sems = []
        names = [("pe", nc.tensor), ("act", nc.scalar), ("dve", nc.vector), ("pool", nc.gpsimd), ("sp", nc.sync)]
        self.eng = {}
        for n, h in names:
            s = es.enter_context(nc.semaphore("e_" + n))
            self.eng[n] = Eng(n, h, s)
        for i in range(80):
            s = es.enter_context(nc.semaphore(f"d{i}"))
            self.free_sems.append([s, 0])
        self.semcnt = {}
        self.phase_bufs = []

    def buf(self, name):
        b = Buf(name)
        self.phase_bufs.append(b)
        return b

    def bufs(self, name, n):
        return [self.buf(f"{name}{i}") for i in range(n)]

    def _deps(self, e, reads, writes, partial):
        need = {}
        for b in reads:
            _merge(need, b.w)
        for b in writes:
            _merge(need, b.r)
            if not partial:
                _merge(need, b.w)
        for sem, val in need.items():
            if sem is e.sem and (e.name in ("pe", "sp") or not SAME_ENGINE_SYNC):
                continue
            if e.known.get(sem, 0) >= val:
                continue
            e.h.wait_ge(sem, val)
            e.known[sem] = val

    def _post(self, tok, reads, writes, partial):
        sem, val = tok
        for b in reads:
            if b.r.get(sem, 0) < val:
                b.r[sem] = val
        for b in writes:
            if partial:
                if b.w.get(sem, 0) < val:
                    b.w[sem] = val
            else:
                b.w = {sem: val}
                b.r = {}

    def op(self, en, fn, reads=(), writes=(), signal=True, partial=False):
        e = self.eng[en]
        self._deps(e, reads, writes, partial)
        inst = fn(e.h)
        if signal:
            e.cnt += 1
            inst.then_inc(e.sem, 1)
            tok = (e.sem, e.cnt)
        else:
            tok = (e.sem, e.cnt + 1)
        self._post(tok, reads, writes, partial)
        return inst

    def dma(self, en, out, in_, sembuf, reads=(), writes=(), partial=False, **kw):
        e = self.eng[en]
        self._deps(e, reads, writes, partial)
        if sembuf.sem is None:
            s = self.free_sems.pop()
            sembuf.sem, sembuf.cnt = s[0], s[1]
        inst = e.h.dma_start(out=out, in_=in_, **kw)
        sembuf.cnt += 16
        inst.then_inc(sembuf.sem, 16)
        tok = (sembuf.sem, sembuf.cnt)
        self.semcnt[id(sembuf.sem)] = tok
        self._post(tok, reads, writes, partial)
        return inst

    def collective(self, ins, outs, sembuf, reads=(), writes=()):
        e = self.eng["pool"]
        self._deps(e, reads, writes, False)
        if sembuf.sem is None:
            s = self.free_sems.pop()
            sembuf.sem, sembuf.cnt = s[0], s[1]
        inst = e.h.collective_compute("AllGather", ALU.bypass, replica_groups=PAIRS, ins=ins, outs=outs)
        sembuf.cnt += 1
        inst.then_inc(sembuf.sem, 1)
        tok = (sembuf.sem, sembuf.cnt)
        self.semcnt[id(sembuf.sem)] = tok
        self._post(tok, reads, writes, False)

    def barrier(self, release=True):
        toks = {}
        for e in self.eng.values():
            if e.cnt > 0:
                toks[e.sem] = e.cnt
        for sem, val in self.semcnt.values():
            toks[sem] = val
        for e in self.eng.values():
            for sem, val in toks.items():
                if sem is e.sem and e.name in ("pe", "sp"):
                    continue
                if e.known.get(sem, 0) >= val:
                    continue
                e.h.wait_ge(sem, val)
                e.known[sem] = val
        if release:
            for b in self.phase_bufs:
                if b.sem is not None:
                    self.free_sems.append([b.sem, b.cnt])
                    b.sem = None
                b.w = {}
                b.r = {}
            self.phase_bufs = []


def _na_tables():
    classes = []
    def tile_chunks(i):
        if i == 0:
            return 0, 6
        if i == NT - 1:
            return NT - 2, 6
        return i, 5
    order = [("I", 5), (0, 6), (1, 5), (NT - 2, 5), (NT - 1, 6)]
    dr = np.zeros((27, 128, 128), np.int64)
    dc = np.zeros((27, 128, 128), np.int64)
    mk = np.zeros((27, 128, 128), np.float32)
    kk = np.arange(128)[:, None]
    qq = np.arange(128)[None, :]
    t = 0
    for cls, n in order:
        i = 5 if cls == "I" else cls
        e0, n2 = tile_chunks(i)
        assert n2 == n
        g = i
        if cls == "I":
            g = 10
            e0 = None
        for ci in range(n):
            if cls == "I":
                m = g - 2 + ci
            else:
                m = e0 + ci - 2
            r = 2 * g + qq // 64
            qc = qq % 64
            kr = 2 * m + kk // 64
            kc = kk % 64
            r0 = np.clip(r - 4, 0, 120)
            c0 = np.clip(qc - 8, 0, 48)
            ok = (kr >= r0) & (kr <= r0 + 7) & (kc >= c0) & (kc < c0 + 16) & (m >= 0) & (m <= NT - 1)
            ok = np.broadcast_to(ok, (128, 128))
            dri = np.clip(kr - r + 7, 0, 14)
            dci = np.clip(kc - qc + 15, 0, 30)
            dr[t] = np.where(ok, dri, 0)
            dc[t] = np.where(ok, dci, 0)
            mk[t] = ok.astype(np.float32)
            t += 1
    assert t == 27
    return dr, dc, mk


def _rope_tables():
    t = np.arange(NLAT)
    pos = np.stack([t // 64, t % 64], -1).astype(np.float32)
    nq = 16
    inv = (np.float32(10000.0) ** (-np.arange(nq, dtype=np.float32) / nq)).astype(np.float32)
    ang = pos[:, :, None] * inv
    cos = np.cos(ang).astype(np.float32)
    sin = np.sin(ang).astype(np.float32)
    C = np.zeros((128, NLAT), np.float32)
    S = np.zeros((128, NLAT), np.float32)
    for hh in range(2):
        for ax in range(2):
            for hf in range(2):
                p0 = hh * 64 + ax * 32 + hf * 16
                C[p0:p0 + 16] = cos[:, ax, :].T
                S[p0:p0 + 16] = sin[:, ax, :].T
    return C, S


def _rot_matrix():
    R = np.zeros((128, 128), np.float32)
    for hh in range(2):
        for ax in range(2):
            for j in range(16):
                a = hh * 64 + ax * 32 + j
                b = a + 16
                R[a, b] = -1.0
                R[b, a] = 1.0
    return np.ascontiguousarray(R.T)


def _consts():
    c = {}
    c["ident"] = np.eye(128, dtype=np.float32)
    bo = np.zeros((128, 128), np.float32)
    bo[:64, :64] = 1.0
    bo[64:, 64:] = 1.0
    c["blockones"] = bo
    c["rotT"] = _rot_matrix()
    C, S = _rope_tables()
    c["ropeC"] = C
    c["ropeS"] = S
    kk = np.arange(128)[:, None]
    qq = np.arange(128)[None, :]
    prev = (kk >= qq).astype(np.float32)
    nxt = (kk <= qq).astype(np.float32)
    sm = np.stack([prev, nxt], 1)
    c["swamask"] = np.ascontiguousarray(sm)
    _, _, mk = _na_tables()
    c["namask"] = np.ascontiguousarray(mk.transpose(1, 0, 2))
    return c


class Builder:
    def __init__(self, nlayers=DEPTH, debug=False):
        self.nlayers = nlayers
        self.l0 = int(os.environ.get("KL0", "0"))
        self.debug = debug
        self.nc = bass.Bass("TRN2", target_bir_lowering=False)
        self.inputs = {}

    def din(self, name, shape, dt=F32):
        t = self.nc.dram_tensor(name, list(shape), dt, kind="ExternalInput")
        self.inputs[name] = t
        return t.ap()

    def dscr(self, name, shape, dt):
        return self.nc.dram_tensor(name, list(shape), dt).ap()

    def sb(self, es, name, shape, dt):
        self.uid = getattr(self, "uid", 0) + 1
        return es.enter_context(self.nc.sbuf_tensor(f"{name}_u{self.uid}", list(shape), dt))

    def ps(self, es, name, shape, dt=F32):
        self.uid = getattr(self, "uid", 0) + 1
        return es.enter_context(self.nc.psum_tensor(f"{name}_u{self.uid}", list(shape), dt))

    def build(self):
        nc = self.nc
        NL = self.nlayers
        bld = self
        SHAPES = {
            "xin": [T, D], "cc": [128, 8, 2], "ident": [128, 128], "blockones": [128, 128], "rotT": [128, 128],
            "ropeC": [128, NLAT], "ropeS": [128, NLAT], "swamask": [128, 2, 128], "namask": [128, 27, 128],
        }
        LSHAPES = {
            "ada_w": [D, 6 * D], "ada_b": [6 * D], "norm1_g": [D], "norm2_g": [D],
            "ev_w_in": [D, 3072], "ev_convw": [128, 4, 3], "ev_qg": [128, 1], "ev_kg": [128, 1], "ev_bias": [128, 27, 8, 128],
            "ev_w_out": [D, D], "ffn_w1": [D, DFF], "ffn_w3": [D, DFF], "ffn_w2": [DFF, D],
            "od_w_in": [D, 2304], "od_g": [128, 4], "od_sink": [8], "od_lam": [4, 64], "od_subln": [128], "od_w_out": [D, D],
            "moe_router": [D, NEXP], "moe_routerT": [NEXP, D], "moe_w1": [NEXP, D, DFE], "moe_w3": [NEXP, D, DFE], "moe_w2": [NEXP, DFE, D],
        }

        class LayerView:
            def __init__(self, name):
                self.name = name
                self.cache = {}

            def __getitem__(self, idx):
                if isinstance(idx, tuple):
                    l, rest = idx[0], idx[1:]
                else:
                    l, rest = idx, None
                if l not in self.cache:
                    self.cache[l] = bld.din(f"{self.name}_{l}", LSHAPES[self.name])
                ap = self.cache[l]
                return ap if rest is None else ap[rest]

        class LazyInputs:
            def __init__(self):
                self.cache = {}

            def __getitem__(self, name):
                if name not in self.cache:
                    if name in SHAPES:
                        self.cache[name] = bld.din(name, SHAPES[name])
                    else:
                        self.cache[name] = LayerView(name)
                return self.cache[name]

        I = LazyInputs()
        self.I = I
        self.out_rows = T if self.debug else NLAT
        out = nc.dram_tensor("out", [self.out_rows, D], F32, kind="ExternalOutput").ap()
        self.out = out
        S = {}
        S["hX"] = self.dscr("hX", [T, D], F32)
        S["hY"] = self.dscr("hY", [T, D], F32)
        S["accA"] = self.dscr("accA", [T, D], F32)
        S["accB"] = self.dscr("accB", [T, D], F32)
        S["modv"] = self.dscr("modv", [DEPTH, 2, 6 * D], F32)
        S["a2T"] = self.dscr("a2T", [8, 128, T], BF16)
        S["gates"] = self.dscr("gates", [T, NEXP], F32)
        S["e_gbT"] = self.dscr("e_gbT", [4, 128, T], BF16)
        S["e_qT"] = self.dscr("e_qT", [4, 128, T], BF16)
        S["e_kctx"] = self.dscr("e_kctx", [4, 128, NCTX], BF16)
        S["e_zctx"] = self.dscr("e_zctx", [4, 128, NCTX + 2], BF16)
        S["e_vctx"] = self.dscr("e_vctx", [128, 2 * 520], BF16)
        S["e_kext"] = self.dscr("e_kext", [4, 128, (NT + 4) * 128], BF16)
        S["e_vext"] = self.dscr("e_vext", [128, (NT + 4) * 520], BF16)
        S["e_zext"] = self.dscr("e_zext", [4, 128, NLAT + 2], BF16)
        S["o_qT"] = self.dscr("o_qT", [8, 128, T], BF16)
        S["o_blobK"] = self.dscr("o_blobK", [5 * 128, NLAT], BF16)
        S["o_kctx"] = self.dscr("o_kctx", [5, 128, NCTX], BF16)
        S["o_blobV"] = self.dscr("o_blobV", [5 * 128, NT * 130], BF16)
        S["o_vctx"] = self.dscr("o_vctx", [5, 128, 2 * 130], BF16)
        self.S = S

        with ExitStack() as es:
            self.sch = Sched(nc, es)
            sch = self.sch
            self.DB = {k: Buf(k) for k in list(S.keys()) + ["xin", "out", "dbg_h"]}
            self.gates_sb = self.sb(es, "gates_sb", [128, NTILE, NEXP], F32)
            self.gates_b = Buf("gates_sb")
            self.phase_mod()
            stop = os.environ.get("KSTOP", "")
            h_cur, h_cur_name = I["xin"], "xin"
            names = ["hX", "hY"]
            ni = 0
            for l in range(self.l0, NL):
                last = l == DEPTH - 1
                if stop == "mod":
                    break
                if l % 2 == 0:
                    self.phase_p1_even(l, h_cur, h_cur_name)
                    if stop == "p1":
                        break
                    h_mid, h_mid_name = S[names[ni]], names[ni]
                    ni ^= 1
                    self.phase_p2_even(l, h_cur, h_cur_name, h_mid, h_mid_name, last)
                else:
                    self.phase_p1_odd(l, h_cur, h_cur_name)
                    if stop == "p1":
                        break
                    h_mid, h_mid_name = S[names[ni]], names[ni]
                    ni ^= 1
                    self.phase_p2_odd(l, h_cur, h_cur_name, h_mid, h_mid_name, last)
                if stop == "p2":
                    cpb = sch.buf("cp_dbg")
                    sch.dma("pool", out[0:self.out_rows, :], h_mid[0:self.out_rows, :], cpb, reads=[self.DB[h_mid_name]], writes=[self.DB["out"]])
                    break
                final = (l == NL - 1)
                if final:
                    h_nxt, h_nxt_name = out, "out"
                else:
                    h_nxt, h_nxt_name = S[names[ni]], names[ni]
                    ni ^= 1
                self.phase_p3a(l, h_mid, h_mid_name, last)
                if stop == "p3a":
                    cpb = sch.buf("cp_dbg")
                    sch.dma("pool", out[0:self.out_rows, :], h_mid[0:self.out_rows, :], cpb, reads=[self.DB[h_mid_name]], writes=[self.DB["out"]])
                    break
                self.phase_p3b(l, h_mid, h_mid_name, h_nxt, h_nxt_name, last, final)
                h_cur, h_cur_name = h_nxt, h_nxt_name
            sch.barrier(release=False)
        return nc

    def load_consts(self, es, names):
        sch = self.sch
        res = {}
        for nm, shape, want_bf in names:
            t = self.sb(es, "c_" + nm, shape, F32)
            b = sch.buf("c_" + nm)
            sch.dma("sp", t[:], self.I[nm] if not isinstance(nm, tuple) else None, b, writes=[b])
            if want_bf:
                tb = self.sb(es, "cb_" + nm, shape, BF16)
                bb = sch.buf("cb_" + nm)
                sch.op("dve", lambda e, tb=tb, t=t: e.tensor_copy(out=tb[:], in_=t[:]), reads=[b], writes=[bb])
                res[nm] = (tb, bb)
            else:
                res[nm] = (t, b)
        return res

    def load_weight_bf16(self, es, name, src_ap, kchunks, ncols, stage, stage_bufs, cast_eng="pool", col_chunk=None):
        sch = self.sch
        wt = self.sb(es, name, [128, kchunks, ncols], BF16)
        wb = sch.buf(name)
        src = src_ap.rearrange("(k p) n -> p k n", p=128)
        cc = col_chunk or stage[0].shape[-1]
        i = 0
        for k in range(kchunks):
            for c0 in range(0, ncols, cc):
                c1 = min(ncols, c0 + cc)
                s = i % len(stage_bufs)
                i += 1
                st, stb = stage[s], stage_bufs[s]
                sch.dma("sp", st[:, 0:c1 - c0], src[:, k, c0:c1], stb, writes=[stb])
                eng = cast_eng if not isinstance(cast_eng, (list, tuple)) else cast_eng[i % len(cast_eng)]
                if eng == "act":
                    sch.op("act", lambda e, k=k, c0=c0, c1=c1, st=st: e.copy(out=wt[:, k, c0:c1], in_=st[:, 0:c1 - c0]),
                           reads=[stb], writes=[wb], partial=True)
                else:
                    sch.op(eng, lambda e, k=k, c0=c0, c1=c1, st=st: e.tensor_copy(out=wt[:, k, c0:c1], in_=st[:, 0:c1 - c0]),
                           reads=[stb], writes=[wb], partial=True)
        return wt, wb

    def make_stage(self, es, name, n, cols):
        tiles = [self.sb(es, f"{name}{i}", [128, cols], F32) for i in range(n)]
        return tiles, self.sch.bufs(name, n)

    def load_bcast(self, es, name, src_row_ap, n):
        t = self.sb(es, name, [128, n], F32)
        b = self.sch.buf(name)
        self.sch.dma("sp", t[:], src_row_ap.partition_broadcast(128), b, writes=[b])
        return t, b

    def make_eps(self, es, name):
        self.eps_t = self.sb(es, name, [128, 1], F32)
        self.eps_b = self.sch.buf(name)
        self.sch.op("pool", lambda e: e.memset(self.eps_t[:], EPS), writes=[self.eps_b])

    def rsqrt(self, out_ap, outb, in_ap, inb, scale):
        sch = self.sch
        eps_t, eps_b = self.eps_t, self.eps_b
        sch.op("act", lambda e: e.activation(out=out_ap, in_=in_ap, func=AF.Sqrt, bias=eps_t[:, 0:1], scale=scale),
               reads=[inb, eps_b], writes=[outb])
        sch.op("dve", lambda e: e.reciprocal(out=out_ap, in_=out_ap), reads=[outb], writes=[outb])

    def rms_mod_tile(self, htile, hb, ss, ssb, junk, junkb, rstd, rstdb, tmp, tmpb, Gb, SHb, out_ap, outb):
        sch = self.sch
        G, Gbuf = Gb
        SH, SHbuf = SHb
        sch.op("act", lambda e: e.activation(out=junk[:], in_=htile[:], func=AF.Square), reads=[hb], writes=[junkb])
        sch.op("dve", lambda e: e.tensor_reduce(out=ss[:, 0:1], in_=junk[:], axis=AX.X, op=ALU.add), reads=[junkb], writes=[ssb])
        self.rsqrt(rstd[:, 0:1], rstdb, ss[:, 0:1], ssb, 1.0 / D)
        sch.op("dve", lambda e: e.scalar_tensor_tensor(out=tmp[:], in0=htile[:], scalar=rstd[:, 0:1], in1=G[:],
                                                       op0=ALU.mult, op1=ALU.mult),
               reads=[hb, rstdb, Gbuf], writes=[tmpb])
        sch.op("dve", lambda e: e.tensor_tensor(out=out_ap, in0=tmp[:], in1=SH[:], op=ALU.add),
               reads=[tmpb, SHbuf], writes=[outb])

    def phase_mod(self):
        nc, sch, I, S = self.nc, self.sch, self.I, self.S
        with ExitStack() as es:
            cc = self.sb(es, "m_cc", [128, 8, 2], F32)
            ccb = sch.buf("m_cc")
            sg = self.sb(es, "m_sg", [128, 8, 2], F32)
            sgb = sch.buf("m_sg")
            sil = self.sb(es, "m_sil", [128, 8, 2], F32)
            silb = sch.buf("m_sil")
            sch.dma("sp", cc[:], I["cc"], ccb, writes=[ccb])
            sch.op("act", lambda e: e.activation(out=sg[:], in_=cc[:], func=AF.Sigmoid), reads=[ccb], writes=[sgb])
            sch.op("dve", lambda e: e.tensor_tensor(out=sil[:], in0=cc[:], in1=sg[:], op=ALU.mult),
                   reads=[ccb, sgb], writes=[silb])
            wst = [self.sb(es, f"m_w{i}", [128, 8, 512], F32) for i in range(2)]
            wstb = sch.bufs("m_w", 2)
            row = self.sb(es, "m_row", [2, 6 * D], F32)
            rowb = sch.buf("m_row")
            bias = self.sb(es, "m_bias", [2, 6 * D], F32)
            biasb = sch.buf("m_bias")
            ng = self.sb(es, "m_ng", [2, 2 * D], F32)
            ngb = sch.buf("m_ng")
            pm = [self.ps(es, f"m_ps{i}", [2, 512]) for i in range(2)]
            pmb = sch.bufs("m_ps", 2)
            it = 0
            for l in range(self.l0, self.nlayers):
                sch.dma("sp", bias[:], I["ada_b"][l].partition_broadcast(2), biasb, writes=[biasb])
                sch.dma("sp", ng[:, 0:D], I["norm1_g"][l].partition_broadcast(2), ngb, writes=[ngb])
                sch.dma("sp", ng[:, D:2 * D], I["norm2_g"][l].partition_broadcast(2), ngb, writes=[ngb], partial=True)
                wsrc = I["ada_w"][l].rearrange("(k p) n -> p k n", p=128)
                for c in range(12):
                    s = it % 2
                    it += 1
                    sch.dma("sp", wst[s][:], wsrc[:, :, c * 512:(c + 1) * 512], wstb[s], writes=[wstb[s]])
                    for k in range(8):
                        sch.op("pe", lambda e, s=s, k=k: e.matmul(pm[s][:], lhsT=sil[:, k, :], rhs=wst[s][:, k, :],
                                                                 start=(k == 0), stop=(k == 7)),
                               reads=[silb, wstb[s]], writes=[pmb[s]], signal=(k == 7))
                    sch.op("dve", lambda e, s=s, c=c: e.tensor_tensor(out=row[:, c * 512:(c + 1) * 512], in0=pm[s][:],
                                                                      in1=bias[:, c * 512:(c + 1) * 512], op=ALU.add),
                           reads=[pmb[s], biasb], writes=[rowb], partial=True)
                sch.op("dve", lambda e: e.scalar_tensor_tensor(out=row[:, D:2 * D], in0=row[:, D:2 * D], scalar=1.0,
                                                               in1=ng[:, 0:D], op0=ALU.add, op1=ALU.mult),
                       reads=[rowb, ngb], writes=[rowb])
                sch.op("dve", lambda e: e.scalar_tensor_tensor(out=row[:, 4 * D:5 * D], in0=row[:, 4 * D:5 * D], scalar=1.0,
                                                               in1=ng[:, D:2 * D], op0=ALU.add, op1=ALU.mult),
                       reads=[rowb, ngb], writes=[rowb])
                sch.dma("pool", S["modv"][l], row[:], rowb, reads=[rowb], writes=[self.DB["modv"]], partial=True)
            sch.barrier()

    def load_mod(self, es, l, idxs):
        res = {}
        for w in (0, 1):
            for ix in idxs:
                t = self.sb(es, f"mod{w}_{ix}", [128, D], F32)
                b = self.sch.buf(f"mod{w}_{ix}")
                self.sch.dma("sp", t[:], self.S["modv"][l, w, ix * D:(ix + 1) * D].partition_broadcast(128), b,
                             reads=[self.DB["modv"]], writes=[b])
                res[(w, ix)] = (t, b)
        return res

    def phase_p1(self, l, h_cur, h_name, even):
        nc, sch, I, S, DB = self.nc, self.sch, self.I, self.S, self.DB
        li = l // 2
        with ExitStack() as es:
            ncols = 3072 if even else 2304
            wsrc = (I["ev_w_in"] if even else I["od_w_in"])[li]
            stage, stageb = self.make_stage(es, "p1_stg", 2, 2048)
            W, Wb = self.load_weight_bf16(es, "p1_W", wsrc, 8, ncols, stage, stageb, cast_eng=["pool", "dve"], col_chunk=1536 if even else 1152)
            mod = self.load_mod(es, l, [0, 1])
            self.make_eps(es, "p1_eps")
            identf, identfb = self.sb(es, "p1_idf", [128, 128], F32), sch.buf("p1_idf")
            ident, identb = self.sb(es, "p1_id", [128, 128], BF16), sch.buf("p1_id")
            sch.dma("sp", identf[:], I["ident"], identfb, writes=[identfb])
            sch.op("dve", lambda e: e.tensor_copy(out=ident[:], in_=identf[:]), reads=[identfb], writes=[identb])
            bof, bofb = self.sb(es, "p1_bof", [128, 128], F32), sch.buf("p1_bof")
            bo, bob = self.sb(es, "p1_bo", [128, 128], BF16), sch.buf("p1_bo")
            sch.dma("sp", bof[:], I["blockones"], bofb, writes=[bofb])
            sch.op("dve", lambda e: e.tensor_copy(out=bo[:], in_=bof[:]), reads=[bofb], writes=[bob])
            if even:
                gq, gqb = self.sb(es, "p1_gq", [128, 1], F32), sch.buf("p1_gq")
                gk, gkb = self.sb(es, "p1_gk", [128, 1], F32), sch.buf("p1_gk")
                sch.dma("sp", gq[:], I["ev_qg"][li], gqb, writes=[gqb])
                sch.dma("sp", gk[:], I["ev_kg"][li], gkb, writes=[gkb])
                sch.op("dve", lambda e: e.tensor_scalar(out=gq[:], in0=gq[:], scalar1=0.125, scalar2=None, op0=ALU.mult),
                       reads=[gqb], writes=[gqb])
                nfm = 20
            else:
                g4, g4b = self.sb(es, "p1_g4", [128, 4], F32), sch.buf("p1_g4")
                sch.dma("sp", g4[:], I["od_g"][li], g4b, writes=[g4b])
                sch.op("dve", lambda e: e.tensor_scalar(out=g4[:, 0:1], in0=g4[:, 0:1], scalar1=0.125, scalar2=None, op0=ALU.mult),
                       reads=[g4b], writes=[g4b])
                sch.op("dve", lambda e: e.tensor_scalar(out=g4[:, 2:3], in0=g4[:, 2:3], scalar1=0.125, scalar2=None, op0=ALU.mult),
                       reads=[g4b], writes=[g4b])
                rotf, rotfb = self.sb(es, "p1_rotf", [128, 128], F32), sch.buf("p1_rotf")
                rot, rotb = self.sb(es, "p1_rot", [128, 128], BF16), sch.buf("p1_rot")
                sch.dma("sp", rotf[:], I["rotT"], rotfb, writes=[rotfb])
                sch.op("dve", lambda e: e.tensor_copy(out=rot[:], in_=rotf[:]), reads=[rotfb], writes=[rotb])
                ropeCt = [self.sb(es, f"p1_rc{i}", [128, 512], F32) for i in range(2)]
                ropeCtb = sch.bufs("p1_rc", 2)
                ropeSt = [self.sb(es, f"p1_rs{i}", [128, 512], F32) for i in range(2)]
                ropeStb = sch.bufs("p1_rs", 2)
            ht = [self.sb(es, f"p1_h{i}", [128, D], F32) for i in range(2)]
            htb = sch.bufs("p1_h", 2)
            junk, junkb = self.sb(es, "p1_junk", [128, D], F32), sch.buf("p1_junk")
            tmp, tmpb = self.sb(es, "p1_tmp", [128, D], F32), sch.buf("p1_tmp")
            ss, ssb = self.sb(es, "p1_ss", [128, 1], F32), sch.buf("p1_ss")
            rstd, rstdb = self.sb(es, "p1_rstd", [128, 1], F32), sch.buf("p1_rstd")
            abf = [self.sb(es, f"p1_a{i}", [128, D], BF16) for i in range(2)]
            abfb = sch.bufs("p1_a", 2)
            aT = [self.sb(es, f"p1_aT{i}", [128, 8, 512], BF16) for i in range(2)]
            aTb = sch.bufs("p1_aT", 2)
            pT = [self.ps(es, f"p1_pT{i}", [128, 8, 128], BF16) for i in range(2)]
            pTb = sch.bufs("p1_pT", 2)
            pf = [self.ps(es, f"p1_pf{i}", [128, 512]) for i in range(3)]
            pfb = sch.bufs("p1_pf", 3)
            pms = [self.ps(es, f"p1_pms{i}", [128, 512]) for i in range(2)]
            pmsb = sch.bufs("p1_pms", 2)
            pv = self.ps(es, "p1_pv", [128, 512])
            pvb = sch.buf("p1_pv")
            sq = [self.sb(es, f"p1_sq{i}", [128, 512], BF16) for i in range(2)]
            sqb = sch.bufs("p1_sq", 2)
            rs = [self.sb(es, f"p1_rs{i}", [128, 512], F32) for i in range(2)]
            rsb = sch.bufs("p1_rsb", 2)
            usb = [self.sb(es, f"p1_u{i}", [128, 512], F32) for i in range(2)]
            usbb = sch.bufs("p1_u", 2)
            if even:
                stg = [self.sb(es, f"p1_st{i}", [128, 16, 512], BF16) for i in range(2)]
                vst = [self.sb(es, f"p1_vst{i}", [128, 8, 65], BF16) for i in range(2)]
            else:
                stg = [self.sb(es, f"p1_st{i}", [128, 13, 512], BF16) for i in range(2)]
                vst = [self.sb(es, f"p1_vst{i}", [128, 650], BF16) for i in range(2)]
                xn = [self.sb(es, f"p1_xn{i}", [128, 512], F32) for i in range(2)]
                xnb = sch.bufs("p1_xn", 2)
                xnh = [self.sb(es, f"p1_xnh{i}", [128, 512], BF16) for i in range(2)]
                xnhb = sch.bufs("p1_xnh", 2)
                t1 = [self.sb(es, f"p1_t1{i}", [128, 512], F32) for i in range(2)]
                t1b = sch.bufs("p1_t1", 2)
            stgb = sch.bufs("p1_st", 2)
            vstb = sch.bufs("p1_vst", 2)
            for i in range(2):
                sch.op("pool", lambda e, i=i: e.memset(vst[i][:], 1.0), writes=[vstb[i]])
            if even:
                zt, ztb = self.sb(es, "p1_zero", [128, 1040], BF16), sch.buf("p1_zero")
                sch.op("pool", lambda e: e.memset(zt[:], 0.0), writes=[ztb])
                kxz = S["e_kext"].rearrange("c p t -> p c t")
                zxz = S["e_zext"].rearrange("c p t -> p c t")
                z4 = zt[:, 0:1024].rearrange("p (c t) -> p c t", c=4)
                sch.dma("pool", kxz[:, :, 0:256], z4, ztb, reads=[ztb], writes=[DB["e_kext"]], partial=True)
                sch.dma("pool", kxz[:, :, 256 + NLAT:512 + NLAT], z4, ztb, reads=[ztb], writes=[DB["e_kext"]], partial=True)
                sch.dma("pool", S["e_vext"][:, 0:1040], zt[:, 0:1040], ztb, reads=[ztb], writes=[DB["e_vext"]], partial=True)
                sch.dma("pool", S["e_vext"][:, (NT + 2) * 520:(NT + 4) * 520], zt[:, 0:1040], ztb, reads=[ztb], writes=[DB["e_vext"]], partial=True)
                sch.dma("pool", zxz[:, :, 0:1], zt[:, 0:4].rearrange("p (c o) -> p c o", o=1), ztb, reads=[ztb], writes=[DB["e_zext"]], partial=True, allow_slow_non_contiguous=True)
                sch.dma("pool", zxz[:, :, NLAT + 1:NLAT + 2], zt[:, 4:8].rearrange("p (c o) -> p c o", o=1), ztb, reads=[ztb], writes=[DB["e_zext"]], partial=True, allow_slow_non_contiguous=True)
                zc = S["e_zctx"].rearrange("c p t -> p c t")
                sch.dma("pool", zc[:, :, 0:1], zt[:, 0:4].rearrange("p (c o) -> p c o", o=1), ztb, reads=[ztb], writes=[DB["e_zctx"]], partial=True, allow_slow_non_contiguous=True)
                sch.dma("pool", zc[:, :, NCTX + 1:NCTX + 2], zt[:, 4:8].rearrange("p (c o) -> p c o", o=1), ztb, reads=[ztb], writes=[DB["e_zctx"]], partial=True, allow_slow_non_contiguous=True)

            cnt = {"ms": 0, "pf": 0, "u": 0, "xn": 0}

            def norm_chunk(psrc, psrcb, nt, gvec, gvecb, dst, dstb, rope_tok0=None):
                k = cnt["ms"] % 2
                cnt["ms"] += 1
                sch.op("act", lambda e: e.activation(out=sq[k][:, 0:nt], in_=psrc[:, 0:nt], func=AF.Square),
                       reads=[psrcb], writes=[sqb[k]])
                sch.op("pe", lambda e: e.matmul(pms[k][:, 0:nt], lhsT=bo[:], rhs=sq[k][:, 0:nt], start=True, stop=True),
                       reads=[bob, sqb[k]], writes=[pmsb[k]])
                self.rsqrt(rs[k][:, 0:nt], rsb[k], pms[k][:, 0:nt], pmsb[k], 1.0 / 64)
                if rope_tok0 is None:
                    sch.op("dve", lambda e: e.scalar_tensor_tensor(out=dst, in0=psrc[:, 0:nt], scalar=gvec, in1=rs[k][:, 0:nt],
                                                                   op0=ALU.mult, op1=ALU.mult),
                           reads=[psrcb, gvecb, rsb[k]], writes=[dstb], partial=True)
                else:
                    x = cnt["xn"] % 2
                    cnt["xn"] += 1
                    ropeC, ropeCb, ropeS, ropeSb = cnt["rope"]
                    sch.op("dve", lambda e: e.scalar_tensor_tensor(out=xn[x][:, 0:nt], in0=psrc[:, 0:nt], scalar=gvec, in1=rs[k][:, 0:nt],
                                                                   op0=ALU.mult, op1=ALU.mult),
                           reads=[psrcb, gvecb, rsb[k]], writes=[xnb[x]])
                    sch.op("act", lambda e: e.copy(out=xnh[x][:, 0:nt], in_=xn[x][:, 0:nt]), reads=[xnb[x]], writes=[xnhb[x]])
                    k2 = cnt["ms"] % 2
                    cnt["ms"] += 1
                    sch.op("pe", lambda e: e.matmul(pms[k2][:, 0:nt], lhsT=rot[:], rhs=xnh[x][:, 0:nt], start=True, stop=True),
                           reads=[rotb, xnhb[x]], writes=[pmsb[k2]])
                    sch.op("dve", lambda e: e.tensor_tensor(out=t1[x][:, 0:nt], in0=xn[x][:, 0:nt], in1=ropeC[:, 0:nt], op=ALU.mult),
                           reads=[xnb[x], ropeCb], writes=[t1b[x]])
                    sch.op("dve", lambda e: e.tensor_tensor(out=xn[x][:, 0:nt], in0=pms[k2][:, 0:nt], in1=ropeS[:, 0:nt], op=ALU.mult),
                           reads=[pmsb[k2], ropeSb], writes=[xnb[x]])
                    sch.op("dve", lambda e: e.tensor_tensor(out=dst, in0=t1[x][:, 0:nt], in1=xn[x][:, 0:nt], op=ALU.add),
                           reads=[t1b[x], xnb[x]], writes=[dstb], partial=True)

            def proj_fm(j, a, ab, nt):
                k = cnt["pf"] % 3
                cnt["pf"] += 1
                for kc in range(8):
                    sch.op("pe", lambda e, kc=kc: e.matmul(pf[k][:, 0:nt], lhsT=W[:, kc, j * 128:(j + 1) * 128], rhs=a[:, kc, 0:nt],
                                                           start=(kc == 0), stop=(kc == 7)),
                           reads=[Wb, ab], writes=[pfb[k]], signal=(kc == 7))
                return pf[k], pfb[k]

            nst = NST + 1
            tile_i = 0
            for st in range(nst):
                isctx = st == NST
                nt = 256 if isctx else 512
                nsub = nt // 128
                tok0 = st * 512
                w = 1 if isctx else 0
                sa = st % 2
                a, ab = aT[sa], aTb[sa]
                for sub in range(nsub):
                    s2 = tile_i % 2
                    tile_i += 1
                    r0 = tok0 + sub * 128
                    sch.dma("sp", ht[s2][:], h_cur[r0:r0 + 128, :], htb[s2], reads=[DB[h_name]], writes=[htb[s2]])
                    self.rms_mod_tile(ht[s2], htb[s2], ss, ssb, junk, junkb, rstd, rstdb, tmp, tmpb,
                                      mod[(w, 1)], mod[(w, 0)], abf[s2][:], abfb[s2])
                    for kc in range(8):
                        sch.op("pe", lambda e, kc=kc: e.transpose(out=pT[s2][:, kc, :], in_=abf[s2][:, kc * 128:(kc + 1) * 128], identity=ident[:]),
                               reads=[abfb[s2], identb], writes=[pTb[s2]], signal=(kc == 7), partial=True)
                    sch.op("act", lambda e: e.copy(out=a[:, :, sub * 128:(sub + 1) * 128], in_=pT[s2][:]),
                           reads=[pTb[s2]], writes=[ab], partial=True)
                sg = st % 2
                sgt, sgb = stg[sg], stgb[sg]
                rope0 = None if (even or isctx) else tok0
                if rope0 is not None:
                    rr = st % 2
                    sch.dma("sp", ropeCt[rr][:], I["ropeC"][:, tok0:tok0 + 512], ropeCtb[rr], writes=[ropeCtb[rr]])
                    sch.dma("sp", ropeSt[rr][:], I["ropeS"][:, tok0:tok0 + 512], ropeStb[rr], writes=[ropeStb[rr]])
                    cnt["rope"] = (ropeCt[rr], ropeCtb[rr], ropeSt[rr], ropeStb[rr])
                if even:
                    for c in range(4):
                        pu, pub = proj_fm(8 + c, a, ab, nt)
                        ku = cnt["u"] % 2
                        cnt["u"] += 1
                        sch.op("act", lambda e: e.copy(out=usb[ku][:, 0:nt], in_=pu[:, 0:nt]), reads=[pub], writes=[usbb[ku]])
                        pg, pgb = proj_fm(4 + c, a, ab, nt)
                        sch.op("dve", lambda e: e.tensor_tensor(out=sgt[:, 4 + c, 0:nt], in0=pg[:, 0:nt], in1=usb[ku][:, 0:nt], op=ALU.mult),
                               reads=[pgb, usbb[ku]], writes=[sgb], partial=True)
                        pb, pbb = proj_fm(c, a, ab, nt)
                        sch.op("act", lambda e: e.copy(out=sgt[:, c, 0:nt], in_=pb[:, 0:nt]), reads=[pbb], writes=[sgb], partial=True)
                    for c in range(4):
                        pq, pqb = proj_fm(12 + c, a, ab, nt)
                        norm_chunk(pq, pqb, nt, gq[:, 0:1], gqb, sgt[:, 8 + c, 0:nt], sgb)
                        pk, pkb = proj_fm(16 + c, a, ab, nt)
                        norm_chunk(pk, pkb, nt, gk[:, 0:1], gkb, sgt[:, 12 + c, 0:nt], sgb)
                else:
                    for c in range(8):
                        pq, pqb = proj_fm(c, a, ab, nt)
                        gi = 0 if c < 4 else 2
                        norm_chunk(pq, pqb, nt, g4[:, gi:gi + 1], g4b, sgt[:, c, 0:nt], sgb, rope0)
                    pk, pkb = proj_fm(8, a, ab, nt)
                    norm_chunk(pk, pkb, nt, g4[:, 1:2], g4b, sgt[:, 8, 0:nt], sgb, rope0)
                    for c in range(4):
                        pk, pkb = proj_fm(10 + c, a, ab, nt)
                        norm_chunk(pk, pkb, nt, g4[:, 3:4], g4b, sgt[:, 9 + c, 0:nt], sgb, rope0)
                for sub in range(nsub):
                    tl = st * 4 + sub
                    sv = tl % 2
                    if even:
                        for kc in range(8):
                            sch.op("pe", lambda e, kc=kc: e.matmul(pv[:, :], lhsT=a[:, kc, sub * 128:(sub + 1) * 128], rhs=W[:, kc, 2560:3072],
                                                                   start=(kc == 0), stop=(kc == 7)),
                                   reads=[Wb, ab], writes=[pvb], signal=(kc == 7))
                        sch.op("act", lambda e: e.copy(out=vst[sv][:, :, 0:64], in_=pv[:, :].rearrange("p (h d) -> p h d", d=64)),
                               reads=[pvb], writes=[vstb[sv]])
                        src = vst[sv][:].rearrange("p h d -> p (h d)")
                        if isctx:
                            sch.dma("pool", S["e_vctx"][:, sub * 520:(sub + 1) * 520], src, vstb[sv], reads=[vstb[sv]], writes=[DB["e_vctx"]], partial=True)
                        else:
                            sch.dma("pool", S["e_vext"][:, (tl + 2) * 520:(tl + 3) * 520], src, vstb[sv], reads=[vstb[sv]], writes=[DB["e_vext"]], partial=True)
                    else:
                        for kc in range(8):
                            sch.op("pe", lambda e, kc=kc: e.matmul(pv[:, 0:128], lhsT=a[:, kc, sub * 128:(sub + 1) * 128], rhs=W[:, kc, 1152:1280],
                                                                   start=(kc == 0), stop=(kc == 7)),
                                   reads=[Wb, ab], writes=[pvb], signal=(kc == 7))
                        sch.op("act", lambda e: e.copy(out=vst[sv][:, 520:650].rearrange("p (h d) -> p h d", d=65)[:, :, 0:64],
                                                       in_=pv[:, 0:128].rearrange("p (h d) -> p h d", d=64)),
                               reads=[pvb], writes=[vstb[sv]])
                        for kc in range(8):
                            sch.op("pe", lambda e, kc=kc: e.matmul(pv[:, :], lhsT=a[:, kc, sub * 128:(sub + 1) * 128], rhs=W[:, kc, 1792:2304],
                                                                   start=(kc == 0), stop=(kc == 7)),
                                   reads=[Wb, ab], writes=[pvb], signal=(kc == 7))
                        sch.op("act", lambda e: e.copy(out=vst[sv][:, 0:520].rearrange("p (h d) -> p h d", d=130)[:, :, 0:128],
                                                       in_=pv[:, :].rearrange("p (h d) -> p h d", d=128)),
                               reads=[pvb], writes=[vstb[sv]], partial=True)
                        for sec in range(5):
                            src = vst[sv][:, sec * 130:(sec + 1) * 130]
                            if isctx:
                                sch.dma("pool", S["o_vctx"][sec, :, sub * 130:(sub + 1) * 130], src, vstb[sv], reads=[vstb[sv]], writes=[DB["o_vctx"]], partial=True)
                            else:
                                sch.dma("pool", S["o_blobV"][sec * 128:(sec + 1) * 128, tl * 130:(tl + 1) * 130], src, vstb[sv],
                                        reads=[vstb[sv]], writes=[DB["o_blobV"]], partial=True)
                if even:
                    sch.dma("pool", S["e_gbT"].rearrange("c p t -> p c t")[:, :, tok0:tok0 + nt], sgt[:, 0:4, 0:nt], sgb, reads=[sgb], writes=[DB["e_gbT"]], partial=True)
                    sch.dma("pool", S["e_qT"].rearrange("c p t -> p c t")[:, :, tok0:tok0 + nt], sgt[:, 8:12, 0:nt], sgb, reads=[sgb], writes=[DB["e_qT"]], partial=True)
                    if isctx:
                        sch.dma("pool", S["e_zctx"].rearrange("c p t -> p c t")[:, :, 1:NCTX + 1], sgt[:, 4:8, 0:nt], sgb, reads=[sgb], writes=[DB["e_zctx"]], partial=True)
                        sch.dma("pool", S["e_kctx"].rearrange("c p t -> p c t"), sgt[:, 12:16, 0:nt], sgb, reads=[sgb], writes=[DB["e_kctx"]], partial=True)
                    else:
                        sch.dma("pool", S["e_kext"].rearrange("c p t -> p c t")[:, :, 256 + tok0:256 + tok0 + nt], sgt[:, 12:16, 0:nt], sgb, reads=[sgb], writes=[DB["e_kext"]], partial=True)
                        sch.dma("pool", S["e_zext"].rearrange("c p t -> p c t")[:, :, 1 + tok0:1 + tok0 + nt], sgt[:, 4:8, 0:nt], sgb, reads=[sgb], writes=[DB["e_zext"]], partial=True)
                else:
                    sch.dma("pool", S["o_qT"].rearrange("c p t -> p c t")[:, :, tok0:tok0 + nt], sgt[:, 0:8, 0:nt], sgb, reads=[sgb], writes=[DB["o_qT"]], partial=True)
                    if isctx:
                        sch.dma("pool", S["o_kctx"].rearrange("c p t -> p c t"), sgt[:, 8:13, 0:nt], sgb, reads=[sgb], writes=[DB["o_kctx"]], partial=True)
                    else:
                        bk = S["o_blobK"].rearrange("(c p) t -> p c t", p=128)
                        sch.dma("pool", bk[:, :, tok0:tok0 + nt], sgt[:, 8:13, 0:nt], sgb, reads=[sgb], writes=[DB["o_blobK"]], partial=True)
            sch.barrier()

    def phase_p1_even(self, l, h_cur, h_name):
        self.phase_p1(l, h_cur, h_name, True)

    def phase_p1_odd(self, l, h_cur, h_name):
        self.phase_p1(l, h_cur, h_name, False)

    def phase_p2_even(self, l, h_cur, h_name, h_out, h_out_name, last):
        nc, sch, I, S, DB = self.nc, self.sch, self.I, self.S, self.DB
        li = l // 2
        with ExitStack() as es:
            stage, stageb = self.make_stage(es, "p2_stg", 2, 1024)
            Wo, Wob = self.load_weight_bf16(es, "p2_Wo", I["ev_w_out"][li], 8, D, stage, stageb, cast_eng=["pool", "dve"])
            mod = self.load_mod(es, l, [2])
            identf, identfb = self.sb(es, "p2_idf", [128, 128], F32), sch.buf("p2_idf")
            ident, identb = self.sb(es, "p2_id", [128, 128], BF16), sch.buf("p2_id")
            sch.dma("sp", identf[:], I["ident"], identfb, writes=[identfb])
            sch.op("dve", lambda e: e.tensor_copy(out=ident[:], in_=identf[:]), reads=[identfb], writes=[identb])
            cw, cwb = self.sb(es, "p2_cw", [128, 4, 3], F32), sch.buf("p2_cw")
            sch.dma("sp", cw[:], I["ev_convw"][li], cwb, writes=[cwb])
            EB, EBb = self.sb(es, "p2_EB", [128, 27, 8, 128], BF16), sch.buf("p2_EB")
            mk, mkb = self.sb(es, "p2_mk", [128, 27, 128], F32), sch.buf("p2_mk")
            sch.dma("sp", mk[:], I["namask"], mkb, writes=[mkb])
            ebt, ebtb = self.sb(es, "p2_ebt", [128, 1024], F32), sch.buf("p2_ebt")
            for t in range(27):
                s = t % 2
                sch.dma("sp", stage[s][:], I["ev_bias"][li, :, t].rearrange("p h q -> p (h q)"), stageb[s], writes=[stageb[s]])
                sch.op("act", lambda e, s=s: e.activation(out=ebt[:], in_=stage[s][:], func=AF.Exp), reads=[stageb[s]], writes=[ebtb])
                sch.op("dve", lambda e, t=t: e.tensor_tensor(out=EB[:, t, :, :], in0=ebt[:].rearrange("p (h q) -> p h q", h=8),
                                                             in1=mk[:, t:t + 1, :].to_broadcast([128, 8, 128]), op=ALU.mult),
                       reads=[ebtb, mkb], writes=[EBb], partial=True)
            kctx, kctxb = self.sb(es, "p2_kctx", [128, 4, NCTX], BF16), sch.buf("p2_kctx")
            vctx, vctxb = self.sb(es, "p2_vctx", [128, 2 * 520], BF16), sch.buf("p2_vctx")
            sch.dma("sp", kctx[:], S["e_kctx"].rearrange("c p t -> p c t"), kctxb, reads=[DB["e_kctx"]], writes=[kctxb])
            sch.dma("sp", vctx[:], S["e_vctx"], vctxb, reads=[DB["e_vctx"]], writes=[vctxb])
            qt = [self.sb(es, f"p2_q{i}", [128, 4, 128], BF16) for i in range(2)]
            qtb = sch.bufs("p2_q", 2)
            kw = [self.sb(es, f"p2_kw{i}", [128, 4, 768], BF16) for i in range(2)]
            kwb = sch.bufs("p2_kw", 2)
            vw = [self.sb(es, f"p2_vw{i}", [128, 6 * 520], BF16) for i in range(2)]
            vwb = sch.bufs("p2_vw", 2)
            zw = [self.sb(es, f"p2_zw{i}", [128, 4, 130], BF16) for i in range(2)]
            zwb = sch.bufs("p2_zw", 2)
            gbw = [self.sb(es, f"p2_gb{i}", [128, 4, 128], BF16) for i in range(2)]
            gbwb = sch.bufs("p2_gb", 2)
            ht = [self.sb(es, f"p2_h{i}", [128, D], F32) for i in range(2)]
            htb = sch.bufs("p2_h", 2)
            ho = [self.sb(es, f"p2_ho{i}", [128, D], F32) for i in range(2)]
            hob = sch.bufs("p2_ho", 2)
            cacc, caccb = self.sb(es, "p2_cacc", [128, 128], F32), sch.buf("p2_cacc")
            mixT = [self.sb(es, f"p2_mixT{i}", [128, 8, 128], BF16) for i in range(2)]
            mixTb = sch.bufs("p2_mixT", 2)
            E = [self.sb(es, f"p2_E{i}", [128, 8, 128], BF16) for i in range(2)]
            Eb = sch.bufs("p2_E", 2)
            Em = [self.sb(es, f"p2_Em{i}", [128, 6, 128], BF16) for i in range(2)]
            Emb = sch.bufs("p2_Em", 2)
            yna, ynab = self.sb(es, "p2_yna", [128, 8, 64], BF16), sch.buf("p2_yna")
            rden, rdenb = self.sb(es, "p2_rden", [128, 4, 1], F32), sch.buf("p2_rden")
            tmp, tmpb = self.sb(es, "p2_tmp", [128, D], F32), sch.buf("p2_tmp")
            pS = [self.ps(es, f"p2_pS{i}", [128, 8, 128]) for i in range(2)]
            pSb = sch.bufs("p2_pS", 2)
            po = [self.ps(es, f"p2_po{i}", [128, 4, 65]) for i in range(2)]
            pob = sch.bufs("p2_po", 2)
            pT = self.ps(es, "p2_pT", [128, 4, 128], BF16)
            pTb = sch.buf("p2_pT")
            py = pS[0][:].rearrange("p (a x) b -> p a (x b)", a=2)
            pyb = pSb[0]

            ntiles = NT if last else NT + 2
            hcnt = 0
            for i in range(ntiles):
                isctx = i >= NT
                s = i % 2
                tok0 = i * 128
                w = 1 if isctx else 0
                if isctx:
                    n = 0
                    tcls = None
                else:
                    if i == 0:
                        e0, n, tcls = 0, 6, 5
                    elif i == 1:
                        e0, n, tcls = 1, 5, 11
                    elif i == NT - 2:
                        e0, n, tcls = NT - 2, 5, 16
                    elif i == NT - 1:
                        e0, n, tcls = NT - 2, 6, 21
                    else:
                        e0, n, tcls = i, 5, 0
                sch.dma("sp", qt[s][:], S["e_qT"].rearrange("c p t -> p c t")[:, :, tok0:tok0 + 128], qtb[s], reads=[DB["e_qT"]], writes=[qtb[s]])
                sch.dma("sp", gbw[s][:], S["e_gbT"].rearrange("c p t -> p c t")[:, :, tok0:tok0 + 128], gbwb[s], reads=[DB["e_gbT"]], writes=[gbwb[s]])
                if isctx:
                    c0 = (i - NT) * 128
                    sch.dma("sp", zw[s][:], S["e_zctx"].rearrange("c p t -> p c t")[:, :, c0:c0 + 130], zwb[s], reads=[DB["e_zctx"]], writes=[zwb[s]])
                else:
                    sch.dma("sp", zw[s][:], S["e_zext"].rearrange("c p t -> p c t")[:, :, tok0:tok0 + 130], zwb[s], reads=[DB["e_zext"]], writes=[zwb[s]])
                    sch.dma("sp", kw[s][:, :, 0:n * 128], S["e_kext"].rearrange("c p t -> p c t")[:, :, e0 * 128:(e0 + n) * 128], kwb[s],
                            reads=[DB["e_kext"]], writes=[kwb[s]])
                    sch.dma("sp", vw[s][:, 0:n * 520], S["e_vext"][:, e0 * 520:(e0 + n) * 520], vwb[s], reads=[DB["e_vext"]], writes=[vwb[s]])
                sch.dma("sp", ht[s][:], h_cur[tok0:tok0 + 128, :], htb[s], reads=[DB[h_name]], writes=[htb[s]])
                mt, mtb = mixT[s], mixTb[s]
                for c in range(4):
                    sch.op("dve", lambda e, c=c: e.tensor_scalar(out=cacc[:], in0=zw[s][:, c, 1:129], scalar1=cw[:, c, 1:2], scalar2=None, op0=ALU.mult),
                           reads=[zwb[s], cwb], writes=[caccb])
                    sch.op("dve", lambda e, c=c: e.scalar_tensor_tensor(out=cacc[:], in0=zw[s][:, c, 0:128], scalar=cw[:, c, 0:1], in1=cacc[:],
                                                                        op0=ALU.mult, op1=ALU.add), reads=[zwb[s], cwb, caccb], writes=[caccb])
                    sch.op("dve", lambda e, c=c: e.scalar_tensor_tensor(out=cacc[:], in0=zw[s][:, c, 2:130], scalar=cw[:, c, 2:3], in1=cacc[:],
                                                                        op0=ALU.mult, op1=ALU.add), reads=[zwb[s], cwb, caccb], writes=[caccb])
                    sch.op("dve", lambda e, c=c: e.tensor_tensor(out=mt[:, c, :], in0=cacc[:], in1=gbw[s][:, c, :], op=ALU.mult),
                           reads=[caccb, gbwb[s]], writes=[mtb], partial=True)
                hsmap = {}

                def emit_sc(h):
                    nonlocal hcnt
                    cr, hh = h // 2, h % 2
                    p0, p1 = hh * 64, hh * 64 + 64
                    hs = hcnt % 2
                    hcnt += 1
                    hsmap[h] = hs
                    nchunks = n + 2
                    for ci in range(n):
                        sch.op("pe", lambda e, ci=ci: e.matmul(pS[hs][:, ci, :], lhsT=kw[s][p0:p1, cr, ci * 128:(ci + 1) * 128], rhs=qt[s][p0:p1, cr, :],
                                                               start=True, stop=True),
                               reads=[kwb[s], qtb[s]], writes=[pSb[hs]], signal=False, partial=True)
                    for j in range(2):
                        sch.op("pe", lambda e, j=j: e.matmul(pS[hs][:, n + j, :], lhsT=kctx[p0:p1, cr, j * 128:(j + 1) * 128], rhs=qt[s][p0:p1, cr, :],
                                                             start=True, stop=True),
                               reads=[kctxb, qtb[s]], writes=[pSb[hs]], signal=(j == 1), partial=True)
                    sch.op("act", lambda e: e.activation(out=E[hs][:, 0:nchunks, :], in_=pS[hs][:, 0:nchunks, :], func=AF.Exp),
                           reads=[pSb[hs]], writes=[Eb[hs]])
                    if n > 0:
                        sch.op("dve", lambda e: e.tensor_tensor(out=Em[hs][:, 0:n, :], in0=E[hs][:, 0:n, :], in1=EB[:, tcls:tcls + n, h, :], op=ALU.mult),
                               reads=[Eb[hs], EBb], writes=[Emb[hs]])

                def emit_pv(h):
                    hs = hsmap.pop(h)
                    pg = po[h // 4]
                    pgb = pob[h // 4]
                    hq = h % 4
                    for ci in range(n):
                        sch.op("pe", lambda e, ci=ci: e.matmul(pg[:, hq, :], lhsT=Em[hs][:, ci, :], rhs=vw[s][:, ci * 520 + h * 65:ci * 520 + (h + 1) * 65],
                                                               start=(ci == 0), stop=False),
                               reads=[Emb[hs], vwb[s]], writes=[pgb], signal=False, partial=True)
                    for j in range(2):
                        sch.op("pe", lambda e, j=j: e.matmul(pg[:, hq, :], lhsT=E[hs][:, n + j, :], rhs=vctx[:, j * 520 + h * 65:j * 520 + (h + 1) * 65],
                                                             start=(n == 0 and j == 0), stop=(j == 1)),
                               reads=[Eb[hs], vctxb], writes=[pgb], signal=(j == 1), partial=True)
                    if hq == 3:
                        g4 = h // 4
                        sch.op("dve", lambda e: e.reciprocal(out=rden[:], in_=pg[:, :, 64:65]), reads=[pgb], writes=[rdenb])
                        sch.op("dve", lambda e: e.tensor_tensor(out=yna[:, g4 * 4:(g4 + 1) * 4, :], in0=pg[:, :, 0:64],
                                                                in1=rden[:].to_broadcast([128, 4, 64]), op=ALU.mult),
                               reads=[pgb, rdenb], writes=[ynab], partial=True)

                emit_sc(0)
                for h in range(8):
                    if h + 1 < 8:
                        emit_sc(h + 1)
                    emit_pv(h)
                ynaf = yna[:].rearrange("p h d -> p (h d)")
                for c in range(4):
                    sch.op("pe", lambda e, c=c: e.transpose(out=pT[:, c, :], in_=ynaf[:, c * 128:(c + 1) * 128], identity=ident[:]),
                           reads=[ynab, identb], writes=[pTb], signal=(c == 3), partial=True)
                sch.op("act", lambda e: e.copy(out=mt[:, 4:8, :], in_=pT[:]), reads=[pTb], writes=[mtb], partial=True)
                for hf in range(2):
                    for kc in range(8):
                        sch.op("pe", lambda e, kc=kc, hf=hf: e.matmul(py[:, hf, :], lhsT=mt[:, kc, :], rhs=Wo[:, kc, hf * 512:(hf + 1) * 512],
                                                                      start=(kc == 0), stop=(kc == 7)),
                               reads=[mtb, Wob], writes=[pyb], signal=(kc == 7 and hf == 1), partial=True)
                g1, g1b = mod[(w, 2)]
                sch.op("dve", lambda e: e.tensor_tensor(out=tmp[:], in0=pS[0][:].rearrange("p a b -> p (a b)"), in1=g1[:], op=ALU.mult),
                       reads=[pyb, g1b], writes=[tmpb])
                sch.op("dve", lambda e: e.tensor_tensor(out=ho[s][:], in0=tmp[:], in1=ht[s][:], op=ALU.add),
                       reads=[tmpb, htb[s]], writes=[hob[s]])
                sch.dma("pool", h_out[tok0:tok0 + 128, :], ho[s][:], hob[s], reads=[hob[s]], writes=[DB[h_out_name]], partial=True)
            sch.barrier()

    def phase_p2_odd(self, l, h_cur, h_name, h_out, h_out_name, last):
        nc, sch, I, S, DB = self.nc, self.sch, self.I, self.S, self.DB
        li = l // 2
        lam_init = 0.8 - 0.6 * math.exp(-0.3 * l)
        with ExitStack() as es:
            stage, stageb = self.make_stage(es, "q2_stg", 2, 1024)
            Wo, Wob = self.load_weight_bf16(es, "q2_Wo", I["od_w_out"][li], 8, D, stage, stageb, cast_eng=["pool", "dve"])
            mod = self.load_mod(es, l, [2])
            self.make_eps(es, "q2_eps")
            identf, identfb = self.sb(es, "q2_idf", [128, 128], F32), sch.buf("q2_idf")
            ident, identb = self.sb(es, "q2_id", [128, 128], BF16), sch.buf("q2_id")
            sch.dma("sp", identf[:], I["ident"], identfb, writes=[identfb])
            sch.op("dve", lambda e: e.tensor_copy(out=ident[:], in_=identf[:]), reads=[identfb], writes=[identb])
            smf, smfb = self.sb(es, "q2_smf", [128, 2, 128], F32), sch.buf("q2_smf")
            sm, smb = self.sb(es, "q2_sm", [128, 2, 128], BF16), sch.buf("q2_sm")
            sch.dma("sp", smf[:], I["swamask"], smfb, writes=[smfb])
            sch.op("dve", lambda e: e.tensor_copy(out=sm[:], in_=smf[:]), reads=[smfb], writes=[smb])
            es8, es8b = self.sb(es, "q2_es8", [128, 8, 1], F32), sch.buf("q2_es8")
            sch.dma("sp", es8[:].rearrange("p h o -> p (h o)"), I["od_sink"][li].partition_broadcast(128), es8b, writes=[es8b])
            sch.op("act", lambda e: e.activation(out=es8[:], in_=es8[:], func=AF.Exp), reads=[es8b], writes=[es8b])
            lamt, lamtb = self.sb(es, "q2_lamt", [128, 2, 2, 64], F32), sch.buf("q2_lamt")
            sch.dma("sp", lamt[:].rearrange("p a b d -> p (a b d)"), I["od_lam"][li].rearrange("a d -> (a d)").partition_broadcast(128), lamtb, writes=[lamtb])
            lp, lpb = self.sb(es, "q2_lp", [128, 2, 64], F32), sch.buf("q2_lp")
            sch.op("dve", lambda e: e.tensor_tensor(out=lp[:], in0=lamt[:, :, 0, :], in1=lamt[:, :, 1, :], op=ALU.mult), reads=[lamtb], writes=[lpb])
            ls, lsb = self.sb(es, "q2_ls", [128, 2], F32), sch.buf("q2_ls")
            sch.op("dve", lambda e: e.tensor_reduce(out=ls[:], in_=lp[:], axis=AX.X, op=ALU.add), reads=[lpb], writes=[lsb])
            sch.op("act", lambda e: e.activation(out=ls[:], in_=ls[:], func=AF.Exp), reads=[lsb], writes=[lsb])
            nlam, nlamb = self.sb(es, "q2_nlam", [128, 1], F32), sch.buf("q2_nlam")
            sch.op("dve", lambda e: e.tensor_tensor(out=nlam[:], in0=ls[:, 1:2], in1=ls[:, 0:1], op=ALU.subtract), reads=[lsb], writes=[nlamb])
            sch.op("dve", lambda e: e.tensor_scalar(out=nlam[:], in0=nlam[:], scalar1=-lam_init, scalar2=None, op0=ALU.add), reads=[nlamb], writes=[nlamb])
            kctx, kctxb = self.sb(es, "q2_kctx", [128, 5, NCTX], BF16), sch.buf("q2_kctx")
            sch.dma("sp", kctx[:], S["o_kctx"].rearrange("c p t -> p c t"), kctxb, reads=[DB["o_kctx"]], writes=[kctxb])
            ckc = [self.sb(es, f"q2_ckc{g}", [128, NCTX], BF16) for g in range(2)]
            ckcb = sch.bufs("q2_ckc", 2)
            for g in range(2):
                for hh in range(2):
                    sch.dma("sp", ckc[g][hh * 64:(hh + 1) * 64, :], S["o_kctx"][0, g * 64:(g + 1) * 64, :], ckcb[g],
                            reads=[DB["o_kctx"]], writes=[ckcb[g]], partial=(hh == 1))
            vctx, vctxb = self.sb(es, "q2_vctx", [128, 5, 260], BF16), sch.buf("q2_vctx")
            sch.dma("sp", vctx[:], S["o_vctx"].rearrange("c p t -> p c t"), vctxb, reads=[DB["o_vctx"]], writes=[vctxb])
            qt = [self.sb(es, f"q2_q{i}", [128, 8, 512], BF16) for i in range(2)]
            qtb = sch.bufs("q2_q", 2)
            kd = [[self.sb(es, f"q2_kd{i}_{g}", [128, 384], BF16) for g in range(2)] for i in range(2)]
            kdb = [sch.bufs(f"q2_kd{i}_", 2) for i in range(2)]
            vw = [self.sb(es, f"q2_vw{i}", [128, 3, 130], BF16) for i in range(2)]
            vwb = sch.bufs("q2_vw", 2)
            KH = [self.sb(es, f"q2_KH{i}", [128, NLAT], BF16) for i in range(2)]
            KHb = sch.bufs("q2_KH", 2)
            VH = [self.sb(es, f"q2_VH{i}", [128, NT * 130], BF16) for i in range(2)]
            VHb = sch.bufs("q2_VH", 2)
            ht = [self.sb(es, f"q2_h{i}", [128, D], F32) for i in range(2)]
            htb = sch.bufs("q2_h", 2)
            ho = [self.sb(es, f"q2_ho{i}", [128, D], F32) for i in range(2)]
            hob = sch.bufs("q2_ho", 2)
            tmp, tmpb = self.sb(es, "q2_tmp", [128, D], F32), sch.buf("q2_tmp")
            mixT = [self.sb(es, f"q2_mixT{i}", [128, 8, 128], BF16) for i in range(4)]
            mixTb = sch.bufs("q2_mixT", 4)
            Esw = [self.sb(es, f"q2_Esw{i}", [128, 5, 128], BF16) for i in range(2)]
            Eswb = sch.bufs("q2_Esw", 2)
            Ed = [self.sb(es, f"q2_Ed{i}", [128, 512], BF16) for i in range(4)]
            Edb = sch.bufs("q2_Ed", 4)
            yc, ycb = self.sb(es, "q2_yc", [128, 8, 64], BF16), sch.buf("q2_yc")
            rden, rdenb = self.sb(es, "q2_rden", [128, 4, 1], F32), sch.buf("q2_rden")
            onesf, onesfb = self.sb(es, "q2_onesf", [128, 128], F32), sch.buf("q2_onesf")
            sch.op("pool", lambda e: e.memset(onesf[:], 1.0), writes=[onesfb])
            accD, accDb = self.sb(es, "q2_accD", [128, 512], F32), sch.buf("q2_accD")
            accP, accPb = self.sb(es, "q2_accP", [128, 512], F32), sch.buf("q2_accP")
            rdb, rdbb = self.sb(es, "q2_rdb", [128, 512], F32), sch.buf("q2_rdb")
            y0T, y0Tb = self.sb(es, "q2_y0T", [128, 512], F32), sch.buf("q2_y0T")
            yT, yTb = self.sb(es, "q2_yT", [128, 512], F32), sch.buf("q2_yT")
            sqT, sqTb = self.sb(es, "q2_sqT", [128, 512], F32), sch.buf("q2_sqT")
            rsT, rsTb = self.sb(es, "q2_rsT", [128, 512], F32), sch.buf("q2_rsT")
            slgc, slgcb = self.sb(es, "q2_slgc", [128, 1], F32), sch.buf("q2_slgc")
            sch.dma("sp", slgc[:], I["od_subln"][li].rearrange("(p o) -> p o", o=1), slgcb, writes=[slgcb])
            sch.op("dve", lambda e: e.tensor_scalar(out=slgc[:], in0=slgc[:], scalar1=1.0 - lam_init, scalar2=None, op0=ALU.mult), reads=[slgcb], writes=[slgcb])
            pSall = self.ps(es, "q2_pS", [128, 3, 512])
            pSb = sch.bufs("q2_pS", 3)
            po = [self.ps(es, f"q2_po{i}", [128, 4, 65]) for i in range(2)]
            pob = sch.bufs("q2_po", 2)
            poT = [self.ps(es, f"q2_poT{i}", [128, 512]) for i in range(2)]
            poTb = sch.bufs("q2_poT", 2)
            pT = self.ps(es, "q2_pT", [128, 4, 128], BF16)
            pTb = sch.buf("q2_pT")

            nst = NST if last else NST + 1
            hcnt = 0
            dcnt = 0
            khc = 0
            tcnt = 0
            gcnt = 0
            for st in range(nst):
                isctx = st == NST
                nt = 256 if isctx else 512
                nsub = nt // 128
                tok0 = st * 512
                w = 1 if isctx else 0
                sa = st % 2
                sch.dma("sp", qt[sa][:, :, 0:nt], S["o_qT"].rearrange("c p t -> p c t")[:, :, tok0:tok0 + nt], qtb[sa], reads=[DB["o_qT"]], writes=[qtb[sa]])
                for sub in range(nsub):
                    i = st * 4 + sub
                    s = tcnt % 2
                    tcnt += 1
                    mt, mtb = mixT[sub], mixTb[sub]
                    if isctx:
                        slots = []
                        jl, jh = 0, 0
                    else:
                        lo_t, hi_t = max(i - 1, 0), min(i + 1, NT - 1)
                        slots = list(range(lo_t - (i - 1), hi_t - (i - 1) + 1))
                        jl, jh = slots[0], slots[-1] + 1
                        for g in range(2):
                            for hh in range(2):
                                sch.dma("sp", kd[s][g][hh * 64:(hh + 1) * 64, jl * 128:jh * 128], S["o_blobK"][g * 64:(g + 1) * 64, lo_t * 128:(hi_t + 1) * 128],
                                        kdb[s][g], reads=[DB["o_blobK"]], writes=[kdb[s][g]], partial=(hh == 1))
                        sch.dma("sp", vw[s][:, jl:jh, :], S["o_blobV"][512:640, lo_t * 130:(hi_t + 1) * 130].rearrange("p (j c) -> p j c", c=130),
                                vwb[s], reads=[DB["o_blobV"]], writes=[vwb[s]])
                    for h in range(8):
                        g = h // 4
                        cr, hh = h // 2, h % 2
                        p0, p1 = hh * 64, hh * 64 + 64
                        hs = hcnt % 2
                        hcnt += 1
                        qv = qt[sa][p0:p1, cr, sub * 128:(sub + 1) * 128]
                        for j in slots:
                            sch.op("pe", lambda e, j=j: e.matmul(pSall[:, 0, j * 128:(j + 1) * 128], lhsT=kd[s][g][p0:p1, j * 128:(j + 1) * 128], rhs=qv,
                                                                 start=True, stop=True),
                                   reads=[kdb[s][g], qtb[sa]], writes=[pSb[0]], signal=(j == slots[-1]), partial=(j != slots[0]))
                        for jj in range(2):
                            sch.op("pe", lambda e, jj=jj: e.matmul(pSall[:, 1, jj * 128:(jj + 1) * 128], lhsT=ckc[g][p0:p1, jj * 128:(jj + 1) * 128], rhs=qv,
                                                                   start=True, stop=True),
                                   reads=[ckcb[g], qtb[sa]], writes=[pSb[1]], signal=(jj == 1), partial=(jj == 1))
                        E, Eb_ = Esw[hs], Eswb[hs]
                        if slots:
                            sch.op("act", lambda e: e.activation(out=E[:, jl:jh, :], in_=pSall[:, 0, jl * 128:jh * 128].rearrange("p (j c) -> p j c", c=128), func=AF.Exp),
                                   reads=[pSb[0]], writes=[Eb_])
                        sch.op("act", lambda e: e.activation(out=E[:, 3:5, :], in_=pSall[:, 1, 0:256].rearrange("p (j c) -> p j c", c=128), func=AF.Exp),
                               reads=[pSb[1]], writes=[Eb_], partial=bool(slots))
                        if 0 in slots:
                            sch.op("dve", lambda e: e.tensor_tensor(out=E[:, 0, :], in0=E[:, 0, :], in1=sm[:, 0, :], op=ALU.mult), reads=[Eb_, smb], writes=[Eb_])
                        if 2 in slots:
                            sch.op("dve", lambda e: e.tensor_tensor(out=E[:, 2, :], in0=E[:, 2, :], in1=sm[:, 1, :], op=ALU.mult), reads=[Eb_, smb], writes=[Eb_])
                        pg, pgb = po[h // 4], pob[h // 4]
                        hq = h % 4
                        for j in slots:
                            sch.op("pe", lambda e, j=j: e.matmul(pg[:, hq, :], lhsT=E[:, j, :], rhs=vw[s][:, j, g * 65:(g + 1) * 65], start=(j == slots[0]), stop=False),
                                   reads=[Eb_, vwb[s]], writes=[pgb], signal=False, partial=(not (hq == 0 and j == slots[0])))
                        for jj in range(2):
                            sch.op("pe", lambda e, jj=jj: e.matmul(pg[:, hq, :], lhsT=E[:, 3 + jj, :], rhs=vctx[:, 4, jj * 130 + g * 65:jj * 130 + (g + 1) * 65],
                                                                   start=(not slots and jj == 0), stop=(jj == 1)),
                                   reads=[Eb_, vctxb], writes=[pgb], signal=(jj == 1), partial=(not (hq == 0 and jj == 0 and not slots)))
                        if hq == 3:
                            g4 = h // 4
                            sch.op("dve", lambda e: e.tensor_tensor(out=rden[:], in0=pg[:, :, 64:65], in1=es8[:, g4 * 4:(g4 + 1) * 4, :], op=ALU.add),
                                   reads=[pgb, es8b], writes=[rdenb])
                            sch.op("dve", lambda e: e.reciprocal(out=rden[:], in_=rden[:]), reads=[rdenb], writes=[rdenb])
                            sch.op("dve", lambda e: e.tensor_tensor(out=yc[:, g4 * 4:(g4 + 1) * 4, :], in0=pg[:, :, 0:64],
                                                                    in1=rden[:].to_broadcast([128, 4, 64]), op=ALU.mult),
                                   reads=[pgb, rdenb], writes=[ycb], partial=(g4 == 1))
                    ycf = yc[:].rearrange("p h d -> p (h d)")
                    for c in range(4):
                        sch.op("pe", lambda e, c=c: e.transpose(out=pT[:, c, :], in_=ycf[:, c * 128:(c + 1) * 128], identity=ident[:]),
                               reads=[ycb, identb], writes=[pTb], signal=(c == 3), partial=(c > 0))
                    sch.op("act", lambda e: e.copy(out=mt[:, 0:4, :], in_=pT[:]), reads=[pTb], writes=[mtb])
                npair = nsub // 2
                for h in range(4):
                    kb = khc % 2
                    khc += 1
                    if not isctx:
                        sch.dma("sp", KH[kb][:], S["o_blobK"][(1 + h) * 128:(2 + h) * 128, :], KHb[kb], reads=[DB["o_blobK"]], writes=[KHb[kb]])
                        sch.dma("sp", VH[kb][:], S["o_blobV"][h * 128:(h + 1) * 128, :], VHb[kb], reads=[DB["o_blobV"]], writes=[VHb[kb]])
                        chunks = [("l", kt) for kt in range(NT)] + [("c", 0), ("c", 1)]
                    else:
                        chunks = [("c", 0), ("c", 1)]
                    for m in range(2):
                        p0, p1 = m * 64, m * 64 + 64
                        nch = len(chunks)
                        SKEW = 2
                        slot = {}
                        gp = gcnt % 2
                        gcnt += 1
                        used = {"dve": False, "pool": False}

                        def emit_qk(ci):
                            nonlocal dcnt
                            kind, kt = chunks[ci]
                            k = dcnt % 3
                            k3 = dcnt % 4
                            dcnt += 1
                            slot[ci] = k3
                            if kind == "l":
                                lh, lhb = KH[kb][p0:p1, kt * 128:(kt + 1) * 128], KHb[kb]
                            else:
                                lh, lhb = kctx[p0:p1, 1 + h, kt * 128:(kt + 1) * 128], kctxb
                            sch.op("pe", lambda e: e.matmul(pSall[:, k, 0:nt], lhsT=lh, rhs=qt[sa][p0:p1, 4 + h, 0:nt], start=True, stop=True),
                                   reads=[lhb, qtb[sa]], writes=[pSb[k]])
                            sch.op("act", lambda e: e.activation(out=Ed[k3][:, 0:nt], in_=pSall[:, k, 0:nt], func=AF.Exp),
                                   reads=[pSb[k]], writes=[Edb[k3]])

                        def emit_pv(ci):
                            kind, kt = chunks[ci]
                            k3 = slot.pop(ci)
                            if kind == "l":
                                rv, rvb = VH[kb][:, kt * 130:kt * 130 + 128], VHb[kb]
                            else:
                                rv, rvb = vctx[:, h, kt * 130:kt * 130 + 128], vctxb
                            sch.op("pe", lambda e: e.matmul(poT[gp][:, 0:nt], lhsT=rv, rhs=Ed[k3][:, 0:nt], start=(ci == 0), stop=(ci == nch - 1)),
                                   reads=[Edb[k3], rvb], writes=[poTb[gp]], signal=(ci == nch - 1), partial=(ci > 0))
                            en = "pool" if ci % 3 == 2 else "dve"
                            acc, accb_ = (accP, accPb) if en == "pool" else (accD, accDb)
                            if not used[en]:
                                used[en] = True
                                sch.op(en, lambda e: e.tensor_copy(out=acc[:, 0:nt], in_=Ed[k3][:, 0:nt]), reads=[Edb[k3]], writes=[accb_])
                            else:
                                sch.op(en, lambda e: e.tensor_tensor(out=acc[:, 0:nt], in0=acc[:, 0:nt], in1=Ed[k3][:, 0:nt], op=ALU.add),
                                       reads=[Edb[k3], accb_], writes=[accb_])

                        for ci in range(nch + SKEW):
                            if ci < nch:
                                emit_qk(ci)
                            if ci >= SKEW:
                                emit_pv(ci - SKEW)
                        k = dcnt % 3
                        dcnt += 1
                        sch.op("pe", lambda e: e.matmul(pSall[:, k, 0:nt], lhsT=onesf[:], rhs=accD[:, 0:nt], start=True, stop=(not used["pool"])),
                               reads=[onesfb, accDb], writes=[pSb[k]], signal=(not used["pool"]))
                        if used["pool"]:
                            sch.op("pe", lambda e: e.matmul(pSall[:, k, 0:nt], lhsT=onesf[:], rhs=accP[:, 0:nt], start=False, stop=True),
                                   reads=[onesfb, accPb], writes=[pSb[k]], partial=True)
                        sch.op("dve", lambda e: e.reciprocal(out=rdb[:, 0:nt], in_=pSall[:, k, 0:nt]), reads=[pSb[k]], writes=[rdbb])
                        if m == 0:
                            sch.op("dve", lambda e: e.tensor_tensor(out=y0T[:, 0:nt], in0=poT[gp][:, 0:nt], in1=rdb[:, 0:nt], op=ALU.mult),
                                   reads=[poTb[gp], rdbb], writes=[y0Tb])
                        else:
                            sch.op("dve", lambda e: e.tensor_tensor(out=yT[:, 0:nt], in0=poT[gp][:, 0:nt], in1=rdb[:, 0:nt], op=ALU.mult),
                                   reads=[poTb[gp], rdbb], writes=[yTb])
                            sch.op("dve", lambda e: e.scalar_tensor_tensor(out=yT[:, 0:nt], in0=yT[:, 0:nt], scalar=nlam[:, 0:1], in1=y0T[:, 0:nt], op0=ALU.mult, op1=ALU.add),
                                   reads=[yTb, nlamb, y0Tb], writes=[yTb])
                            sch.op("act", lambda e: e.activation(out=sqT[:, 0:nt], in_=yT[:, 0:nt], func=AF.Square), reads=[yTb], writes=[sqTb])
                            k = dcnt % 3
                            dcnt += 1
                            sch.op("pe", lambda e: e.matmul(pSall[:, k, 0:nt], lhsT=onesf[:], rhs=sqT[:, 0:nt], start=True, stop=True),
                                   reads=[onesfb, sqTb], writes=[pSb[k]])
                            self.rsqrt(rsT[:, 0:nt], rsTb, pSall[:, k, 0:nt], pSb[k], 1.0 / 128)
                            sch.op("dve", lambda e: e.tensor_tensor(out=yT[:, 0:nt], in0=yT[:, 0:nt], in1=rsT[:, 0:nt], op=ALU.mult),
                                   reads=[yTb, rsTb], writes=[yTb])
                            for sub in range(nsub):
                                sch.op("dve", lambda e, sub=sub: e.tensor_scalar(out=mixT[sub][:, 4 + h, :], in0=yT[:, sub * 128:(sub + 1) * 128], scalar1=slgc[:, 0:1], scalar2=None,
                                                                                op0=ALU.mult),
                                       reads=[yTb, slgcb], writes=[mixTb[sub]], partial=True)
                py = pSall
                for sub in range(nsub):
                    r0 = tok0 + sub * 128
                    s = tcnt % 2
                    tcnt += 1
                    mt, mtb = mixT[sub], mixTb[sub]
                    sch.dma("sp", ht[s][:], h_cur[r0:r0 + 128, :], htb[s], reads=[DB[h_name]], writes=[htb[s]])
                    for hf in range(2):
                        for kc in range(8):
                            sch.op("pe", lambda e, kc=kc, hf=hf, mt=mt: e.matmul(py[:, hf, :], lhsT=mt[:, kc, :], rhs=Wo[:, kc, hf * 512:(hf + 1) * 512],
                                                                                 start=(kc == 0), stop=(kc == 7)),
                                   reads=[mtb, Wob], writes=[pSb[hf]], signal=(kc == 7))
                    g1, g1b = mod[(w, 2)]
                    sch.op("dve", lambda e: e.tensor_tensor(out=tmp[:], in0=py[:, 0:2, :].rearrange("p a b -> p (a b)"), in1=g1[:], op=ALU.mult),
                           reads=[pSb[0], pSb[1], g1b], writes=[tmpb])
                    sch.op("dve", lambda e, s=s: e.tensor_tensor(out=ho[s][:], in0=tmp[:], in1=ht[s][:], op=ALU.add),
                           reads=[tmpb, htb[s]], writes=[hob[s]])
                    sch.dma("pool", h_out[r0:r0 + 128, :], ho[s][:], hob[s], reads=[hob[s]], writes=[DB[h_out_name]], partial=True)
            sch.barrier()

    def phase_p3a(self, l, h_mid, h_name, last):
        nc, sch, I, S, DB = self.nc, self.sch, self.I, self.S, self.DB
        li = l // 2
        moe = (l % 2 == 1)
        with ExitStack() as es:
            mod = self.load_mod(es, l, [3, 4])
            self.make_eps(es, "p3_eps")
            identf, identfb = self.sb(es, "p3_idf", [128, 128], F32), sch.buf("p3_idf")
            sch.dma("sp", identf[:], I["ident"], identfb, writes=[identfb])
            if moe:
                rt, rtb = self.sb(es, "p3_rt", [128, NEXP, D], F32), sch.buf("p3_rt")
                sch.dma("sp", rt[:].rearrange("p e d -> p (e d)"), I["moe_routerT"][li].rearrange("e d -> (e d)").partition_broadcast(128), rtb, writes=[rtb])
                prod, prodb = self.sb(es, "p3_prod", [128, NEXP, D], F32), sch.buf("p3_prod")
            ht = [self.sb(es, f"p3_h{i}", [128, D], F32) for i in range(2)]
            htb = sch.bufs("p3_h", 2)
            junk, junkb = self.sb(es, "p3_junk", [128, D], F32), sch.buf("p3_junk")
            tmp, tmpb = self.sb(es, "p3_tmp", [128, D], F32), sch.buf("p3_tmp")
            ss, ssb = self.sb(es, "p3_ss", [128, 1], F32), sch.buf("p3_ss")
            rstd, rstdb = self.sb(es, "p3_rstd", [128, 1], F32), sch.buf("p3_rstd")
            a2 = [self.sb(es, f"p3_a{i}", [128, D], F32) for i in range(2)]
            a2b = sch.bufs("p3_a", 2)
            aTh = [self.sb(es, f"p3_aTh{i}", [128, 8, 128], BF16) for i in range(2)]
            aThb = sch.bufs("p3_aTh", 2)
            pT = [self.ps(es, f"p3_pT{i}", [128, 8, 128]) for i in range(2)]
            pTb = sch.bufs("p3_pT", 2)
            if moe:
                lg, lgb = self.sb(es, "p3_lg", [128, NEXP], F32), sch.buf("p3_lg")
                l2, l2b = self.sb(es, "p3_l2", [128, NEXP], F32), sch.buf("p3_l2")
                eq1, eq1b = self.sb(es, "p3_eq1", [128, NEXP], F32), sch.buf("p3_eq1")
                eq2, eq2b = self.sb(es, "p3_eq2", [128, NEXP], F32), sch.buf("p3_eq2")
                m1, m1b = self.sb(es, "p3_m1", [128, 1], F32), sch.buf("p3_m1")
                m2, m2b = self.sb(es, "p3_m2", [128, 1], F32), sch.buf("p3_m2")
                w2, w2b = self.sb(es, "p3_w2", [128, 1], F32), sch.buf("p3_w2")
                w1, w1b = self.sb(es, "p3_w1", [128, 1], F32), sch.buf("p3_w1")
            ntiles = NT if last else NT + 2
            for i in range(ntiles):
                s = i % 2
                w = 1 if i >= NT else 0
                tok0 = i * 128
                sch.dma("sp", ht[s][:], h_mid[tok0:tok0 + 128, :], htb[s], reads=[DB[h_name]], writes=[htb[s]])
                self.rms_mod_tile(ht[s], htb[s], ss, ssb, junk, junkb, rstd, rstdb, tmp, tmpb,
                                  mod[(w, 4)], mod[(w, 3)], a2[s][:], a2b[s])
                for kc in range(8):
                    sch.op("pe", lambda e, kc=kc: e.transpose(out=pT[s][:, kc, :], in_=a2[s][:, kc * 128:(kc + 1) * 128], identity=identf[:]),
                           reads=[a2b[s], identfb], writes=[pTb[s]], signal=(kc == 7), partial=True)
                sch.op("act", lambda e: e.copy(out=aTh[s][:], in_=pT[s][:]), reads=[pTb[s]], writes=[aThb[s]])
                sch.dma("pool", S["a2T"].rearrange("c p t -> p c t")[:, :, tok0:tok0 + 128], aTh[s][:], aThb[s], reads=[aThb[s]], writes=[DB["a2T"]], partial=True)
                if moe:
                    sch.op("dve", lambda e: e.tensor_tensor(out=prod[:], in0=rt[:], in1=a2[s][:].rearrange("p (o d) -> p o d", o=1).to_broadcast([128, NEXP, D]), op=ALU.mult),
                           reads=[rtb, a2b[s]], writes=[prodb])
                    sch.op("dve", lambda e: e.tensor_reduce(out=lg[:], in_=prod[:], axis=AX.X, op=ALU.add), reads=[prodb], writes=[lgb])
                    sch.op("dve", lambda e: e.tensor_reduce(out=m1[:], in_=lg[:], axis=AX.X, op=ALU.max), reads=[lgb], writes=[m1b])
                    sch.op("dve", lambda e: e.tensor_scalar(out=eq1[:], in0=lg[:], scalar1=m1[:, 0:1], scalar2=None, op0=ALU.is_equal),
                           reads=[lgb, m1b], writes=[eq1b])
                    sch.op("dve", lambda e: e.scalar_tensor_tensor(out=l2[:], in0=eq1[:], scalar=-1e30, in1=lg[:], op0=ALU.mult, op1=ALU.add),
                           reads=[eq1b, lgb], writes=[l2b])
                    sch.op("dve", lambda e: e.tensor_reduce(out=m2[:], in_=l2[:], axis=AX.X, op=ALU.max), reads=[l2b], writes=[m2b])
                    sch.op("dve", lambda e: e.tensor_scalar(out=eq2[:], in0=l2[:], scalar1=m2[:, 0:1], scalar2=None, op0=ALU.is_equal),
                           reads=[l2b, m2b], writes=[eq2b])
                    sch.op("dve", lambda e: e.tensor_tensor(out=w2[:], in0=m2[:], in1=m1[:], op=ALU.subtract), reads=[m1b, m2b], writes=[w2b])
                    sch.op("act", lambda e: e.activation(out=w2[:], in_=w2[:], func=AF.Sigmoid), reads=[w2b], writes=[w2b])
                    sch.op("dve", lambda e: e.tensor_scalar(out=w1[:], in0=w2[:], scalar1=-1.0, scalar2=1.0, op0=ALU.mult, op1=ALU.add),
                           reads=[w2b], writes=[w1b])
                    sch.op("dve", lambda e: e.tensor_scalar(out=eq1[:], in0=eq1[:], scalar1=w1[:, 0:1], scalar2=None, op0=ALU.mult),
                           reads=[eq1b, w1b], writes=[eq1b])
                    sch.op("dve", lambda e, i=i: e.scalar_tensor_tensor(out=self.gates_sb[:, i, :], in0=eq2[:], scalar=w2[:, 0:1], in1=eq1[:], op0=ALU.mult, op1=ALU.add),
                           reads=[eq2b, w2b, eq1b], writes=[self.gates_b], partial=True)
            sch.barrier()

    def phase_p3b(self, l, h_mid, h_name, h_nxt, h_nxt_name, last, final):
        nc, sch, I, S, DB = self.nc, self.sch, self.I, self.S, self.DB
        li = l // 2
        moe = (l % 2 == 1)
        nex = NEXP if moe else 2
        with ExitStack() as es:
            mod = self.load_mod(es, l, [5])
            stage, stageb = self.make_stage(es, "p3b_stg", 3, 1408)
            W1 = self.sb(es, "p3b_W1", [128, 8, DFE], BF16)
            W3 = self.sb(es, "p3b_W3", [128, 8, DFE], BF16)
            W2 = self.sb(es, "p3b_W2", [128, 11, D], BF16)
            W1b, W3b, W2b = sch.buf("W1"), sch.buf("W3"), sch.buf("W2")
            aT = [self.sb(es, f"p3b_aT{i}", [128, 8, 512], BF16) for i in range(2)]
            aTb = sch.bufs("p3b_aT", 2)
            gT = [self.sb(es, f"p3b_gT{i}", [128, 11, 512], BF16) for i in range(2)]
            gTb = sch.bufs("p3b_gT", 2)
            sl = [self.sb(es, f"p3b_sl{i}", [128, 512], F32) for i in range(2)]
            slb = sch.bufs("p3b_sl", 2)
            acc = [self.sb(es, f"p3b_acc{i}", [128, D], F32) for i in range(3)]
            accb = sch.bufs("p3b_acc", 3)
            tmp = [self.sb(es, f"p3b_tmp{i}", [128, D], F32) for i in range(2)]
            tmpb = sch.bufs("p3b_tmp", 2)
            ones, onesb = self.sb(es, "p3b_ones", [128, 1], F32), sch.buf("p3b_ones")
            sch.op("pool", lambda e: e.memset(ones[:], 1.0), writes=[onesb])
            p1 = [self.ps(es, f"p3b_p1{i}", [128, 512]) for i in range(2)]
            p1b = sch.bufs("p3b_p1", 2)
            p3 = [self.ps(es, f"p3b_p3{i}", [128, 512]) for i in range(2)]
            p3b_ = sch.bufs("p3b_p3", 2)
            py = [self.ps(es, f"p3b_py{i}", [128, 2, 512]) for i in range(2)]
            pyb = sch.bufs("p3b_py", 2)
            nst = NST if last else NST + 1
            stc = 0
            jc = 0
            tc_ = 0
            chain = [(h_mid, h_name)]
            ab = [(S["accA"], "accA"), (S["accB"], "accB")]
            for e_ in range(nex - 1):
                chain.append(ab[e_ % 2])
            chain.append((h_nxt, h_nxt_name))
            for ex in range(nex):
                src, srcn = chain[ex]
                dst, dstn = chain[ex + 1]
                if moe:
                    w1s = I["moe_w1"][li, ex].rearrange("(k p) n -> p k n", p=128)
                    w3s = I["moe_w3"][li, ex].rearrange("(k p) n -> p k n", p=128)
                    w2s = I["moe_w2"][li, ex].rearrange("(k p) n -> p k n", p=128)
                else:
                    w1s = I["ffn_w1"][li].rearrange("(k p) n -> p k n", p=128)[:, :, ex * DFE:(ex + 1) * DFE]
                    w3s = I["ffn_w3"][li].rearrange("(k p) n -> p k n", p=128)[:, :, ex * DFE:(ex + 1) * DFE]
                    w2s = I["ffn_w2"][li][ex * DFE:(ex + 1) * DFE, :].rearrange("(k p) n -> p k n", p=128)
                for (wt, wb, ws, nk, ncol) in ((W1, W1b, w1s, 8, DFE), (W3, W3b, w3s, 8, DFE), (W2, W2b, w2s, 11, D)):
                    for k in range(nk):
                        sx = stc % 3
                        stc += 1
                        sch.dma("sp", stage[sx][:, 0:ncol], ws[:, k, :], stageb[sx], writes=[stageb[sx]])
                        if stc % 2 == 0:
                            sch.op("pool", lambda e, wt=wt, k=k, sx=sx, ncol=ncol: e.tensor_copy(out=wt[:, k, :], in_=stage[sx][:, 0:ncol]),
                                   reads=[stageb[sx]], writes=[wb], partial=(k > 0))
                        else:
                            sch.op("dve", lambda e, wt=wt, k=k, sx=sx, ncol=ncol: e.tensor_copy(out=wt[:, k, :], in_=stage[sx][:, 0:ncol]),
                                   reads=[stageb[sx]], writes=[wb], partial=(k > 0))
                for st in range(nst):
                    isctx = st == NST
                    nt = 256 if isctx else 512
                    nsub = nt // 128
                    tok0 = st * 512
                    w = 1 if isctx else 0
                    sa = st % 2
                    sch.dma("sp", aT[sa][:, :, 0:nt], S["a2T"].rearrange("c p t -> p c t")[:, :, tok0:tok0 + nt], aTb[sa], reads=[DB["a2T"]], writes=[aTb[sa]])
                    g, gb_ = gT[sa], gTb[sa]
                    for j in range(11):
                        k = jc % 2
                        jc += 1
                        for kc in range(8):
                            sch.op("pe", lambda e, kc=kc: e.matmul(p1[k][:, 0:nt], lhsT=W1[:, kc, j * 128:(j + 1) * 128], rhs=aT[sa][:, kc, 0:nt],
                                                                   start=(kc == 0), stop=(kc == 7)),
                                   reads=[W1b, aTb[sa]], writes=[p1b[k]], signal=(kc == 7))
                        for kc in range(8):
                            sch.op("pe", lambda e, kc=kc: e.matmul(p3[k][:, 0:nt], lhsT=W3[:, kc, j * 128:(j + 1) * 128], rhs=aT[sa][:, kc, 0:nt],
                                                                   start=(kc == 0), stop=(kc == 7)),
                                   reads=[W3b, aTb[sa]], writes=[p3b_[k]], signal=(kc == 7))
                        sch.op("act", lambda e: e.activation(out=sl[k][:, 0:nt], in_=p1[k][:, 0:nt], func=AF.Silu), reads=[p1b[k]], writes=[slb[k]])
                        sch.op("dve", lambda e: e.tensor_tensor(out=g[:, j, 0:nt], in0=sl[k][:, 0:nt], in1=p3[k][:, 0:nt], op=ALU.mult),
                               reads=[slb[k], p3b_[k]], writes=[gb_], partial=True)
                    for sub in range(nsub):
                        t = tc_ % 2
                        t3 = tc_ % 3
                        tc_ += 1
                        r0 = tok0 + sub * 128
                        sch.dma("sp", acc[t3][:], src[r0:r0 + 128, :], accb[t3], reads=[DB[srcn]], writes=[accb[t3]])
                        for hf in range(2):
                            for j in range(11):
                                sch.op("pe", lambda e, j=j, hf=hf: e.matmul(py[t][:, hf, :], lhsT=g[:, j, sub * 128:(sub + 1) * 128], rhs=W2[:, j, hf * 512:(hf + 1) * 512],
                                                                           start=(j == 0), stop=(j == 10)),
                                       reads=[gb_, W2b], writes=[pyb[t]], signal=(j == 10 and hf == 1), partial=True)
                        g2, g2b = mod[(w, 5)]
                        if moe:
                            gsc, gscb = self.gates_sb[:, st * 4 + sub, ex:ex + 1], self.gates_b
                        else:
                            gsc, gscb = ones[:, 0:1], onesb
                        sch.op("dve", lambda e, gsc=gsc: e.scalar_tensor_tensor(out=tmp[t][:], in0=py[t][:].rearrange("p a b -> p (a b)"), scalar=gsc, in1=g2[:],
                                                                                op0=ALU.mult, op1=ALU.mult),
                               reads=[pyb[t], gscb, g2b], writes=[tmpb[t]])
                        sch.op("pool", lambda e: e.tensor_tensor(out=acc[t3][:], in0=tmp[t][:], in1=acc[t3][:], op=ALU.add),
                               reads=[tmpb[t], accb[t3]], writes=[accb[t3]])
                        if dstn == "out" and r0 >= self.out_rows:
                            continue
                        sch.dma("pool", dst[r0:r0 + 128, :], acc[t3][:], accb[t3], reads=[accb[t3]], writes=[DB[dstn]], partial=True)
                        if self.debug and ex == nex - 1 and not final:
                            pass
            sch.barrier()


def _prep_inputs(inp):
    f = lambda a: np.ascontiguousarray(np.asarray(a, dtype=np.float32))
    x, c, ctx, c_ctx = f(inp["x"]), f(inp["c"]), f(inp["ctx"]), f(inp["c_ctx"])
    shared = {}
    for k in ["ada_w", "ada_b", "norm1_g", "norm2_g", "ev_w_in", "ev_w_out", "ffn_w1", "ffn_w3", "ffn_w2",
              "od_w_in", "od_sink", "od_w_out", "moe_router", "moe_w1", "moe_w3", "moe_w2"]:
        shared[k] = f(inp[k])
    cw = f(inp["ev_conv_w"])
    shared["ev_convw"] = np.ascontiguousarray(cw.reshape(2, 3, 4, 128).transpose(0, 3, 2, 1))
    shared["ev_qg"] = np.ascontiguousarray(np.tile(f(inp["ev_q_g"]), (1, 2))[:, :, None])
    shared["ev_kg"] = np.ascontiguousarray(np.tile(f(inp["ev_k_g"]), (1, 2))[:, :, None])
    shared["od_g"] = np.ascontiguousarray(np.stack([np.tile(f(inp[k]), (1, 2)) for k in ("od_cq_g", "od_ck_g", "od_dq_g", "od_dk_g")], -1))
    shared["od_lam"] = np.ascontiguousarray(np.stack([f(inp[k]) for k in ("od_lam_q1", "od_lam_k1", "od_lam_q2", "od_lam_k2")], 1))
    shared["od_subln"] = f(inp["od_subln_g"])
    shared["moe_routerT"] = np.ascontiguousarray(f(inp["moe_router"]).transpose(0, 2, 1))
    rpb = f(inp["ev_rpb"])
    cst = _consts()
    dr, dc, _ = _na_tables()
    bias = rpb[:, :, dr, dc]
    cst["ev_bias"] = np.ascontiguousarray(bias.transpose(0, 3, 2, 1, 4))

    def split(d):
        o = {}
        for k, v in d.items():
            if k in ("ident", "blockones", "rotT", "ropeC", "ropeS", "swamask", "namask"):
                o[k] = v
            else:
                for l in range(v.shape[0]):
                    o[f"{k}_{l}"] = np.ascontiguousarray(v[l])
        return o
    shared = split(shared)
    shared.update(split(cst))
    in_maps = []
    for core in range(NCORES):
        b = core % 4
        m = dict(shared)
        m["xin"] = np.ascontiguousarray(np.concatenate([x[b], ctx[b]], 0))
        cc = np.stack([c[b].reshape(8, 128).T, c_ctx.reshape(8, 128).T], -1)
        m["cc"] = np.ascontiguousarray(cc)
        in_maps.append(m)
    return in_maps


def _run(inp, nlayers=DEPTH, debug=False):
    bld = Builder(nlayers=nlayers, debug=debug)
    nc = bld.build()
    in_maps = _prep_inputs(inp)
    names = set(bld.inputs.keys())
    in_maps = [{k: v for k, v in m.items() if k in names} for m in in_maps]
    res = run_bass_kernel_spmd(nc, in_maps, core_ids=list(range(NCORES)))
    return res.results


def kernel(**inputs):
    results = _run(inputs, DEPTH, False)
    B = 4
    out = np.empty((B, NLAT, D), np.float32)
    for b in range(B):
        out[b] = results[b]["out"]
    return out
```
